# Optimizing a Trainium2 kernel written in Bass

```python
import math
import jax, jax.numpy as jnp
from jax import lax
import numpy as np

D_MODEL = 2048
BATCH = 8
SEQ = 2048
DEPTH = 2

HEAD_DIM = 128
N_MEM = 256
MEM_HEADS = 4
MEM_WIDTH = MEM_HEADS * HEAD_DIM
MIX_WIDTH = D_MODEL - MEM_WIDTH
MLSTM_HEADS = 6
MLSTM_DV = MIX_WIDTH // MLSTM_HEADS
MLSTM_DQK = MLSTM_DV // 2
MLSTM_QK_WIDTH = MLSTM_HEADS * MLSTM_DQK
MLSTM_CHUNK = 64
CONV_WIDTH = 4
MOBA_HEADS = MIX_WIDTH // HEAD_DIM
MOBA_BLOCK = 256
MOBA_TOPK = 3
MOBA_QCHUNK = 8
ROPE_THETA = 500000.0
ROPE_DIM = HEAD_DIM // 4
N_EXPERTS = 32
TOP_K = 4
D_FF = D_MODEL
SWIGLU_LIMIT = 7.0
SWIGLU_ALPHA = 1.702
MOE_ROW_BLOCK = 256
LN_EPS = 1e-5
DEEPNORM_ALPHA = (2 * DEPTH) ** 0.25
DEEPNORM_BETA = (8 * DEPTH) ** -0.25
MLSTM_IN = 2 * MLSTM_QK_WIDTH + 2 * MIX_WIDTH + 2 * MLSTM_HEADS + MEM_WIDTH
MOBA_IN = 3 * MIX_WIDTH + MEM_WIDTH

kernel_name = "hybrid_mlstm_moba_memxattn_moe_deepnorm"


def layer_norm(x, g, b):
    xf = x.astype(jnp.float32)
    mu = xf.mean(-1, keepdims=True)
    var = jnp.square(xf - mu).mean(-1, keepdims=True)
    y = (xf - mu) * lax.rsqrt(var + LN_EPS)
    return (y * g.astype(jnp.float32) + b.astype(jnp.float32)).astype(x.dtype)


def partial_rotary(t, pos):
    half = ROPE_DIM // 2
    inv_freq = ROPE_THETA ** (-jnp.arange(half, dtype=jnp.float32) * 2.0 / ROPE_DIM)
    ang = pos.astype(jnp.float32)[:, None] * inv_freq[None, :]
    cos = jnp.cos(ang)[None, :, None, :]
    sin = jnp.sin(ang)[None, :, None, :]
    tf = t.astype(jnp.float32)
    t1, t2, rest = tf[..., :half], tf[..., half:ROPE_DIM], tf[..., ROPE_DIM:]
    out = jnp.concatenate([t1 * cos - t2 * sin, t2 * cos + t1 * sin, rest], axis=-1)
    return out.astype(t.dtype)


def causal_depthwise_conv(u, w, b):
    c = u.shape[-1]
    y = lax.conv_general_dilated(u, w[:, None, :].astype(u.dtype), window_strides=(1,),
                                 padding=[(CONV_WIDTH - 1, 0)],
                                 dimension_numbers=('NWC', 'WIO', 'NWC'),
                                 feature_group_count=c)
    return y + b.astype(u.dtype)


def memory_cross_attention(qm, mem, w_mem_kv):
    bsz, s, _ = qm.shape
    kv = mem @ w_mem_kv
    k, v = jnp.split(kv, 2, axis=-1)
    q = qm.reshape(bsz, s, MEM_HEADS, HEAD_DIM)
    k = k.reshape(bsz, N_MEM, MEM_HEADS, HEAD_DIM)
    v = v.reshape(bsz, N_MEM, MEM_HEADS, HEAD_DIM)
    sc = jnp.einsum('bshd,bmhd->bhsm', q, k).astype(jnp.float32) * (HEAD_DIM ** -0.5)
    p = jax.nn.softmax(sc, axis=-1).astype(v.dtype)
    o = jnp.einsum('bhsm,bmhd->bshd', p, v)
    return o.reshape(bsz, s, MEM_WIDTH)


def mlstm_chunkwise(q, k, v, ig, fg):
    f32 = jnp.float32
    bsz, nh, s, dqk = q.shape
    dv = v.shape[-1]
    L = MLSTM_CHUNK
    nc = s // L
    qc = q.astype(f32).reshape(bsz, nh, nc, L, dqk)
    kc = (k.astype(f32) * (dqk ** -0.5)).reshape(bsz, nh, nc, L, dqk)
    vc = v.astype(f32).reshape(bsz, nh, nc, L, dv)
    log_f = jax.nn.log_sigmoid(fg).reshape(bsz, nh, nc, L)
    log_i = ig.reshape(bsz, nh, nc, L)
    bcum = jnp.cumsum(log_f, axis=-1)
    g = bcum[..., -1]
    a = g[..., None] - bcum + log_i

    def step(carry, xs):
        c_st, n_st, m_st = carry
        a_c, g_c, k_c, v_c = xs
        m_new = jnp.maximum(g_c + m_st, a_c.max(-1))
        decay = jnp.exp(g_c + m_st - m_new)
        w = jnp.exp(a_c - m_new[..., None])
        c_new = decay[..., None, None] * c_st + jnp.einsum('bhl,bhld,bhle->bhde', w, k_c, v_c)
        n_new = decay[..., None] * n_st + jnp.einsum('bhl,bhld->bhd', w, k_c)
        return (c_new, n_new, m_new), (c_st, n_st, m_st)

    init = (jnp.zeros((bsz, nh, dqk, dv), f32), jnp.zeros((bsz, nh, dqk), f32),
            jnp.zeros((bsz, nh), f32))
    xs = (jnp.moveaxis(a, 2, 0), jnp.moveaxis(g, 2, 0), jnp.moveaxis(kc, 2, 0), jnp.moveaxis(vc, 2, 0))
    _, (c_prev, n_prev, m_prev) = lax.scan(step, init, xs)
    c_prev = jnp.moveaxis(c_prev, 0, 2)
    n_prev = jnp.moveaxis(n_prev, 0, 2)
    m_prev = jnp.moveaxis(m_prev, 0, 2)

    inter_log = bcum + m_prev[..., None]
    causal = jnp.tril(jnp.ones((L, L), dtype=bool))
    dmat = bcum[..., :, None] - bcum[..., None, :] + log_i[..., None, :]
    dmat = jnp.where(causal, dmat, -jnp.inf)
    m_t = jnp.maximum(inter_log, dmat.max(-1))
    s_qk = jnp.einsum('bhcld,bhcjd->bhclj', qc, kc) * jnp.exp(dmat - m_t[..., None])
    inter_w = jnp.exp(inter_log - m_t)
    num = (inter_w[..., None] * jnp.einsum('bhcld,bhcde->bhcle', qc, c_prev)
           + jnp.einsum('bhclj,bhcje->bhcle', s_qk, vc))
    den = inter_w * jnp.einsum('bhcld,bhcd->bhcl', qc, n_prev) + s_qk.sum(-1)
    h = num / jnp.maximum(jnp.abs(den), jnp.exp(-m_t))[..., None]
    return h.reshape(bsz, nh, s, dv)


def mlstm_mixer(z, conv_w, conv_b, b_igate, b_fgate):
    bsz, s, _ = z.shape
    f32 = jnp.float32
    qk_raw, v, o_pre, gates, qm = jnp.split(
        z, [2 * MLSTM_QK_WIDTH, 2 * MLSTM_QK_WIDTH + MIX_WIDTH,
            2 * MLSTM_QK_WIDTH + 2 * MIX_WIDTH, 2 * MLSTM_QK_WIDTH + 2 * MIX_WIDTH + 2 * MLSTM_HEADS], axis=-1)
    qk = jax.nn.silu(causal_depthwise_conv(qk_raw, conv_w, conv_b))
    q, k = jnp.split(qk, 2, axis=-1)

    def heads(t, d):
        return t.reshape(bsz, s, MLSTM_HEADS, d).transpose(0, 2, 1, 3)

    ig = (gates[..., :MLSTM_HEADS].astype(f32) + b_igate.astype(f32)).transpose(0, 2, 1)
    fg = (gates[..., MLSTM_HEADS:].astype(f32) + b_fgate.astype(f32)).transpose(0, 2, 1)
    h = mlstm_chunkwise(heads(q, MLSTM_DQK), heads(k, MLSTM_DQK), heads(v, MLSTM_DV), ig, fg)
    h = h.transpose(0, 2, 1, 3).reshape(bsz, s, MIX_WIDTH).astype(z.dtype)
    return jax.nn.sigmoid(o_pre) * h, qm


def moba_attention(q, k, v):
    f32 = jnp.float32
    bsz, s, nh, hd = q.shape
    nb = -(-s // MOBA_BLOCK)
    sp = nb * MOBA_BLOCK
    topk = min(MOBA_TOPK, nb)
    scale = hd ** -0.5
    qh = q.transpose(0, 2, 1, 3)
    pad = ((0, 0), (0, 0), (0, sp - s), (0, 0))
    kb = jnp.pad(k.transpose(0, 2, 1, 3), pad).reshape(bsz, nh, nb, MOBA_BLOCK, hd)
    vb = jnp.pad(v.transpose(0, 2, 1, 3), pad).reshape(bsz, nh, nb, MOBA_BLOCK, hd)

    kbar = kb.astype(f32).mean(axis=3)
    gate = jnp.einsum('bhsd,bhnd->bhsn', qh.astype(f32), kbar)
    q_blk = jnp.arange(s) // MOBA_BLOCK
    fully_past = jnp.arange(nb)[None, :] < q_blk[:, None]
    gate = jnp.where(fully_past, gate, -jnp.inf)
    gval, gidx = lax.top_k(gate, topk)
    gvalid = jnp.isfinite(gval)

    qc_n = MOBA_QCHUNK
    nq = s // qc_n
    q_chunks = qh.reshape(bsz, nh, nq, qc_n, hd).transpose(2, 0, 1, 3, 4)
    idx_chunks = gidx.reshape(bsz, nh, nq, qc_n, topk).transpose(2, 0, 1, 3, 4)
    val_chunks = gvalid.reshape(bsz, nh, nq, qc_n, topk).transpose(2, 0, 1, 3, 4)
    bi = jnp.arange(bsz)[:, None, None, None]
    hi = jnp.arange(nh)[None, :, None, None]
    kpos = jnp.arange(MOBA_BLOCK)

    def chunk(args):
        ci, qc, idx, valid = args
        k_sel = kb[bi, hi, idx]
        v_sel = vb[bi, hi, idx]
        s_sel = jnp.einsum('bhqd,bhqnkd->bhqnk', qc, k_sel).astype(f32) * scale
        s_sel = jnp.where(valid[..., None], s_sel, -jnp.inf).reshape(bsz, nh, qc_n, topk * MOBA_BLOCK)
        start = ci * qc_n
        own = start // MOBA_BLOCK
        k_own = lax.dynamic_index_in_dim(kb, own, axis=2, keepdims=False)
        v_own = lax.dynamic_index_in_dim(vb, own, axis=2, keepdims=False)
        qpos = start + jnp.arange(qc_n) - own * MOBA_BLOCK
        s_own = jnp.einsum('bhqd,bhkd->bhqk', qc, k_own).astype(f32) * scale
        s_own = jnp.where(kpos[None, :] <= qpos[:, None], s_own, -jnp.inf)
        p = jax.nn.softmax(jnp.concatenate([s_sel, s_own], axis=-1), axis=-1).astype(v.dtype)
        p_sel = p[..., :topk * MOBA_BLOCK].reshape(bsz, nh, qc_n, topk, MOBA_BLOCK)
        p_own = p[..., topk * MOBA_BLOCK:]
        return (jnp.einsum('bhqnk,bhqnkd->bhqd', p_sel, v_sel)
                + jnp.einsum('bhqk,bhkd->bhqd', p_own, v_own))

    out = lax.map(chunk, (jnp.arange(nq), q_chunks, idx_chunks, val_chunks))
    return out.transpose(1, 0, 3, 2, 4).reshape(bsz, s, nh * hd)


def moe_ffn(x, w_router, b_router, w_gu, b_gu, w_down, b_down):
    bsz, s, d = x.shape
    f32 = jnp.float32
    xf = x.reshape(-1, d)
    n = xf.shape[0]
    logits = (xf @ w_router + b_router).astype(f32)
    top_logit, top_e = lax.top_k(logits, TOP_K)
    gate = jax.nn.softmax(top_logit, axis=-1)
    nk = n * TOP_K
    e_flat = top_e.reshape(-1)
    tok_flat = jnp.repeat(jnp.arange(n, dtype=jnp.int32), TOP_K)
    g_flat = gate.reshape(-1)
    order = jnp.argsort(e_flat)
    e_sorted = e_flat[order]
    counts = jnp.bincount(e_flat, length=N_EXPERTS)
    padded = (counts + MOE_ROW_BLOCK - 1) // MOE_ROW_BLOCK * MOE_ROW_BLOCK
    start = jnp.cumsum(counts) - counts
    pend = jnp.cumsum(padded)
    pstart = pend - padded
    dest = pstart[e_sorted] + jnp.arange(nk) - start[e_sorted]
    nblk = -(-nk // MOE_ROW_BLOCK) + N_EXPERTS
    p_rows = nblk * MOE_ROW_BLOCK
    buf_tok = jnp.zeros((p_rows,), jnp.int32).at[dest].set(tok_flat[order])
    buf_gate = jnp.zeros((p_rows,), f32).at[dest].set(g_flat[order])
    blk_e = jnp.minimum(jnp.searchsorted(pend, jnp.arange(nblk) * MOE_ROW_BLOCK, side='right'),
                        N_EXPERTS - 1)
    xb = xf[buf_tok].reshape(nblk, MOE_ROW_BLOCK, d)

    def expert_block(args):
        e, xr = args
        hgu = xr @ w_gu[e] + b_gu[e]
        hg, hu = jnp.split(hgu, 2, axis=-1)
        hg = jnp.minimum(hg, SWIGLU_LIMIT)
        hu = jnp.clip(hu, -SWIGLU_LIMIT, SWIGLU_LIMIT)
        hid = (hu + 1.0) * (hg * jax.nn.sigmoid(SWIGLU_ALPHA * hg))
        return hid @ w_down[e] + b_down[e]

    yb = lax.map(expert_block, (blk_e, xb)).reshape(p_rows, d)
    out = jnp.zeros((n, d), x.dtype).at[buf_tok].add(yb * buf_gate[:, None].astype(yb.dtype))
    return out.reshape(bsz, s, d)


def setup_inputs(seed: int = 0) -> dict:
    key = jax.random.key(seed)
    ks = iter(jax.random.split(key, 32))
    f32 = jnp.float32
    n_ml = (DEPTH + 1) // 2
    n_mb = DEPTH // 2

    def nrm(shape, scale):
        return jax.random.normal(next(ks), shape, f32) * scale

    return {
        "x": nrm((BATCH, SEQ, D_MODEL), 1.0),
        "mem": nrm((BATCH, N_MEM, D_MODEL), 1.0),
        "mlstm_w_in": nrm((n_ml, D_MODEL, MLSTM_IN), D_MODEL ** -0.5),
        "mlstm_conv_w": nrm((n_ml, CONV_WIDTH, 2 * MLSTM_QK_WIDTH), CONV_WIDTH ** -0.5),
        "mlstm_conv_b": nrm((n_ml, 2 * MLSTM_QK_WIDTH), 0.01),
        "mlstm_b_igate": nrm((n_ml, MLSTM_HEADS), 0.1),
        "mlstm_b_fgate": 3.0 + 3.0 * jax.random.uniform(next(ks), (n_ml, MLSTM_HEADS), f32),
        "moba_w_in": nrm((n_mb, D_MODEL, MOBA_IN), D_MODEL ** -0.5),
        "w_mem_kv": nrm((DEPTH, D_MODEL, 2 * MEM_WIDTH), D_MODEL ** -0.5),
        "w_out": nrm((DEPTH, MIX_WIDTH + MEM_WIDTH, D_MODEL), DEEPNORM_BETA * D_MODEL ** -0.5),
        "ln1_g": 1.0 + nrm((DEPTH, D_MODEL), 0.02),
        "ln1_b": nrm((DEPTH, D_MODEL), 0.02),
        "w_router": nrm((DEPTH, D_MODEL, N_EXPERTS), D_MODEL ** -0.5),
        "b_router": nrm((DEPTH, N_EXPERTS), 0.01),
        "w_gu": nrm((DEPTH, N_EXPERTS, D_MODEL, 2 * D_FF), D_MODEL ** -0.5),
        "b_gu": nrm((DEPTH, N_EXPERTS, 2 * D_FF), 0.01),
        "w_down": nrm((DEPTH, N_EXPERTS, D_FF, D_MODEL), DEEPNORM_BETA * D_FF ** -0.5),
        "b_down": nrm((DEPTH, N_EXPERTS, D_MODEL), 0.01),
        "ln2_g": 1.0 + nrm((DEPTH, D_MODEL), 0.02),
        "ln2_b": nrm((DEPTH, D_MODEL), 0.02),
    }


def reference(x, mem, mlstm_w_in, mlstm_conv_w, mlstm_conv_b, mlstm_b_igate, mlstm_b_fgate,
              moba_w_in, w_mem_kv, w_out, ln1_g, ln1_b, w_router, b_router, w_gu, b_gu,
              w_down, b_down, ln2_g, ln2_b):
    bsz, s, _ = x.shape
    pos = jnp.arange(s)
    for i in range(DEPTH):
        j = i // 2
        if i % 2 == 0:
            z = x @ mlstm_w_in[j]
            h_mix, qm = mlstm_mixer(z, mlstm_conv_w[j], mlstm_conv_b[j], mlstm_b_igate[j], mlstm_b_fgate[j])
        else:
            z = x @ moba_w_in[j]
            q, k, v, qm = jnp.split(z, [MIX_WIDTH, 2 * MIX_WIDTH, 3 * MIX_WIDTH], axis=-1)
            q = partial_rotary(q.reshape(bsz, s, MOBA_HEADS, HEAD_DIM), pos)
            k = partial_rotary(k.reshape(bsz, s, MOBA_HEADS, HEAD_DIM), pos)
            v = v.reshape(bsz, s, MOBA_HEADS, HEAD_DIM)
            h_mix = moba_attention(q, k, v)
        h_mem = memory_cross_attention(qm, mem, w_mem_kv[i])
        y = jnp.concatenate([h_mix, h_mem], axis=-1) @ w_out[i]
        x = layer_norm(DEEPNORM_ALPHA * x + y, ln1_g[i], ln1_b[i])
        f = moe_ffn(x, w_router[i], b_router[i], w_gu[i], b_gu[i], w_down[i], b_down[i])
        x = layer_norm(DEEPNORM_ALPHA * x + f, ln2_g[i], ln2_b[i])
    return x
```

```python
import contextlib
import numpy as np
import ml_dtypes
import concourse.bass as bass
import concourse.mybir as mybir
from concourse.bass_utils import run_bass_kernel_spmd

F32 = mybir.dt.float32
BF16 = mybir.dt.bfloat16
I32 = mybir.dt.int32
ALU = mybir.AluOpType
AF = mybir.ActivationFunctionType
AX = mybir.AxisListType
NPBF = ml_dtypes.bfloat16

S = 2048
D = 2048
NT = 16
NE = 32
CAP = 384
NROWS = NE * CAP
ALPHA = 4 ** 0.25
EPS = 1e-5
BIGM = 30000.0
NCORES = 8


class Prog:
    ENGS = ("pe", "act", "dve", "pool", "sp")

    def __init__(self, nc, n_dma_sems=14):
        self.nc = nc
        self.ops = []
        self.eng_ops = {e: [] for e in self.ENGS}
        self.state = {}
        self.n_dma_sems = n_dma_sems
        self.dma_rr = {e: 0 for e in self.ENGS}
        self.dma_last = {}
        self.last_on = {}

    def op(self, eng, fn, reads=(), writes=(), dma=False, extra=()):
        oid = len(self.ops)
        deps = set()
        for k in reads:
            st = self.state.get(k)
            if st is not None:
                for d in st[0].values():
                    deps.add((d, "raw"))
        for k in writes:
            st = self.state.get(k)
            if st is not None:
                for d in st[0].values():
                    deps.add((d, "waw"))
                for d in st[1].values():
                    deps.add((d, "war"))
        o = dict(id=oid, eng=eng, fn=fn, dma=dma, deps=set(extra), needed=False, slot=None)
        for (d, kind) in deps:
            do = self.ops[d]
            if not do["dma"] and not dma and do["eng"] == eng:
                if eng == "pe" or kind != "raw":
                    continue
            o["deps"].add(d)
        ek = (eng, None)
        if dma:
            slot = self.dma_rr[eng] % self.n_dma_sems
            self.dma_rr[eng] += 1
            o["slot"] = slot
            ek = (eng, slot)
            prev = self.dma_last.get((eng, slot))
            if prev is not None:
                o["deps"].add(prev)
            self.dma_last[(eng, slot)] = oid
        for d in o["deps"]:
            self.ops[d]["needed"] = True
        self.ops.append(o)
        self.eng_ops[eng].append(oid)
        if fn is not None and not dma:
            self.last_on[eng] = oid
        for k in reads:
            st = self.state.setdefault(k, [{}, {}])
            st[1][ek] = oid
        for k in writes:
            st = self.state.setdefault(k, [{}, {}])
            st[0][ek] = oid
        return oid

    def barrier(self):
        deps = set(self.last_on.values()) | set(self.dma_last.values())
        for e in self.ENGS:
            self.op(e, None, extra=[d for d in deps])
        self.state = {}

    def emit(self, final_wait_ops=()):
        nc = self.nc
        with contextlib.ExitStack() as es:
            sem_eng = {e: es.enter_context(nc.semaphore("s_" + e)) for e in self.ENGS}
            sem_dma = {}
            for e in ("sp", "act", "pool"):
                for s in range(min(self.n_dma_sems, self.dma_rr[e])):
                    sem_dma[(e, s)] = es.enter_context(nc.semaphore(f"d_{e}{s}"))
            cnt = {e: 0 for e in self.ENGS}
            dcnt = {}
            for o in self.ops:
                if o["dma"]:
                    key = (o["eng"], o["slot"])
                    dcnt[key] = dcnt.get(key, 0) + 16
                    o["tok"] = (sem_dma[key], dcnt[key], key)
                elif o["needed"]:
                    assert o["fn"] is not None
                    cnt[o["eng"]] += 1
                    o["tok"] = (sem_eng[o["eng"]], cnt[o["eng"]], o["eng"])
                else:
                    o["tok"] = None
            final = [self.ops[i]["tok"] for i in final_wait_ops]
            block = es.enter_context(nc.Block())
            regs = {"pe": block.tensor, "act": block.scalar, "dve": block.vector,
                    "pool": block.gpsimd, "sp": block.sync}
            for e in self.ENGS:
                def body(engine, e=e):
                    waited = {}
                    if e == "pool":
                        self.bc_reg = engine.to_reg(NROWS - 1)
                    for oid in self.eng_ops[e]:
                        o = self.ops[oid]
                        need = {}
                        for d in o["deps"]:
                            sem, val, key = self.ops[d]["tok"]
                            if waited.get(key, 0) >= val:
                                continue
                            if need.get(key, (None, 0))[1] < val:
                                need[key] = (sem, val)
                        for key, (sem, val) in need.items():
                            engine.wait_ge(sem, val)
                            waited[key] = val
                        if o["fn"] is None:
                            continue
                        ins = o["fn"](engine)
                        if o["tok"] is not None:
                            ins.then_inc(o["tok"][0], 16 if o["dma"] else 1)
                    if e == "sp":
                        for (sem, val, key) in final:
                            if waited.get(key, 0) < val:
                                engine.wait_ge(sem, val)
                                waited[key] = val
                regs[e](body)


SB_BASE = 16512
SB_LIMIT = 229376


class Scope:
    def __init__(self, top):
        self.top = top


class Ctx:
    def __init__(self, nc):
        self.nc = nc
        self.P = Prog(nc)
        self.es = contextlib.ExitStack()
        self.root = Scope(SB_BASE)
        self.uid = 0
        self.outs = []

    def dram(self, name, shape, dt, kind):
        return self.nc.dram_tensor(name, list(shape), dt, kind=kind).ap()

    def sb(self, sc, name, shape, dt):
        n = 1
        for d in shape[1:]:
            n *= d
        size = n * (4 if dt in (F32, I32) else 2)
        size = (size + 31) // 32 * 32
        off = sc.top
        sc.top += size
        assert sc.top <= SB_LIMIT, (name, sc.top)
        return self.nc.alloc_sbuf_tensor_at(name, list(shape), dt, offset=off)

    @contextlib.contextmanager
    def scope(self, parent):
        yield Scope(parent.top)

    def ps(self, name, shape, dt):
        return self.es.enter_context(self.nc.psum_tensor(name, list(shape), dt))

    def dma(self, q, out, in_, r, w):
        return self.P.op(q, lambda e: e.dma_start(out=out, in_=in_), reads=r, writes=w, dma=True)

    def mm(self, out, lhsT, rhs, start, stop, r, w):
        return self.P.op("pe", lambda e: e.matmul(out, lhsT=lhsT, rhs=rhs, start=start, stop=stop), reads=r, writes=w)

    def tr(self, out, in_, ident, r, w):
        return self.P.op("pe", lambda e: e.transpose(out, in_, ident), reads=r, writes=w)

    def act(self, out, in_, func, r, w, bias=0.0, scale=1.0, accum_out=None):
        if accum_out is None:
            f = lambda e: e.activation(out=out, in_=in_, func=func, bias=bias, scale=scale)
        else:
            f = lambda e: e.activation(out=out, in_=in_, func=func, bias=bias, scale=scale, accum_out=accum_out)
        return self.P.op("act", f, reads=r, writes=w)

    def ts(self, eng, out, in0, s1, s2, op0, op1, r, w):
        if op1 is None:
            f = lambda e: e.tensor_scalar(out=out, in0=in0, scalar1=s1, scalar2=None, op0=op0)
        else:
            f = lambda e: e.tensor_scalar(out=out, in0=in0, scalar1=s1, scalar2=s2, op0=op0, op1=op1)
        return self.P.op(eng, f, reads=r, writes=w)

    def tt(self, eng, out, in0, in1, op, r, w):
        return self.P.op(eng, lambda e: e.tensor_tensor(out=out, in0=in0, in1=in1, op=op), reads=r, writes=w)

    def stt(self, out, in0, scalar, in1, op0, op1, r, w, accum_out=None):
        if accum_out is None:
            f = lambda e: e.scalar_tensor_tensor(out=out, in0=in0, scalar=scalar, in1=in1, op0=op0, op1=op1)
        else:
            f = lambda e: e.scalar_tensor_tensor(out=out, in0=in0, scalar=scalar, in1=in1, op0=op0, op1=op1, accum_out=accum_out)
        return self.P.op("dve", f, reads=r, writes=w)

    def cp(self, eng, out, in_, r, w):
        if eng == "act":
            return self.P.op("act", lambda e: e.activation(out=out, in_=in_, func=AF.Copy), reads=r, writes=w)
        return self.P.op(eng, lambda e: e.tensor_copy(out=out, in_=in_), reads=r, writes=w)

    def memset(self, eng, ap, val, w):
        return self.P.op(eng, lambda e: e.memset(ap, val), writes=w)

    def recip(self, out, in_, r, w):
        return self.P.op("dve", lambda e: e.reciprocal(out=out, in_=in_), reads=r, writes=w)


def host_consts():
    c = {}
    c["ident_bf"] = np.eye(128, dtype=np.float32).astype(NPBF)
    c["ident_f"] = np.eye(128, dtype=np.float32)
    j = np.arange(128)[:, None]
    l = np.arange(128)[None, :]
    c["tri_incl_f"] = (j <= l).astype(np.float32)
    c["tri_strict_bf"] = (j < l).astype(np.float32).astype(NPBF)
    c["ones_f"] = np.ones((128, 128), np.float32)
    c["ones_bf"] = np.ones((128, 128), np.float32).astype(NPBF)
    q = np.arange(512)[None, None, :]
    r = np.arange(4)[None, :, None]
    jj = np.arange(128)[:, None, None]
    c["causal_bf"] = ((128 * r + jj) <= q).astype(np.float32).astype(NPBF)
    half = 16
    inv = 500000.0 ** (-np.arange(half, dtype=np.float32) * 2.0 / 32.0)
    ang = np.arange(S, dtype=np.float32)[None, :] * inv[:, None]
    cos = np.cos(ang).astype(np.float32)
    sin = np.sin(ang).astype(np.float32)
    c["rope_cos"] = np.concatenate([cos, cos], 0)
    c["rope_sin"] = np.concatenate([-sin, sin], 0)
    R = np.zeros((128, 128), np.float32)
    for i in range(16):
        R[i + 16, i] = 1.0
        R[i, i + 16] = 1.0
    c["rot_f"] = R
    vm = np.zeros((128, 16, 8), np.float32)
    for tt in range(16):
        vm[:, tt, : tt // 2] = 1.0
    c["moba_valid"] = vm
    c["moba_neg"] = ((1.0 - vm) * -1e30).astype(np.float32)
    es = np.zeros((128, 8, 128), np.float32)
    for n in range(8):
        es[n, n, :] = 1.0
    c["esel_bf"] = es.astype(NPBF)
    c["ebase"] = np.broadcast_to((np.arange(NE, dtype=np.float32) * CAP)[None, :], (128, NE)).copy()
    return c


CONST_SPECS = {
    "ident_bf": ([128, 128], BF16), "ident_f": ([128, 128], F32), "tri_incl_f": ([128, 128], F32),
    "tri_strict_bf": ([128, 128], BF16), "ones_f": ([128, 128], F32), "ones_bf": ([128, 128], BF16),
    "causal_bf": ([128, 4, 512], BF16), "rope_cos": ([32, S], F32), "rope_sin": ([32, S], F32),
    "rot_f": ([128, 128], F32), "moba_valid": ([128, 16, 8], F32), "moba_neg": ([128, 16, 8], F32),
    "esel_bf": ([128, 8, 128], BF16), "ebase": ([128, NE], F32),
}


def load_consts(C, es, names):
    t = {}
    for n in names:
        shape, dt = CONST_SPECS[n]
        d = C.dram("c_" + n, shape, dt, "ExternalInput")
        s = C.sb(es, "k_" + n, shape, dt)
        C.dma("sp", s[:], d, [], ["k_" + n])
        t[n] = s
    return t


def layer_norm_tile(C, K, r, gbc, bbc, out, tagr, tagw):
    st, mv, sd = K["ln_st"], K["ln_mv"], K["ln_sd"]
    for i in range(4):
        C.P.op("dve", lambda e, i=i: e.bn_stats(out=st[:, 6 * i:6 * i + 6], in_=r[:, 512 * i:512 * i + 512]),
               reads=[tagr], writes=["ln_st"])
    C.P.op("dve", lambda e: e.bn_aggr(out=mv[:], in_=st[:]), reads=["ln_st"], writes=["ln_mv"])
    C.act(sd[:], mv[:, 1:2], AF.Sqrt, ["ln_mv", "c_eps"], ["ln_sd"], bias=K["eps"][:], scale=1.0)
    C.recip(sd[:], sd[:], ["ln_sd"], ["ln_sd"])
    C.ts("dve", out, r, mv[:, 0:1], sd[:, 0:1], ALU.subtract, ALU.mult, [tagr, "ln_mv", "ln_sd"], [tagw])
    C.tt("pool", out, out, gbc, ALU.mult, [tagw, "lnp"], [tagw])
    C.tt("pool", out, out, bbc, ALU.add, [tagw, "lnp"], [tagw])


def emit_xT(C, K, src_bf, tt, xT, tag_src):
    for g in range(4):
        pt = K["ptr"][g % 2]
        for j in range(4):
            kc = 4 * g + j
            C.tr(pt[:, j, :], src_bf[:, 128 * kc:128 * kc + 128], K["ident_bf"][:], [tag_src, "k_ident_bf"], [f"ptr{g%2}"])
        C.cp("dve" if g % 2 == 0 else "act", xT[:, 4 * g:4 * g + 4, 128 * tt:128 * tt + 128], pt[:], [f"ptr{g%2}"], ["xT"])


def mem_attention(C, K, es, xT, hT_d, memd, wkv_d, w_in_d, qm_col0):
    P = C.P
    pb = K["pb"]
    wq = C.sb(es, "ma_wq", [128, 16, 512], BF16)
    wkv_v = wkv_d.rearrange("(kc p) c -> p kc c", p=128)
    w_in_v = w_in_d.rearrange("(kc p) c -> p kc c", p=128)
    C.dma("pool", wq[:], w_in_v[:, :, qm_col0:qm_col0 + 512], [], ["ma_wq"])
    wkv = C.sb(es, "ma_wkv", [128, 16, 1024], BF16)
    C.dma("pool", wkv[:, :, 0:512], wkv_v[:, :, 0:512], [], ["ma_wkv0"])
    C.dma("pool", wkv[:, :, 512:1024], wkv_v[:, :, 512:1024], [], ["ma_wkv1"])
    mem_bf = C.sb(es, "ma_mem", [128, 2, 2048], BF16)
    memT = C.sb(es, "ma_memT", [128, 16, 256], BF16)
    for mt in range(2):
        C.dma("pool", mem_bf[:, mt, :], memd[128 * mt:128 * mt + 128, :], [], [f"ma_mem{mt}"])
        for g in range(4):
            pt = K["ptr"][g % 2]
            for j in range(4):
                kc = 4 * g + j
                C.tr(pt[:, j, :], mem_bf[:, mt, 128 * kc:128 * kc + 128], K["ident_bf"][:], [f"ma_mem{mt}", "k_ident_bf"], [f"ptr{g%2}"])
            C.cp("dve", memT[:, 4 * g:4 * g + 4, 128 * mt:128 * mt + 128], pt[:], [f"ptr{g%2}"], ["ma_memT"])
    kmT = C.sb(es, "ma_kmT", [128, 4, 256], BF16)
    vm = C.sb(es, "ma_vm", [128, 2, 512], BF16)
    qmT = C.sb(es, "ma_qmT", [128, 4, 2048], BF16)
    for h in range(4):
        for kc in range(16):
            C.mm(pb[0][:, 0:256], wkv[:, kc, 128 * h:128 * h + 128], memT[:, kc, :], kc == 0, kc == 15, ["ma_wkv0", "ma_memT"], ["pb0"])
        C.cp("act", kmT[:, h, :], pb[0][:, 0:256], ["pb0"], ["ma_kmT"])
    for mt in range(2):
        for kc in range(16):
            C.mm(pb[1][:], memT[:, kc, 128 * mt:128 * mt + 128], wkv[:, kc, 512:1024], kc == 0, kc == 15, ["ma_wkv1", "ma_memT"], ["pb1"])
        C.cp("act", vm[:, mt, :], pb[1][:], ["pb1"], ["ma_vm"])
    for h in range(4):
        for tg in range(4):
            b = pb[(h * 4 + tg) % 2]
            bk = f"pb{(h * 4 + tg) % 2}"
            for kc in range(16):
                C.mm(b[:], wq[:, kc, 128 * h:128 * h + 128], xT[:, kc, 512 * tg:512 * tg + 512], kc == 0, kc == 15, ["ma_wq", "xT"], [bk])
            C.cp("dve" if tg % 2 else "act", qmT[:, h, 512 * tg:512 * tg + 512], b[:], [bk], ["ma_qmT"])
    eT = C.sb(es, "ma_eT", [128, 2, 512], BF16)
    rdn = C.sb(es, "ma_rdn", [128, 512], F32)
    hst = [C.sb(es, f"ma_hst{i}", [128, 512], BF16) for i in range(2)]
    scale = 128 ** -0.5
    it = 0
    for h in range(4):
        for tg in range(4):
            for mt in range(2):
                C.mm(pb[2 + mt][:], kmT[:, h, 128 * mt:128 * mt + 128], qmT[:, h, 512 * tg:512 * tg + 512], True, True, ["ma_kmT", "ma_qmT"], [f"pb{2+mt}"])
                C.act(eT[:, mt, :], pb[2 + mt][:], AF.Exp, [f"pb{2+mt}"], [f"ma_eT{mt}"], scale=scale)
            for mt in range(2):
                C.mm(pb[4][:], vm[:, mt, 128 * h:128 * h + 128], eT[:, mt, :], mt == 0, mt == 1, ["ma_vm", f"ma_eT{mt}"], ["pb4"])
            for mt in range(2):
                C.mm(pb[5][:], K["ones_bf"][:], eT[:, mt, :], mt == 0, mt == 1, ["k_ones_bf", f"ma_eT{mt}"], ["pb5"])
            C.recip(rdn[:], pb[5][:], ["pb5"], ["ma_rdn"])
            hs = hst[it % 2]
            C.tt("dve", hs[:], pb[4][:], rdn[:], ALU.mult, ["pb4", "ma_rdn"], [f"ma_hst{it%2}"])
            C.dma("sp", hT_d[1536 + 128 * h:1536 + 128 * h + 128, 512 * tg:512 * tg + 512], hs[:], [f"ma_hst{it%2}"], ["hT_d"])
            it += 1


def _interleave(ga, gb):
    da = db = False
    while not (da and db):
        if not da:
            try:
                next(ga)
            except StopIteration:
                da = True
        if not db:
            try:
                next(gb)
            except StopIteration:
                db = True


def _empty():
    return
    yield


def mixer_mlstm(C, K, es, xT, hT_d, w_in_d, convw_d, convb_d, gbias_d):
    pb = K["pb"]
    w_in_v = w_in_d.rearrange("(kc p) c -> p kc c", p=128)
    cw = C.sb(es, "ml_cw", [128, 12, 4], F32)
    cb = C.sb(es, "ml_cb", [128, 12], F32)
    gb = C.sb(es, "ml_gb", [128, 12], F32)
    C.dma("sp", cw[:], convw_d, [], ["ml_cw"])
    C.dma("sp", cb[:], convb_d, [], ["ml_cb"])
    C.dma("sp", gb[:], gbias_d, [], ["ml_gb"])
    wg = C.sb(es, "ml_wg", [128, 16, 12], BF16)
    C.dma("pool", wg[:], w_in_v[:, :, 4608:4620], [], ["ml_wg"])
    gts = C.sb(es, "ml_gts", [128, 16, 12], F32)
    for tt in range(NT):
        for kc in range(16):
            C.mm(pb[0][:, 12 * tt:12 * tt + 12], xT[:, kc, 128 * tt:128 * tt + 128], wg[:, kc, :], kc == 0, kc == 15, ["xT", "ml_wg"], ["pb0"])
    for tt in range(NT):
        C.tt("dve", gts[:, tt, :], pb[0][:, 12 * tt:12 * tt + 12], gb[:], ALU.add, ["pb0", "ml_gb"], ["ml_gts"])
    lf = C.sb(es, "ml_lf", [128, 16, 6], F32)
    C.act(lf[:], gts[:, :, 6:12], AF.Exp, ["ml_gts"], ["ml_lf"], scale=-1.0)
    C.act(lf[:], lf[:], AF.Ln, ["ml_lf", "c_one"], ["ml_lf"], bias=K["one"][:], scale=1.0)
    lf2 = lf[:].rearrange("p a b -> p (a b)")
    C.mm(pb[1][:, 0:96], K["tri_incl_f"][:], lf2, True, True, ["k_tri_incl_f", "ml_lf"], ["pb1"])
    C.mm(pb[1][:, 128:224], K["ones_f"][:], lf2, True, True, ["k_ones_f", "ml_lf"], ["pb1"])
    ksc = C.sb(es, "ml_ksc", [128, 96], F32)
    qsc = C.sb(es, "ml_qsc", [128, 96], F32)
    egd = C.sb(es, "ml_eg", [128, 96], F32)
    tmp96 = C.sb(es, "ml_t96", [128, 16, 6], F32)
    C.tt("dve", tmp96[:], gts[:, :, 0:6], pb[1][:, 0:96].rearrange("p (a b) -> p a b", b=6), ALU.add, ["ml_gts", "pb1"], ["ml_t96"])
    C.act(ksc[:], tmp96[:].rearrange("p a b -> p (a b)"), AF.Exp, ["ml_t96", "c_lnk"], ["ml_ksc"], bias=K["lnk"][:], scale=1.0)
    C.act(qsc[:], pb[1][:, 0:96], AF.Exp, ["pb1"], ["ml_qsc"], scale=-1.0)
    C.act(egd[:], pb[1][:, 128:224], AF.Exp, ["pb1"], ["ml_eg"], scale=-1.0)
    wq = C.sb(es, "ml_wq", [128, 16, 128], BF16)
    wk = C.sb(es, "ml_wk", [128, 16, 128], BF16)
    wv = C.sb(es, "ml_wv", [128, 16, 256], BF16)
    wo = C.sb(es, "ml_wo", [128, 16, 256], BF16)
    uq = C.sb(es, "ml_uq", [128, 3 + S], F32)
    cq = C.sb(es, "ml_cq", [128, S], F32)
    qTs = [C.sb(es, f"ml_qT{i}", [128, S], BF16) for i in range(2)]
    kTs = [C.sb(es, f"ml_kT{i}", [128, S], BF16) for i in range(2)]
    ktss = [C.sb(es, f"ml_kts{i}", [128, 16, 128], BF16) for i in range(2)]
    vxs = [C.sb(es, f"ml_vx{i}", [128, 16, 264], BF16) for i in range(2)]
    ogs = [C.sb(es, f"ml_og{i}", [128, 16, 256], BF16) for i in range(2)]
    Cf = C.sb(es, "ml_Cf", [128, 264], F32)
    Cb = C.sb(es, "ml_Cb", [128, 264], BF16)
    stm = C.sb(es, "ml_stm", [128, 128], BF16)
    hm = C.sb(es, "ml_hm", [128, 256], BF16)
    sm = C.sb(es, "ml_sm", [128, 4], F32)
    hst = C.sb(es, "ml_hst", [128, 2, S], BF16)
    mask = K["tri_incl_f"]
    C.memset("pool", uq[:, 0:3], 0.0, ["ml_uq"])
    NH = 6

    def prep(h):
        p = h % 2
        qT, kT, kts, vx, og = qTs[p], kTs[p], ktss[p], vxs[p], ogs[p]
        C.dma("pool", wq[:], w_in_v[:, :, 128 * h:128 * h + 128], [], ["ml_wq"])
        C.dma("pool", wk[:], w_in_v[:, :, 768 + 128 * h:768 + 128 * h + 128], [], ["ml_wk"])
        C.dma("pool", wv[:], w_in_v[:, :, 1536 + 256 * h:1536 + 256 * h + 256], [], ["ml_wv"])
        C.dma("pool", wo[:], w_in_v[:, :, 3072 + 256 * h:3072 + 256 * h + 256], [], ["ml_wo"])
        for (w_, wtag, cidx, dst, dtag) in ((wq, "ml_wq", h, qT, f"ml_qT{p}"), (wk, "ml_wk", 6 + h, kT, f"ml_kT{p}")):
            for tg in range(4):
                b = pb[2 + tg % 2]
                bk = f"pb{2 + tg % 2}"
                for kc in range(16):
                    C.mm(b[:], w_[:, kc, :], xT[:, kc, 512 * tg:512 * tg + 512], kc == 0, kc == 15, [wtag, "xT"], [bk])
                C.cp("act", uq[:, 3 + 512 * tg:3 + 512 * tg + 512], b[:], [bk], ["ml_uq"])
                yield
            C.ts("dve", cq[:], uq[:, 0:S], cw[:, cidx, 0:1], cb[:, cidx:cidx + 1], ALU.mult, ALU.add, ["ml_uq", "ml_cw", "ml_cb"], ["ml_cq"])
            for w in range(1, 4):
                C.stt(cq[:], uq[:, w:w + S], cw[:, cidx, w:w + 1], cq[:], ALU.mult, ALU.add, ["ml_uq", "ml_cw", "ml_cq"], ["ml_cq"])
            C.act(dst[:], cq[:], AF.Silu, ["ml_cq"], [dtag])
            yield
        for tt in range(NT):
            b = pb[2 + tt % 2]
            bk = f"pb{2 + tt % 2}"
            for kc in range(16):
                C.mm(b[:, 0:256], xT[:, kc, 128 * tt:128 * tt + 128], wv[:, kc, :], kc == 0, kc == 15, ["xT", "ml_wv"], [bk])
            C.cp("dve", vx[:, tt, 0:256], b[:, 0:256], [bk], [f"ml_vx{p}"])
            yield
        C.memset("pool", vx[:, :, 256:257], 1.0, [f"ml_vx{p}"])
        for tt in range(NT):
            b = pb[2 + tt % 2]
            bk = f"pb{2 + tt % 2}"
            for kc in range(16):
                C.mm(b[:, 0:256], xT[:, kc, 128 * tt:128 * tt + 128], wo[:, kc, :], kc == 0, kc == 15, ["xT", "ml_wo"], [bk])
            C.act(og[:, tt, :], b[:, 0:256], AF.Sigmoid, [bk], [f"ml_og{p}"])
            yield
        for tt in range(NT):
            pt = K["ptr"][0]
            C.tr(pt[:, tt % 4, :], kT[:, 128 * tt:128 * tt + 128], K["ident_bf"][:], [f"ml_kT{p}", "k_ident_bf"], ["ptr0"])
            C.ts("dve", kts[:, tt, :], pt[:, tt % 4, :], ksc[:, 6 * tt + h:6 * tt + h + 1], None, ALU.mult, None, ["ptr0", "ml_ksc"], [f"ml_kts{p}"])
            if tt % 4 == 3:
                yield

    def chunks(h):
        p = h % 2
        qT, kT, kts, vx, og = qTs[p], kTs[p], ktss[p], vxs[p], ogs[p]
        C.memset("pool", Cf[:], 0.0, ["ml_Cf"])
        C.memset("pool", Cb[:], 0.0, ["ml_Cb"])
        for c in range(NT):
            sl = slice(128 * c, 128 * c + 128)
            i6 = 6 * c + h
            C.mm(pb[4][:, 0:128], kT[:, sl], qT[:, sl], True, True, [f"ml_kT{p}", f"ml_qT{p}"], ["pb4"])
            C.stt(stm[:], pb[4][:, 0:128], ksc[:, i6:i6 + 1], mask[:], ALU.mult, ALU.mult, ["pb4", "ml_ksc", "k_tri_incl_f"], ["ml_stm"])
            C.mm(pb[0][:, 0:257], kts[:, c, :], vx[:, c, 0:257], True, True, [f"ml_kts{p}", f"ml_vx{p}"], ["pb0"])
            C.mm(pb[5][:, 0:257], qT[:, sl], Cb[:, 0:257], True, False, [f"ml_qT{p}", "ml_Cb"], ["pb5"])
            C.mm(pb[5][:, 0:257], stm[:], vx[:, c, 0:257], False, True, ["ml_stm", f"ml_vx{p}"], ["pb5"])
            yield
            C.act(sm[:, 3:4], pb[5][:, 256:257], AF.Abs, ["pb5", "ml_qsc"], ["ml_sm3"], scale=qsc[:, i6:i6 + 1])
            C.ts("dve", sm[:, 0:1], sm[:, 3:4], 1.0, None, ALU.max, None, ["ml_sm3"], ["ml_sm0"])
            C.recip(sm[:, 1:2], sm[:, 0:1], ["ml_sm0"], ["ml_sm1"])
            C.tt("dve", sm[:, 2:3], sm[:, 1:2], qsc[:, i6:i6 + 1], ALU.mult, ["ml_sm1", "ml_qsc"], ["ml_sm2"])
            C.stt(hm[:], pb[5][:, 0:256], sm[:, 2:3], og[:, c, :], ALU.mult, ALU.mult, ["pb5", "ml_sm2", f"ml_og{p}"], ["ml_hm"])
            C.tt("dve", Cf[:, 0:257], pb[0][:, 0:257], Cf[:, 0:257], ALU.add, ["pb0", "ml_Cf"], ["ml_Cf"])
            C.ts("dve", Cf[:, 0:257], Cf[:, 0:257], egd[:, i6:i6 + 1], None, ALU.mult, None, ["ml_Cf", "ml_eg"], ["ml_Cf"])
            C.cp("act", Cb[:, 0:257], Cf[:, 0:257], ["ml_Cf"], ["ml_Cb"])
            pt = K["ptr"][1]
            for j in range(2):
                C.tr(pt[:, j, :], hm[:, 128 * j:128 * j + 128], K["ident_bf"][:], ["ml_hm", "k_ident_bf"], ["ptr1"])
            C.cp("act", hst[:, :, sl], pt[:, 0:2, :], ["ptr1"], ["ml_hst"])
            yield
        for j in range(2):
            C.dma("sp", hT_d[256 * h + 128 * j:256 * h + 128 * j + 128, :], hst[:, j, :], ["ml_hst"], ["hT_d"])

    for _ in prep(0):
        pass
    for h in range(NH):
        _interleave(chunks(h), prep(h + 1) if h + 1 < NH else _empty())


def mixer_moba(C, K, es, xT, hT_d, w_in_d):
    pb = K["pb"]
    w_in_v = w_in_d.rearrange("(kc p) c -> p kc c", p=128)
    wq = C.sb(es, "mo_wq", [128, 16, 128], BF16)
    wk = C.sb(es, "mo_wk", [128, 16, 128], BF16)
    wv = C.sb(es, "mo_wv", [128, 16, 128], BF16)
    zf = C.sb(es, "mo_zf", [128, 512], F32)
    qf = C.sb(es, "mo_qf", [128, S], F32)
    kf = C.sb(es, "mo_kf", [128, S], F32)
    qTs = [C.sb(es, f"mo_qT{i}", [128, S], BF16) for i in range(2)]
    kTs = [C.sb(es, f"mo_kT{i}", [128, S], BF16) for i in range(2)]
    vts = [C.sb(es, f"mo_vt{i}", [128, 16, 128], BF16) for i in range(2)]
    negTs = [C.sb(es, f"mo_negT{i}", [128, S], BF16) for i in range(2)]
    t1 = C.sb(es, "mo_t1", [32, 512], F32)
    kbar = C.sb(es, "mo_kbar", [128, 8], F32)
    gm = C.sb(es, "mo_gm", [128, 16, 8], F32)
    top8 = C.sb(es, "mo_top8", [128, 8], F32)
    negm = C.sb(es, "mo_negm", [128, 16, 8], F32)
    pT = [C.sb(es, f"mo_pT{i}", [128, 512], BF16) for i in range(2)]
    rdn = C.sb(es, "mo_rdn", [128, 512], F32)
    hst = [C.sb(es, f"mo_hst{i}", [128, 512], BF16) for i in range(2)]
    cos, sin = K["rope_cos"], K["rope_sin"]
    for i in range(2):
        C.memset("pool", negTs[i][:], 0.0, [f"mo_negT{i}"])
    scale = 128 ** -0.5
    NH = 12

    def prep(h):
        p = h % 2
        qT, kT, vt, negT = qTs[p], kTs[p], vts[p], negTs[p]
        C.dma("pool", wq[:], w_in_v[:, :, 128 * h:128 * h + 128], [], ["mo_wq"])
        C.dma("pool", wk[:], w_in_v[:, :, 1536 + 128 * h:1536 + 128 * h + 128], [], ["mo_wk"])
        C.dma("pool", wv[:], w_in_v[:, :, 3072 + 128 * h:3072 + 128 * h + 128], [], ["mo_wv"])
        for (w_, wtag, df, dftag, db, dbtag) in ((wq, "mo_wq", qf, "mo_qf", qT, f"mo_qT{p}"), (wk, "mo_wk", kf, "mo_kf", kT, f"mo_kT{p}")):
            for tg in range(4):
                ts_ = slice(512 * tg, 512 * tg + 512)
                for kc in range(16):
                    C.mm(pb[4][:], w_[:, kc, :], xT[:, kc, ts_], kc == 0, kc == 15, [wtag, "xT"], ["pb4"])
                C.cp("act", zf[:], pb[4][:], ["pb4"], ["mo_zf"])
                yield
                C.cp("pool", df[:, ts_], zf[:], ["mo_zf"], [dftag])
                C.mm(pb[5][:], K["rot_f"][:], zf[:], True, True, ["k_rot_f", "mo_zf"], ["pb5"])
                C.tt("dve", t1[:], pb[5][0:32, :], sin[:, ts_], ALU.mult, ["pb5", "k_rope_sin"], ["mo_t1"])
                C.tt("pool", df[0:32, ts_], zf[0:32, :], cos[:, ts_], ALU.mult, ["mo_zf", "k_rope_cos", dftag], [dftag])
                C.tt("dve", df[0:32, ts_], df[0:32, ts_], t1[:], ALU.add, [dftag, "mo_t1"], [dftag])
                yield
            C.cp("act", db[:], df[:], [dftag], [dbtag])
            yield
        for tt in range(NT):
            b = pb[4 + tt % 2]
            bk = f"pb{4 + tt % 2}"
            for kc in range(16):
                C.mm(b[:, 0:128], xT[:, kc, 128 * tt:128 * tt + 128], wv[:, kc, :], kc == 0, kc == 15, ["xT", "mo_wv"], [bk])
            C.cp("dve", vt[:, tt, :], b[:, 0:128], [bk], [f"mo_vt{p}"])
            if tt % 2 == 1:
                yield
        C.P.op("dve", lambda e: e.tensor_reduce(out=kbar[:], in_=kf[:].rearrange("p (n b) -> p n b", b=256), axis=AX.X, op=ALU.add),
               reads=["mo_kf"], writes=["mo_kbar"])
        C.ts("dve", kbar[:], kbar[:], 1.0 / 256.0, None, ALU.mult, None, ["mo_kbar"], ["mo_kbar"])
        yield
        for tt in range(NT):
            C.mm(pb[4][:, 8 * tt:8 * tt + 8], qf[:, 128 * tt:128 * tt + 128], kbar[:], True, True, ["mo_qf", "mo_kbar"], ["pb4"])
        C.tt("dve", gm[:], pb[4][:, 0:128].rearrange("p (a b) -> p a b", b=8), K["moba_neg"][:], ALU.add, ["pb4", "k_moba_neg"], ["mo_gm"])
        yield
        for tt in range(NT):
            C.P.op("dve", lambda e, tt=tt: e.max(out=top8[:], in_=gm[:, tt, :]), reads=["mo_gm"], writes=["mo_top8"])
            C.ts("dve", negm[:, tt, :], gm[:, tt, :], top8[:, 2:3], None, ALU.is_ge, None, ["mo_gm", "mo_top8"], ["mo_negm"])
            if tt % 2 == 1:
                yield
        C.tt("dve", negm[:], negm[:], K["moba_valid"][:], ALU.mult, ["mo_negm", "k_moba_valid"], ["mo_negm"])
        C.tt("dve", negm[:], negm[:], K["moba_valid"][:], ALU.subtract, ["mo_negm", "k_moba_valid"], ["mo_negm"])
        C.ts("dve", negm[:], negm[:], BIGM, None, ALU.mult, None, ["mo_negm"], ["mo_negm"])
        yield
        for g in range(4):
            for j in range(4):
                tt = 4 * g + j
                C.tr(pb[5][0:8, 128 * j:128 * j + 128], negm[:, tt, :], K["ident_f"][:], ["mo_negm", "k_ident_f"], ["pb5"])
            C.cp("act", negT[0:8, 512 * g:512 * g + 512], pb[5][0:8, :], ["pb5"], [f"mo_negT{p}"])
            yield

    def attn(h):
        p = h % 2
        qT, kT, vt, negT = qTs[p], kTs[p], vts[p], negTs[p]
        for g in range(4):
            qs = slice(512 * g, 512 * g + 512)
            nt_ = 4 * g + 4
            for t in range(nt_):
                sb_ = pb[2 + t % 2]
                sk = f"pb{2 + t % 2}"
                p_ = pT[t % 2]
                pk = f"mo_pT{t % 2}"
                C.mm(sb_[:], kT[:, 128 * t:128 * t + 128], qT[:, qs], True, False, [f"mo_kT{p}", f"mo_qT{p}"], [sk])
                C.mm(sb_[:], K["esel_bf"][:, t // 2, :], negT[:, qs], False, True, ["k_esel_bf", f"mo_negT{p}"], [sk])
                C.act(p_[:], sb_[:], AF.Exp, [sk], [pk], scale=scale)
                if t >= 4 * g:
                    C.tt("pool", p_[:], p_[:], K["causal_bf"][:, t - 4 * g, :], ALU.mult, [pk, "k_causal_bf"], [pk])
                C.mm(pb[0][:], vt[:, t, :], p_[:], t == 0, t == nt_ - 1, [f"mo_vt{p}", pk], ["pb0"])
                C.mm(pb[1][:], K["ones_bf"][:], p_[:], t == 0, t == nt_ - 1, ["k_ones_bf", pk], ["pb1"])
                yield
            C.recip(rdn[:], pb[1][:], ["pb1"], ["mo_rdn"])
            hs = hst[g % 2]
            C.tt("dve", hs[:], pb[0][:], rdn[:], ALU.mult, ["pb0", "mo_rdn"], [f"mo_hst{g%2}"])
            C.dma("sp", hT_d[128 * h:128 * h + 128, qs], hs[:], [f"mo_hst{g%2}"], ["hT_d"])
            yield

    for _ in prep(0):
        pass
    for h in range(NH):
        _interleave(attn(h), prep(h + 1) if h + 1 < NH else _empty())


def post_mixer(C, K, es, hT_d, xres_d, wout_d, lnp_d, wr_d, br_d, x1_d, xg_d, idx_d, gk_d):
    pb = K["pb"]
    hcT = C.sb(es, "pm_hcT", [128, 16, S], BF16)
    hv = hT_d.rearrange("(kc p) t -> p kc t", p=128)
    for q4 in range(4):
        C.dma("sp", hcT[:, 4 * q4:4 * q4 + 4, :], hv[:, 4 * q4:4 * q4 + 4, :], ["hT_d"], ["pm_hcT"])
    wout = C.sb(es, "pm_wout", [128, 16, D], BF16)
    wv_ = wout_d.rearrange("(kc p) c -> p kc c", p=128)
    for cg in range(4):
        C.dma("pool", wout[:, :, 512 * cg:512 * cg + 512], wv_[:, :, 512 * cg:512 * cg + 512], [], ["pm_wout"])
    gbc = C.sb(es, "pm_g", [128, D], F32)
    bbc = C.sb(es, "pm_b", [128, D], F32)
    C.dma("sp", gbc[:], lnp_d[0], [], ["lnp"])
    C.dma("sp", bbc[:], lnp_d[1], [], ["lnp"])
    wr = C.sb(es, "pm_wr", [128, 16, NE], F32)
    C.dma("sp", wr[:], wr_d.rearrange("(kc p) e -> p kc e", p=128), [], ["pm_wr"])
    br = C.sb(es, "pm_br", [128, NE], F32)
    C.dma("sp", br[:], br_d, [], ["pm_br"])
    x1s = [C.sb(es, f"pm_x1{i}", [128, D], F32) for i in range(2)]
    x1bs = [C.sb(es, f"pm_x1b{i}", [128, D], BF16) for i in range(2)]
    x1T = C.sb(es, "pm_x1T", [128, 16, 128], F32)
    lg = C.sb(es, "pm_lg", [128, NE], F32)
    top8 = C.sb(es, "pm_top8", [128, 8], F32)
    sel = C.sb(es, "pm_sel", [128, NE], F32)
    selb = C.sb(es, "pm_selb", [128, NE], BF16)
    ex = C.sb(es, "pm_ex", [128, NE], F32)
    sm = C.sb(es, "pm_sm", [128, 4], F32)
    gmv = C.sb(es, "pm_gm", [128, NE], F32)
    cnt = C.sb(es, "pm_cnt", [128, NE], F32)
    pos = C.sb(es, "pm_pos", [128, NE], F32)
    rix = C.sb(es, "pm_rix", [128, NE], F32)
    val = C.sb(es, "pm_val", [128, NE], F32)
    junk = C.sb(es, "pm_junk", [128, NE], F32)
    rks = [C.sb(es, f"pm_rk{i}", [128, 4], F32) for i in range(2)]
    gks = [C.sb(es, f"pm_gk{i}", [128, 4], F32) for i in range(2)]
    ris = [C.sb(es, f"pm_ri{i}", [128, 4], I32) for i in range(2)]
    C.memset("pool", cnt[:], 0.0, ["pm_cnt"])

    def stage_a(tt):
        p = tt % 2
        x1, x1b = x1s[p], x1bs[p]
        xk, xbk = f"pm_x1{p}", f"pm_x1b{p}"
        rows = slice(128 * tt, 128 * tt + 128)
        C.dma("sp", x1[:], xres_d[rows, :], [], [xk])
        for cg in range(4):
            for kc in range(16):
                C.mm(pb[cg][:], hcT[:, kc, rows], wout[:, kc, 512 * cg:512 * cg + 512], kc == 0, kc == 15, ["pm_hcT", "pm_wout"], [f"pb{cg}"])
            C.stt(x1[:, 512 * cg:512 * cg + 512], x1[:, 512 * cg:512 * cg + 512], ALPHA, pb[cg][:], ALU.mult, ALU.add, [xk, f"pb{cg}"], [xk])
            yield
        layer_norm_tile(C, K, x1[:], gbc[:], bbc[:], x1[:], xk, xk)
        yield
        C.dma("sp", x1_d[rows, :], x1[:], [xk], ["x1_d"])
        C.cp("act", x1b[:], x1[:], [xk], [xbk])
        yield

    def stage_b(tt):
        p = tt % 2
        x1, x1b = x1s[p], x1bs[p]
        xk, xbk = f"pm_x1{p}", f"pm_x1b{p}"
        rk, gk, ri = rks[p], gks[p], ris[p]
        rows = slice(128 * tt, 128 * tt + 128)
        for kc in range(16):
            b = pb[4 + kc % 2]
            bk = f"pb{4 + kc % 2}"
            C.tr(b[:, 0:128], x1[:, 128 * kc:128 * kc + 128], K["ident_f"][:], [xk, "k_ident_f"], [bk])
            C.cp("act" if kc % 2 else "dve", x1T[:, kc, :], b[:, 0:128], [bk], [f"pm_x1T{kc}"])
            if kc % 4 == 3:
                yield
        for kc in range(16):
            C.mm(pb[4][:, 256:256 + NE], x1T[:, kc, :], wr[:, kc, :], kc == 0, kc == 15, [f"pm_x1T{kc}", "pm_wr"], ["pb4"])
        C.tt("dve", lg[:], pb[4][:, 256:256 + NE], br[:], ALU.add, ["pb4", "pm_br"], ["pm_lg"])
        yield
        C.P.op("dve", lambda e: e.max(out=top8[:], in_=lg[:]), reads=["pm_lg"], writes=["pm_top8"])
        C.ts("dve", sel[:], lg[:], top8[:, 3:4], None, ALU.is_ge, None, ["pm_lg", "pm_top8"], ["pm_sel"])
        C.cp("pool", selb[:], sel[:], ["pm_sel"], ["pm_selb"])
        C.ts("dve", sm[:, 0:1], top8[:, 0:1], -1.0, None, ALU.mult, None, ["pm_top8"], ["pm_sm0"])
        C.act(ex[:], lg[:], AF.Exp, ["pm_lg", "pm_sm0"], ["pm_ex"], bias=sm[:, 0:1], scale=1.0)
        yield
        C.stt(ex[:], ex[:], 1.0, sel[:], ALU.mult, ALU.mult, ["pm_ex", "pm_sel"], ["pm_ex"], accum_out=sm[:, 1:2])
        C.recip(sm[:, 2:3], sm[:, 1:2], ["pm_ex"], ["pm_sm2"])
        C.ts("dve", gmv[:], ex[:], sm[:, 2:3], None, ALU.mult, None, ["pm_ex", "pm_sm2"], ["pm_gm"])
        C.mm(pb[5][:, 256:256 + NE], K["tri_strict_bf"][:], selb[:], True, True, ["k_tri_strict_bf", "pm_selb"], ["pb5"])
        C.mm(pb[5][:, 320:320 + NE], K["ones_bf"][:], selb[:], True, True, ["k_ones_bf", "pm_selb"], ["pb5"])
        yield
        C.tt("dve", pos[:], pb[5][:, 256:256 + NE], cnt[:], ALU.add, ["pb5", "pm_cnt"], ["pm_pos"])
        C.tt("dve", cnt[:], pb[5][:, 320:320 + NE], cnt[:], ALU.add, ["pb5", "pm_cnt"], ["pm_cnt"])
        C.ts("dve", val[:], pos[:], CAP - 0.5, None, ALU.is_lt, None, ["pm_pos"], ["pm_val"])
        C.tt("dve", val[:], val[:], sel[:], ALU.mult, ["pm_val", "pm_sel"], ["pm_val"])
        C.tt("dve", rix[:], pos[:], K["ebase"][:], ALU.add, ["pm_pos", "k_ebase"], ["pm_rix"])
        yield
        C.ts("dve", rix[:], rix[:], -float(NROWS), None, ALU.add, None, ["pm_rix"], ["pm_rix"])
        C.tt("dve", rix[:], rix[:], val[:], ALU.mult, ["pm_rix", "pm_val"], ["pm_rix"])
        C.ts("dve", rix[:], rix[:], float(NROWS), None, ALU.add, None, ["pm_rix"], ["pm_rix"])
        yield
        for k in range(4):
            C.stt(junk[:], lg[:], top8[:, k:k + 1], rix[:], ALU.is_equal, ALU.mult, ["pm_lg", "pm_top8", "pm_rix"], ["pm_junk"], accum_out=rk[:, k:k + 1])
            C.stt(junk[:], lg[:], top8[:, k:k + 1], gmv[:], ALU.is_equal, ALU.mult, ["pm_lg", "pm_top8", "pm_gm"], ["pm_junk"], accum_out=gk[:, k:k + 1])
            if k % 2 == 1:
                yield
        C.cp("dve", ri[:], rk[:], ["pm_junk"], [f"pm_ri{p}"])
        C.dma("sp", idx_d[rows, :], ri[:], [f"pm_ri{p}"], ["idx_d"])
        C.dma("sp", gk_d[rows, :], gk[:], ["pm_junk"], ["gk_d"])
        yield
        for k in range(4):
            C.P.op("pool", lambda e, k=k, ri=ri, x1b=x1b: e.indirect_dma_start(
                out=xg_d[:, :], out_offset=bass.IndirectOffsetOnAxis(ap=ri[:, k:k + 1], axis=0),
                in_=x1b[:, :], in_offset=None, bounds_check=C.P.bc_reg, oob_is_err=False),
                reads=[f"pm_ri{p}", xbk], writes=["xg_d"], dma=True)
        yield

    for _ in stage_a(0):
        pass
    for tt in range(NT):
        _interleave(stage_b(tt), stage_a(tt + 1) if tt + 1 < NT else _empty())


def combine_ln2(C, K, es, x1_d, y_d, idx_d, gk_d, lnp_d, out_d, xT=None):
    gbc = C.sb(es, "cb_g", [128, D], F32)
    bbc = C.sb(es, "cb_b", [128, D], F32)
    C.dma("sp", gbc[:], lnp_d[0], [], ["lnp"])
    C.dma("sp", bbc[:], lnp_d[1], [], ["lnp"])
    accs = [C.sb(es, f"cb_acc{i}", [128, D], F32) for i in range(2)]
    yks = [[C.sb(es, f"cb_yk{i}_{k}", [128, D], F32) for k in range(4)] for i in range(2)]
    x2 = C.sb(es, "cb_x2", [128, D], F32)
    x2b = C.sb(es, "cb_x2b", [128, D], BF16)
    ris = [C.sb(es, f"cb_ri{i}", [128, 4], I32) for i in range(2)]
    gks = [C.sb(es, f"cb_gk{i}", [128, 4], F32) for i in range(2)]
    outs = []
    for tt in range(NT):
        p = tt % 2
        acc, yk, ri, gk = accs[p], yks[p], ris[p], gks[p]
        rows = slice(128 * tt, 128 * tt + 128)
        C.dma("sp", acc[:], x1_d[rows, :], ["x1_d"], [f"cb_acc{p}"])
        C.dma("sp", ri[:], idx_d[rows, :], ["idx_d"], [f"cb_ri{p}"])
        C.dma("sp", gk[:], gk_d[rows, :], ["gk_d"], [f"cb_gk{p}"])
        for k in range(4):
            C.memset("pool", yk[k][:], 0.0, [f"cb_yk{p}_{k}"])
            C.P.op("pool", lambda e, k=k, yk=yk, ri=ri: e.indirect_dma_start(
                out=yk[k][:, :], out_offset=None, in_=y_d[:, :],
                in_offset=bass.IndirectOffsetOnAxis(ap=ri[:, k:k + 1], axis=0),
                bounds_check=C.P.bc_reg, oob_is_err=False),
                reads=[f"cb_ri{p}", "y_d"], writes=[f"cb_yk{p}_{k}"], dma=True)
        C.ts("dve", acc[:], acc[:], ALPHA, None, ALU.mult, None, [f"cb_acc{p}"], [f"cb_acc{p}"])
        for k in range(4):
            C.stt(acc[:], yk[k][:], gk[:, k:k + 1], acc[:], ALU.mult, ALU.add, [f"cb_yk{p}_{k}", f"cb_gk{p}", f"cb_acc{p}"], [f"cb_acc{p}"])
        layer_norm_tile(C, K, acc[:], gbc[:], bbc[:], x2[:], f"cb_acc{p}", "cb_x2")
        outs.append(C.dma("sp", out_d[rows, :], x2[:], ["cb_x2"], ["x2_d"]))
        if xT is not None:
            C.cp("act", x2b[:], x2[:], ["cb_x2"], ["cb_x2b"])
            emit_xT(C, K, x2b, tt, xT, "cb_x2b")
    return outs


def _build_dp(kind):
    nc = bass.Bass("TRN2", target_bir_lowering=False)
    C = Ctx(nc)
    fin = []
    with C.es:
        es0 = C.root
        names = ["ident_bf", "ident_f", "ones_bf", "ones_f", "tri_incl_f", "tri_strict_bf", "ebase"]
        if kind == "k3":
            names += ["causal_bf", "rope_cos", "rope_sin", "rot_f", "moba_valid", "moba_neg", "esel_bf"]
        K = load_consts(C, es0, names)
        npb = 8 if kind == "k3" else 8
        K["ptr"] = [C.ps(f"ptr{i}", [128, 4, 128], BF16) for i in range(2)]
        K["pb"] = [C.ps(f"pb{i}", [128, 512], F32) for i in range(6)]
        for nm, shp in (("ln_st", [128, 24]), ("ln_mv", [128, 2]), ("ln_sd", [128, 1]), ("eps", [128, 1]), ("one", [128, 1]), ("lnk", [128, 1])):
            K[nm] = C.sb(es0, "s_" + nm, shp, F32)
        C.memset("pool", K["eps"][:], EPS, ["c_eps"])
        C.memset("pool", K["one"][:], 1.0, ["c_one"])
        C.memset("pool", K["lnk"][:], float(-0.5 * np.log(128.0)), ["c_lnk"])
        di = lambda n, s, dt=F32: C.dram(n, s, dt, "ExternalInput")
        do = lambda n, s, dt=F32: C.dram(n, s, dt, "ExternalOutput")
        if kind in ("k1", "k3"):
            hT_d = C.dram("hT_d", [D, S], BF16, "Internal")
            x1_d = do("x1_o", [S, D])
            xg_d = do("xg_o", [NROWS, D], BF16)
            idx_d = do("idx_o", [S, 4], I32)
            gk_d = do("gk_o", [S, 4])
            mem_d = di("mem", [256, D])
            wkv_d = di("w_mem_kv", [D, 1024])
            wout_d = di("w_out", [D, D])
            ln1_d = di("ln1", [2, 128, D])
            wr_d = di("w_router", [D, NE])
            br_d = di("b_router", [128, NE])
        if kind == "k1":
            x_d = di("x", [S, D])
            w_in_d = di("w_in", [D, 5132])
            convw_d = di("convw", [128, 12, 4])
            convb_d = di("convb", [128, 12])
            gbias_d = di("gbias", [128, 12])
            with C.scope(es0) as es:
                xT = C.sb(es, "xT", [128, 16, S], BF16)
                xb = [C.sb(es, f"xb{i}", [128, D], BF16) for i in range(2)]
                for tt in range(NT):
                    C.dma("pool", xb[tt % 2][:], x_d[128 * tt:128 * tt + 128, :], [], [f"xb{tt%2}"])
                    emit_xT(C, K, xb[tt % 2], tt, xT, f"xb{tt%2}")
                with C.scope(es) as es2:
                    mem_attention(C, K, es2, xT, hT_d, mem_d, wkv_d, w_in_d, 4620)
                C.P.barrier()
                with C.scope(es) as es2:
                    mixer_mlstm(C, K, es2, xT, hT_d, w_in_d, convw_d, convb_d, gbias_d)
                C.P.barrier()
            with C.scope(es0) as es:
                post_mixer(C, K, es, hT_d, x_d, wout_d, ln1_d, wr_d, br_d, x1_d, xg_d, idx_d, gk_d)
            fin = list(C.P.dma_last.values())
        if kind in ("k3", "k5"):
            x1i_d = di("x1_i", [S, D])
            y_d = di("y_i", [NROWS, D])
            idxi_d = di("idx_i", [S, 4], I32)
            gki_d = di("gk_i", [S, 4])
            ln2_d = di("ln2", [2, 128, D])
        if kind == "k5":
            out_d = do("out", [S, D])
            with C.scope(es0) as es:
                combine_ln2(C, K, es, x1i_d, y_d, idxi_d, gki_d, ln2_d, out_d)
            fin = list(C.P.dma_last.values())
        if kind == "k3":
            x2_d = C.dram("x2_d", [S, D], F32, "Internal")
            w_in_d = di("w_in", [D, 5120])
            with C.scope(es0) as es:
                xT = C.sb(es, "xT", [128, 16, S], BF16)
                with C.scope(es) as es2:
                    combine_ln2(C, K, es2, x1i_d, y_d, idxi_d, gki_d, ln2_d, x2_d, xT=xT)
                C.P.barrier()
                with C.scope(es) as es2:
                    mem_attention(C, K, es2, xT, hT_d, mem_d, wkv_d, w_in_d, 4608)
                C.P.barrier()
                with C.scope(es) as es2:
                    mixer_moba(C, K, es2, xT, hT_d, w_in_d)
                C.P.barrier()
            with C.scope(es0) as es:
                post_mixer(C, K, es, hT_d, x2_d, wout_d, ln1_d, wr_d, br_d, x1_d, xg_d, idx_d, gk_d)
            fin = list(C.P.dma_last.values())
        C.P.emit(final_wait_ops=fin)
    return nc


def build_ffn():
    nc = bass.Bass("TRN2", target_bir_lowering=False)
    C = Ctx(nc)
    NSL = NCORES * CAP
    GS = 1024
    NG = NSL // GS
    with C.es:
        es = C.root
        K = load_consts(C, es, ["ident_bf"])
        K["ptr"] = [C.ps(f"ptr{i}", [128, 4, 128], BF16) for i in range(2)]
        K["pb"] = [C.ps(f"pb{i}", [128, 512], F32) for i in range(6)]
        pb = K["pb"]
        xg_d = C.dram("xg_i", [4 * NSL, D], BF16, "ExternalInput")
        wgu_d = C.dram("w_gu", [4, D, 2 * D], F32, "ExternalInput")
        bgu_d = C.dram("b_gu", [4, 128, 32], F32, "ExternalInput")
        wdn_d = C.dram("w_down", [4, D, D], F32, "ExternalInput")
        bdn_d = C.dram("b_down", [4, 128, D], F32, "ExternalInput")
        y_d = C.dram("y_o", [4 * NSL, D], F32, "ExternalOutput")
        wt = [C.sb(es, f"wt{i}", [128, 16, 512], BF16) for i in range(4)]
        xgt = [C.sb(es, f"xgt{i}", [128, D], BF16) for i in range(2)]
        xgT = C.sb(es, "xgT", [128, 16, GS], BF16)
        hidT = C.sb(es, "hidT", [128, 16, GS], BF16)
        bgu = C.sb(es, "bgu", [128, 32], F32)
        bdn = C.sb(es, "bdn", [128, D], F32)
        hgc = C.sb(es, "hgc", [128, 512], F32)
        sg = C.sb(es, "sg", [128, 512], F32)
        huc = C.sb(es, "huc", [128, 512], F32)
        yst = [C.sb(es, f"yst{i}", [128, D], F32) for i in range(2)]
        wslot = 0
        it = 0
        for e in range(4):
            wgv = wgu_d[e].rearrange("(kc p) c -> p kc c", p=128)
            wdv = wdn_d[e].rearrange("(kc p) c -> p kc c", p=128)
            C.dma("sp", bgu[:], bgu_d[e], [], ["bgu"])
            C.dma("sp", bdn[:], bdn_d[e], [], ["bdn"])
            for gi in range(NG):
                r0 = e * NSL + gi * GS
                for st in range(GS // 128):
                    xt = xgt[st % 2]
                    C.dma("sp", xt[:], xg_d[r0 + 128 * st:r0 + 128 * st + 128, :], [], [f"xgt{st%2}"])
                    for g in range(4):
                        pt = K["ptr"][g % 2]
                        for j in range(4):
                            kc = 4 * g + j
                            C.tr(pt[:, j, :], xt[:, 128 * kc:128 * kc + 128], K["ident_bf"][:], [f"xgt{st%2}", "k_ident_bf"], [f"ptr{g%2}"])
                        C.cp("dve" if g % 2 == 0 else "act", xgT[:, 4 * g:4 * g + 4, 128 * st:128 * st + 128], pt[:], [f"ptr{g%2}"], ["xgT"])
                for j in range(4):
                    wg_ = wt[wslot % 4]; gk_ = f"wt{wslot % 4}"; wslot += 1
                    wu_ = wt[wslot % 4]; uk_ = f"wt{wslot % 4}"; wslot += 1
                    C.dma("pool", wg_[:], wgv[:, :, 512 * j:512 * j + 512], [], [gk_])
                    C.dma("pool", wu_[:], wgv[:, :, D + 512 * j:D + 512 * j + 512], [], [uk_])
                    for i in range(4):
                        ffc = 4 * j + i
                        for n in range(GS // 512):
                            ns = slice(512 * n, 512 * n + 512)
                            for kc in range(16):
                                C.mm(pb[0][:], wg_[:, kc, 128 * i:128 * i + 128], xgT[:, kc, ns], kc == 0, kc == 15, [gk_, "xgT"], ["pb0"])
                            for kc in range(16):
                                C.mm(pb[1][:], wu_[:, kc, 128 * i:128 * i + 128], xgT[:, kc, ns], kc == 0, kc == 15, [uk_, "xgT"], ["pb1"])
                            C.ts("dve", hgc[:], pb[0][:], bgu[:, ffc:ffc + 1], 7.0, ALU.add, ALU.min, ["pb0", "bgu"], ["hgc"])
                            C.act(sg[:], hgc[:], AF.Sigmoid, ["hgc"], ["sg"], scale=1.702)
                            C.ts("dve", huc[:], pb[1][:], bgu[:, 16 + ffc:16 + ffc + 1], 7.0, ALU.add, ALU.min, ["pb1", "bgu"], ["huc"])
                            C.ts("dve", huc[:], huc[:], -7.0, 1.0, ALU.max, ALU.add, ["huc"], ["huc"])
                            C.tt("dve", sg[:], sg[:], hgc[:], ALU.mult, ["sg", "hgc"], ["sg"])
                            C.tt("dve", hidT[:, ffc, ns], sg[:], huc[:], ALU.mult, ["sg", "huc"], ["hidT"])
                wd = []
                for dg in range(4):
                    w_ = wt[wslot % 4]; k_ = f"wt{wslot % 4}"; wslot += 1
                    C.dma("pool", w_[:], wdv[:, :, 512 * dg:512 * dg + 512], [], [k_])
                    wd.append((w_, k_))
                for st in range(GS // 128):
                    ys = yst[it % 2]; yk_ = f"yst{it % 2}"; it += 1
                    for dg in range(4):
                        w_, k_ = wd[dg]
                        for kc in range(16):
                            C.mm(pb[2 + dg][:], hidT[:, kc, 128 * st:128 * st + 128], w_[:, kc, :], kc == 0, kc == 15, ["hidT", k_], [f"pb{2+dg}"])
                        C.tt("dve", ys[:, 512 * dg:512 * dg + 512], pb[2 + dg][:], bdn[:, 512 * dg:512 * dg + 512], ALU.add, [f"pb{2+dg}", "bdn"], [yk_])
                    C.dma("sp", y_d[r0 + 128 * st:r0 + 128 * st + 128, :], ys[:], [yk_], ["y_d"])
        C.P.emit(final_wait_ops=list(C.P.dma_last.values()))
    return nc


def ffn_local(C, K, es, xg_d, y_d, wgu_d, bgu_d, wdn_d, bdn_d):
    pb = K["pb"]
    NW = 6
    wt = [C.sb(es, f"wt{i}", [128, 16, 512], BF16) for i in range(NW)]
    xgt = [C.sb(es, f"xgt{i}", [128, D], BF16) for i in range(2)]
    xgT = [C.sb(es, f"xgT{i}", [128, 16, CAP], BF16) for i in range(2)]
    hidT = C.sb(es, "hidT", [128, 16, CAP], BF16)
    bgu = [C.sb(es, f"bgu{i}", [128, 32], F32) for i in range(2)]
    bdn = [C.sb(es, f"bdn{i}", [128, D], F32) for i in range(2)]
    hgc = C.sb(es, "hgc", [128, CAP], F32)
    sg = C.sb(es, "sg", [128, CAP], F32)
    huc = C.sb(es, "huc", [128, CAP], F32)
    yst = [C.sb(es, f"yst{i}", [128, D], F32) for i in range(2)]
    wslot = 0
    it = 0
    xi = 0
    for e in range(NE):
        wgv = wgu_d[e].rearrange("(kc p) c -> p kc c", p=128)
        wdv = wdn_d[e].rearrange("(kc p) c -> p kc c", p=128)
        bg = bgu[e % 2]; bgk = f"bgu{e % 2}"
        bd = bdn[e % 2]; bdk = f"bdn{e % 2}"
        C.dma("sp", bg[:], bgu_d[e], [], [bgk])
        C.dma("sp", bd[:], bdn_d[e].partition_broadcast(128), [], [bdk])
        xT_ = xgT[e % 2]; xk = f"xgT{e % 2}"
        r0 = e * CAP
        for st in range(CAP // 128):
            xt = xgt[xi % 2]; xtk = f"xgt{xi % 2}"; xi += 1
            C.dma("sp", xt[:], xg_d[r0 + 128 * st:r0 + 128 * st + 128, :], [], [xtk])
            for g in range(4):
                pt = K["ptr"][g % 2]
                for j in range(4):
                    kc = 4 * g + j
                    C.tr(pt[:, j, :], xt[:, 128 * kc:128 * kc + 128], K["ident_bf"][:], [xtk, "k_ident_bf"], [f"ptr{g%2}"])
                C.cp("dve" if g % 2 == 0 else "act", xT_[:, 4 * g:4 * g + 4, 128 * st:128 * st + 128], pt[:], [f"ptr{g%2}"], [xk])
        for j in range(4):
            wg_ = wt[wslot % NW]; gk_ = f"wt{wslot % NW}"; wslot += 1
            wu_ = wt[wslot % NW]; uk_ = f"wt{wslot % NW}"; wslot += 1
            C.dma("pool", wg_[:], wgv[:, :, 512 * j:512 * j + 512], [], [gk_])
            C.dma("pool", wu_[:], wgv[:, :, D + 512 * j:D + 512 * j + 512], [], [uk_])
            for i in range(4):
                ffc = 4 * j + i
                bA, bB = (0, 1) if ffc % 2 == 0 else (4, 5)
                for kc in range(16):
                    C.mm(pb[bA][:, 0:CAP], wg_[:, kc, 128 * i:128 * i + 128], xT_[:, kc, :], kc == 0, kc == 15, [gk_, xk], [f"pb{bA}"])
                for kc in range(16):
                    C.mm(pb[bB][:, 0:CAP], wu_[:, kc, 128 * i:128 * i + 128], xT_[:, kc, :], kc == 0, kc == 15, [uk_, xk], [f"pb{bB}"])
                C.ts("dve", hgc[:], pb[bA][:, 0:CAP], bg[:, ffc:ffc + 1], 7.0, ALU.add, ALU.min, [f"pb{bA}", bgk], ["hgc"])
                C.act(sg[:], hgc[:], AF.Sigmoid, ["hgc"], ["sg"], scale=1.702)
                C.ts("dve", huc[:], pb[bB][:, 0:CAP], bg[:, 16 + ffc:16 + ffc + 1], 7.0, ALU.add, ALU.min, [f"pb{bB}", bgk], ["huc"])
                C.ts("dve", huc[:], huc[:], -7.0, 1.0, ALU.max, ALU.add, ["huc"], ["huc"])
                C.tt("dve", sg[:], sg[:], hgc[:], ALU.mult, ["sg", "hgc"], ["sg"])
                C.tt("dve", hidT[:, ffc, :], sg[:], huc[:], ALU.mult, ["sg", "huc"], ["hidT"])
        wd = []
        for dg in range(4):
            w_ = wt[wslot % NW]; k_ = f"wt{wslot % NW}"; wslot += 1
            C.dma("pool", w_[:], wdv[:, :, 512 * dg:512 * dg + 512], [], [k_])
            wd.append((w_, k_))
        for st in range(CAP // 128):
            ys = yst[it % 2]; yk_ = f"yst{it % 2}"; it += 1
            for dg in range(4):
                w_, k_ = wd[dg]
                for kc in range(16):
                    C.mm(pb[2 + dg][:], hidT[:, kc, 128 * st:128 * st + 128], w_[:, kc, :], kc == 0, kc == 15, ["hidT", k_], [f"pb{2+dg}"])
                C.tt("dve", ys[:, 512 * dg:512 * dg + 512], pb[2 + dg][:], bd[:, 512 * dg:512 * dg + 512], ALU.add, [f"pb{2+dg}", bdk], [yk_])
            C.dma("sp", y_d[r0 + 128 * st:r0 + 128 * st + 128, :], ys[:], [yk_], ["y_d"])


def build_fused():
    nc = bass.Bass("TRN2", target_bir_lowering=False)
    C = Ctx(nc)
    with C.es:
        es0 = C.root
        names = ["ident_bf", "ident_f", "ones_bf", "ones_f", "tri_incl_f", "tri_strict_bf", "ebase",
                 "causal_bf", "rope_cos", "rope_sin", "rot_f", "moba_valid", "moba_neg", "esel_bf"]
        K = load_consts(C, es0, names)
        K["ptr"] = [C.ps(f"ptr{i}", [128, 4, 128], BF16) for i in range(2)]
        K["pb"] = [C.ps(f"pb{i}", [128, 512], F32) for i in range(6)]
        for nm, shp in (("ln_st", [128, 24]), ("ln_mv", [128, 2]), ("ln_sd", [128, 1]), ("eps", [128, 1]), ("one", [128, 1]), ("lnk", [128, 1])):
            K[nm] = C.sb(es0, "s_" + nm, shp, F32)
        C.memset("pool", K["eps"][:], EPS, ["c_eps"])
        C.memset("pool", K["one"][:], 1.0, ["c_one"])
        C.memset("pool", K["lnk"][:], float(-0.5 * np.log(128.0)), ["c_lnk"])
        di = lambda n, s, dt=F32: C.dram(n, s, dt, "ExternalInput")
        dn = lambda n, s, dt=F32: C.dram(n, s, dt, "Internal")
        x_d = di("x", [S, D]); mem_d = di("mem", [256, D])
        w_in0 = di("w_in0", [D, 5132]); w_in1 = di("w_in1", [D, 5120])
        convw_d = di("convw", [128, 12, 4]); convb_d = di("convb", [128, 12]); gbias_d = di("gbias", [128, 12])
        wkv_d = di("w_mem_kv", [2, D, 1024]); wout_d = di("w_out", [2, D, D])
        ln1_d = di("ln1", [2, 2, 128, D]); ln2_d = di("ln2", [2, 2, 128, D])
        wr_d = di("w_router", [2, D, NE]); br_d = di("b_router", [2, 128, NE])
        wgu_d = di("w_gu", [2, NE, D, 2 * D]); bgu_d = di("b_gu", [2, NE, 128, 32])
        wdn_d = di("w_down", [2, NE, D, D]); bdn_d = di("b_down", [2, NE, D])
        out_d = C.dram("out", [S, D], F32, "ExternalOutput")
        hT_d = dn("hT_d", [D, S], BF16)
        x1_d = dn("x1_d", [S, D]); x2_d = dn("x2_d", [S, D])
        xg_d = dn("xg_d", [NROWS, D], BF16); y_d = dn("y_d", [NROWS, D])
        idx_d = dn("idx_d", [S, 4], I32); gk_d = dn("gk_d", [S, 4])
        with C.scope(es0) as es:
            xT = C.sb(es, "xT", [128, 16, S], BF16)
            with C.scope(es) as es2:
                xb = [C.sb(es2, f"xb{i}", [128, D], BF16) for i in range(2)]
                for tt in range(NT):
                    C.dma("pool", xb[tt % 2][:], x_d[128 * tt:128 * tt + 128, :], [], [f"xb{tt%2}"])
                    emit_xT(C, K, xb[tt % 2], tt, xT, f"xb{tt%2}")
            C.P.barrier()
            with C.scope(es) as es2:
                mem_attention(C, K, es2, xT, hT_d, mem_d, wkv_d[0], w_in0, 4620)
            C.P.barrier()
            with C.scope(es) as es2:
                mixer_mlstm(C, K, es2, xT, hT_d, w_in0, convw_d, convb_d, gbias_d)
            C.P.barrier()
        with C.scope(es0) as es:
            post_mixer(C, K, es, hT_d, x_d, wout_d[0], ln1_d[0], wr_d[0], br_d[0], x1_d, xg_d, idx_d, gk_d)
        C.P.barrier()
        with C.scope(es0) as es:
            ffn_local(C, K, es, xg_d, y_d, wgu_d[0], bgu_d[0], wdn_d[0], bdn_d[0])
        C.P.barrier()
        with C.scope(es0) as es:
            xT = C.sb(es, "xT1", [128, 16, S], BF16)
            with C.scope(es) as es2:
                combine_ln2(C, K, es2, x1_d, y_d, idx_d, gk_d, ln2_d[0], x2_d, xT=xT)
            C.P.barrier()
            with C.scope(es) as es2:
                mem_attention(C, K, es2, xT, hT_d, mem_d, wkv_d[1], w_in1, 4608)
            C.P.barrier()
            with C.scope(es) as es2:
                mixer_moba(C, K, es2, xT, hT_d, w_in1)
            C.P.barrier()
        with C.scope(es0) as es:
            post_mixer(C, K, es, hT_d, x2_d, wout_d[1], ln1_d[1], wr_d[1], br_d[1], x1_d, xg_d, idx_d, gk_d)
        C.P.barrier()
        with C.scope(es0) as es:
            ffn_local(C, K, es, xg_d, y_d, wgu_d[1], bgu_d[1], wdn_d[1], bdn_d[1])
        C.P.barrier()
        with C.scope(es0) as es:
            outs = combine_ln2(C, K, es, x1_d, y_d, idx_d, gk_d, ln2_d[1], out_d)
        C.P.emit(final_wait_ops=list(C.P.dma_last.values()))
    return nc


_PROG_CACHE = {}


def _prog(kind):
    if kind not in _PROG_CACHE:
        _PROG_CACHE[kind] = build_ffn() if kind == "ffn" else (build_fused() if kind == "fused" else _build_dp(kind))
    return _PROG_CACHE[kind]


def _bc(v, n=128):
    return np.ascontiguousarray(np.broadcast_to(np.asarray(v, np.float32)[None], (n,) + tuple(np.shape(v))))


def _run(kind, in_maps):
    nc = _prog(kind)
    res = run_bass_kernel_spmd(nc, in_maps, core_ids=list(range(len(in_maps))))
    return res.results


def _fused_maps(cores, x, mem, mlstm_w_in, mlstm_conv_w, mlstm_conv_b, mlstm_b_igate, mlstm_b_fgate,
                moba_w_in, w_mem_kv, w_out, ln1_g, ln1_b, w_router, b_router, w_gu, b_gu,
                w_down, b_down, ln2_g, ln2_b):
    f32 = np.float32
    cst = host_consts()
    A = lambda a: np.ascontiguousarray(np.asarray(a, f32))
    shared = {
        "w_in0": A(mlstm_w_in[0]), "w_in1": A(moba_w_in[0]),
        "convw": np.ascontiguousarray(A(mlstm_conv_w[0]).reshape(4, 12, 128).transpose(2, 1, 0)),
        "convb": np.ascontiguousarray(A(mlstm_conv_b[0]).reshape(12, 128).T),
        "gbias": _bc(np.concatenate([A(mlstm_b_igate[0]), A(mlstm_b_fgate[0])])),
        "w_mem_kv": A(w_mem_kv), "w_out": A(w_out),
        "ln1": np.stack([np.stack([_bc(ln1_g[i]), _bc(ln1_b[i])], 0) for i in range(2)], 0),
        "ln2": np.stack([np.stack([_bc(ln2_g[i]), _bc(ln2_b[i])], 0) for i in range(2)], 0),
        "w_router": A(w_router), "b_router": np.stack([_bc(b_router[i]) for i in range(2)], 0),
        "w_gu": A(w_gu),
        "b_gu": np.ascontiguousarray(A(b_gu).reshape(2, NE, 32, 128).transpose(0, 1, 3, 2)),
        "w_down": A(w_down), "b_down": A(b_down),
    }
    for n in CONST_SPECS:
        shared["c_" + n] = cst[n]
    maps = []
    for c in cores:
        m = dict(shared)
        m["x"] = A(x[c]); m["mem"] = A(mem[c])
        maps.append(m)
    return maps


def kernel(**inputs):
    maps = _fused_maps(list(range(NCORES)), **inputs)
    nc = _prog("fused")
    res = run_bass_kernel_spmd(nc, maps, core_ids=list(range(NCORES)))
    return np.stack([r["out"] for r in res.results], 0).astype(np.float32)


def kernel_unfused(x, mem, mlstm_w_in, mlstm_conv_w, mlstm_conv_b, mlstm_b_igate, mlstm_b_fgate,
           moba_w_in, w_mem_kv, w_out, ln1_g, ln1_b, w_router, b_router, w_gu, b_gu,
           w_down, b_down, ln2_g, ln2_b):
    f32 = np.float32
    cst = host_consts()

    def consts_for(names):
        return {"c_" + n: cst[n] for n in names}

    base = ["ident_bf", "ident_f", "ones_bf", "ones_f", "tri_incl_f", "tri_strict_bf", "ebase"]
    k3n = base + ["causal_bf", "rope_cos", "rope_sin", "rot_f", "moba_valid", "moba_neg", "esel_bf"]

    def lnp(g, b):
        return np.stack([_bc(g), _bc(b)], 0)

    def dp_common(i):
        return {"w_mem_kv": np.asarray(w_mem_kv[i], f32), "w_out": np.asarray(w_out[i], f32),
                "ln1": lnp(ln1_g[i], ln1_b[i]), "w_router": np.asarray(w_router[i], f32),
                "b_router": _bc(b_router[i])}

    def ffn_maps(i, xg_all):
        xs = np.stack([a.reshape(NE, CAP, D) for a in xg_all], 0)
        maps = []
        for c in range(NCORES):
            sl = slice(4 * c, 4 * c + 4)
            xi = np.ascontiguousarray(xs[:, sl].transpose(1, 0, 2, 3)).reshape(4 * NCORES * CAP, D)
            maps.append({
                "c_ident_bf": cst["ident_bf"], "xg_i": xi,
                "w_gu": np.asarray(w_gu[i, sl], f32),
                "b_gu": np.ascontiguousarray(np.asarray(b_gu[i, sl], f32).reshape(4, 32, 128).transpose(0, 2, 1)),
                "w_down": np.asarray(w_down[i, sl], f32),
                "b_down": np.stack([_bc(b_down[i, e]) for e in range(4 * c, 4 * c + 4)], 0),
            })
        return maps

    def y_back(res):
        ys = np.stack([r["y_o"].reshape(4, NCORES, CAP, D) for r in res], 0)
        out = []
        for s_ in range(NCORES):
            out.append(np.ascontiguousarray(ys[:, :, s_]).reshape(NROWS, D))
        return out

    convw = np.ascontiguousarray(np.asarray(mlstm_conv_w[0], f32).reshape(4, 12, 128).transpose(2, 1, 0))
    convb = np.ascontiguousarray(np.asarray(mlstm_conv_b[0], f32).reshape(12, 128).T)
    gbias = _bc(np.concatenate([np.asarray(mlstm_b_igate[0], f32), np.asarray(mlstm_b_fgate[0], f32)]))
    maps = []
    for c in range(NCORES):
        m = {"x": np.asarray(x[c], f32), "mem": np.asarray(mem[c], f32), "w_in": np.asarray(mlstm_w_in[0], f32),
             "convw": convw, "convb": convb, "gbias": gbias}
        m.update(dp_common(0)); m.update(consts_for(base))
        maps.append(m)
    r1 = _run("k1", maps)
    r2 = _run("ffn", ffn_maps(0, [r["xg_o"] for r in r1]))
    yb = y_back(r2)
    maps = []
    for c in range(NCORES):
        m = {"x1_i": r1[c]["x1_o"], "y_i": yb[c], "idx_i": r1[c]["idx_o"], "gk_i": r1[c]["gk_o"],
             "ln2": lnp(ln2_g[0], ln2_b[0]), "mem": np.asarray(mem[c], f32), "w_in": np.asarray(moba_w_in[0], f32)}
        m.update(dp_common(1)); m.update(consts_for(k3n))
        maps.append(m)
    r3 = _run("k3", maps)
    r4 = _run("ffn", ffn_maps(1, [r["xg_o"] for r in r3]))
    yb = y_back(r4)
    maps = []
    for c in range(NCORES):
        m = {"x1_i": r3[c]["x1_o"], "y_i": yb[c], "idx_i": r3[c]["idx_o"], "gk_i": r3[c]["gk_o"],
             "ln2": lnp(ln2_g[1], ln2_b[1])}
        m.update(consts_for(base))
        maps.append(m)
    r5 = _run("k5", maps)
    return np.stack([r["out"] for r in r5], 0).astype(np.float32)
```

```python
import contextlib
import numpy as np
import ml_dtypes
import concourse.bass as bass
import concourse.mybir as mybir
from concourse.bass_utils import run_bass_kernel_spmd

F32 = mybir.dt.float32
BF16 = mybir.dt.bfloat16
I32 = mybir.dt.int32
ALU = mybir.AluOpType
AF = mybir.ActivationFunctionType
AX = mybir.AxisListType
NPBF = ml_dtypes.bfloat16

S = 2048
D = 2048
NT = 16
NE = 32
CAP = 384
NROWS = NE * CAP
ALPHA = 4 ** 0.25
EPS = 1e-5
BIGM = 30000.0
NCORES = 8


class Prog:
    ENGS = ("pe", "act", "dve", "pool", "sp")

    def __init__(self, nc, n_dma_sems=10):
        self.nc = nc
        self.ops = []
        self.eng_ops = {e: [] for e in self.ENGS}
        self.state = {}
        self.n_dma_sems = n_dma_sems
        self.dma_rr = {e: 0 for e in self.ENGS}
        self.dma_last = {}
        self.last_on = {}

    def op(self, eng, fn, reads=(), writes=(), dma=False, extra=()):
        oid = len(self.ops)
        deps = set()
        for k in reads:
            st = self.state.get(k)
            if st is not None:
                for d in st[0].values():
                    deps.add((d, "raw"))
        for k in writes:
            st = self.state.get(k)
            if st is not None:
                for d in st[0].values():
                    deps.add((d, "waw"))
                for d in st[1].values():
                    deps.add((d, "war"))
        o = dict(id=oid, eng=eng, fn=fn, dma=dma, deps=set(extra), needed=False, slot=None)
        for (d, kind) in deps:
            do = self.ops[d]
            if not do["dma"] and not dma and do["eng"] == eng:
                if eng == "pe" or kind != "raw":
                    continue
            o["deps"].add(d)
        ek = (eng, None)
        if dma:
            slot = self.dma_rr[eng] % self.n_dma_sems
            self.dma_rr[eng] += 1
            o["slot"] = slot
            ek = (eng, slot)
            prev = self.dma_last.get((eng, slot))
            if prev is not None:
                o["deps"].add(prev)
            self.dma_last[(eng, slot)] = oid
        for d in o["deps"]:
            self.ops[d]["needed"] = True
        self.ops.append(o)
        self.eng_ops[eng].append(oid)
        if fn is not None and not dma:
            self.last_on[eng] = oid
        for k in reads:
            st = self.state.setdefault(k, [{}, {}])
            st[1][ek] = oid
        for k in writes:
            st = self.state.setdefault(k, [{}, {}])
            st[0][ek] = oid
        return oid

    def barrier(self):
        deps = set(self.last_on.values()) | set(self.dma_last.values())
        for e in self.ENGS:
            self.op(e, None, extra=[d for d in deps])
        self.state = {}

    def emit(self, final_wait_ops=()):
        nc = self.nc
        with contextlib.ExitStack() as es:
            sem_eng = {e: es.enter_context(nc.semaphore("s_" + e)) for e in self.ENGS}
            sem_dma = {}
            for e in ("sp", "act", "pool"):
                for s in range(min(self.n_dma_sems, self.dma_rr[e])):
                    sem_dma[(e, s)] = es.enter_context(nc.semaphore(f"d_{e}{s}"))
            cnt = {e: 0 for e in self.ENGS}
            dcnt = {}
            for o in self.ops:
                if o["dma"]:
                    key = (o["eng"], o["slot"])
                    dcnt[key] = dcnt.get(key, 0) + 16
                    o["tok"] = (sem_dma[key], dcnt[key], key)
                elif o["needed"]:
                    assert o["fn"] is not None
                    cnt[o["eng"]] += 1
                    o["tok"] = (sem_eng[o["eng"]], cnt[o["eng"]], o["eng"])
                else:
                    o["tok"] = None
            final = [self.ops[i]["tok"] for i in final_wait_ops]
            block = es.enter_context(nc.Block())
            regs = {"pe": block.tensor, "act": block.scalar, "dve": block.vector,
                    "pool": block.gpsimd, "sp": block.sync}
            for e in self.ENGS:
                def body(engine, e=e):
                    waited = {}
                    if e == "pool":
                        self.bc_reg = engine.to_reg(NROWS - 1)
                    for oid in self.eng_ops[e]:
                        o = self.ops[oid]
                        need = {}
                        for d in o["deps"]:
                            sem, val, key = self.ops[d]["tok"]
                            if waited.get(key, 0) >= val:
                                continue
                            if need.get(key, (None, 0))[1] < val:
                                need[key] = (sem, val)
                        for key, (sem, val) in need.items():
                            engine.wait_ge(sem, val)
                            waited[key] = val
                        if o["fn"] is None:
                            continue
                        ins = o["fn"](engine)
                        if o["tok"] is not None:
                            ins.then_inc(o["tok"][0], 16 if o["dma"] else 1)
                    if e == "sp":
                        for (sem, val, key) in final:
                            if waited.get(key, 0) < val:
                                engine.wait_ge(sem, val)
                                waited[key] = val
                regs[e](body)


SB_BASE = 16512
SB_LIMIT = 229376


class Scope:
    def __init__(self, top):
        self.top = top


class Ctx:
    def __init__(self, nc):
        self.nc = nc
        self.P = Prog(nc)
        self.es = contextlib.ExitStack()
        self.root = Scope(SB_BASE)
        self.uid = 0
        self.outs = []

    def dram(self, name, shape, dt, kind):
        return self.nc.dram_tensor(name, list(shape), dt, kind=kind).ap()

    def sb(self, sc, name, shape, dt):
        n = 1
        for d in shape[1:]:
            n *= d
        size = n * (4 if dt in (F32, I32) else 2)
        size = (size + 31) // 32 * 32
        off = sc.top
        sc.top += size
        assert sc.top <= SB_LIMIT, (name, sc.top)
        return self.nc.alloc_sbuf_tensor_at(name, list(shape), dt, offset=off)

    @contextlib.contextmanager
    def scope(self, parent):
        yield Scope(parent.top)

    def ps(self, name, shape, dt):
        return self.es.enter_context(self.nc.psum_tensor(name, list(shape), dt))

    def dma(self, q, out, in_, r, w):
        return self.P.op(q, lambda e: e.dma_start(out=out, in_=in_), reads=r, writes=w, dma=True)

    def mm(self, out, lhsT, rhs, start, stop, r, w):
        return self.P.op("pe", lambda e: e.matmul(out, lhsT=lhsT, rhs=rhs, start=start, stop=stop), reads=r, writes=w)

    def tr(self, out, in_, ident, r, w):
        return self.P.op("pe", lambda e: e.transpose(out, in_, ident), reads=r, writes=w)

    def act(self, out, in_, func, r, w, bias=0.0, scale=1.0, accum_out=None):
        if accum_out is None:
            f = lambda e: e.activation(out=out, in_=in_, func=func, bias=bias, scale=scale)
        else:
            f = lambda e: e.activation(out=out, in_=in_, func=func, bias=bias, scale=scale, accum_out=accum_out)
        return self.P.op("act", f, reads=r, writes=w)

    def ts(self, eng, out, in0, s1, s2, op0, op1, r, w):
        if op1 is None:
            f = lambda e: e.tensor_scalar(out=out, in0=in0, scalar1=s1, scalar2=None, op0=op0)
        else:
            f = lambda e: e.tensor_scalar(out=out, in0=in0, scalar1=s1, scalar2=s2, op0=op0, op1=op1)
        return self.P.op(eng, f, reads=r, writes=w)

    def tt(self, eng, out, in0, in1, op, r, w):
        return self.P.op(eng, lambda e: e.tensor_tensor(out=out, in0=in0, in1=in1, op=op), reads=r, writes=w)

    def stt(self, out, in0, scalar, in1, op0, op1, r, w, accum_out=None):
        if accum_out is None:
            f = lambda e: e.scalar_tensor_tensor(out=out, in0=in0, scalar=scalar, in1=in1, op0=op0, op1=op1)
        else:
            f = lambda e: e.scalar_tensor_tensor(out=out, in0=in0, scalar=scalar, in1=in1, op0=op0, op1=op1, accum_out=accum_out)
        return self.P.op("dve", f, reads=r, writes=w)

    def cp(self, eng, out, in_, r, w):
        if eng == "act":
            return self.P.op("act", lambda e: e.activation(out=out, in_=in_, func=AF.Copy), reads=r, writes=w)
        return self.P.op(eng, lambda e: e.tensor_copy(out=out, in_=in_), reads=r, writes=w)

    def memset(self, eng, ap, val, w):
        return self.P.op(eng, lambda e: e.memset(ap, val), writes=w)

    def recip(self, out, in_, r, w):
        return self.P.op("dve", lambda e: e.reciprocal(out=out, in_=in_), reads=r, writes=w)


def host_consts():
    c = {}
    c["ident_bf"] = np.eye(128, dtype=np.float32).astype(NPBF)
    c["ident_f"] = np.eye(128, dtype=np.float32)
    j = np.arange(128)[:, None]
    l = np.arange(128)[None, :]
    c["tri_incl_f"] = (j <= l).astype(np.float32)
    c["tri_strict_bf"] = (j < l).astype(np.float32).astype(NPBF)
    c["ones_f"] = np.ones((128, 128), np.float32)
    c["ones_bf"] = np.ones((128, 128), np.float32).astype(NPBF)
    q = np.arange(512)[None, None, :]
    r = np.arange(4)[None, :, None]
    jj = np.arange(128)[:, None, None]
    c["causal_bf"] = ((128 * r + jj) <= q).astype(np.float32).astype(NPBF)
    half = 16
    inv = 500000.0 ** (-np.arange(half, dtype=np.float32) * 2.0 / 32.0)
    ang = np.arange(S, dtype=np.float32)[None, :] * inv[:, None]
    cos = np.cos(ang).astype(np.float32)
    sin = np.sin(ang).astype(np.float32)
    c["rope_cos"] = np.concatenate([cos, cos], 0)
    c["rope_sin"] = np.concatenate([-sin, sin], 0)
    R = np.zeros((128, 128), np.float32)
    for i in range(16):
        R[i + 16, i] = 1.0
        R[i, i + 16] = 1.0
    c["rot_f"] = R
    vm = np.zeros((128, 16, 8), np.float32)
    for tt in range(16):
        vm[:, tt, : tt // 2] = 1.0
    c["moba_valid"] = vm
    c["moba_neg"] = ((1.0 - vm) * -1e30).astype(np.float32)
    es = np.zeros((128, 8, 128), np.float32)
    for n in range(8):
        es[n, n, :] = 1.0
    c["esel_bf"] = es.astype(NPBF)
    c["ebase"] = np.broadcast_to((np.arange(NE, dtype=np.float32) * CAP)[None, :], (128, NE)).copy()
    return c


CONST_SPECS = {
    "ident_bf": ([128, 128], BF16), "ident_f": ([128, 128], F32), "tri_incl_f": ([128, 128], F32),
    "tri_strict_bf": ([128, 128], BF16), "ones_f": ([128, 128], F32), "ones_bf": ([128, 128], BF16),
    "causal_bf": ([128, 4, 512], BF16), "rope_cos": ([32, S], F32), "rope_sin": ([32, S], F32),
    "rot_f": ([128, 128], F32), "moba_valid": ([128, 16, 8], F32), "moba_neg": ([128, 16, 8], F32),
    "esel_bf": ([128, 8, 128], BF16), "ebase": ([128, NE], F32),
}


def load_consts(C, es, names):
    t = {}
    for n in names:
        shape, dt = CONST_SPECS[n]
        d = C.dram("c_" + n, shape, dt, "ExternalInput")
        s = C.sb(es, "k_" + n, shape, dt)
        C.dma("sp", s[:], d, [], ["k_" + n])
        t[n] = s
    return t


def layer_norm_tile(C, K, r, gbc, bbc, out, tagr, tagw):
    st, mv, sd = K["ln_st"], K["ln_mv"], K["ln_sd"]
    for i in range(4):
        C.P.op("dve", lambda e, i=i: e.bn_stats(out=st[:, 6 * i:6 * i + 6], in_=r[:, 512 * i:512 * i + 512]),
               reads=[tagr], writes=["ln_st"])
    C.P.op("dve", lambda e: e.bn_aggr(out=mv[:], in_=st[:]), reads=["ln_st"], writes=["ln_mv"])
    C.act(sd[:], mv[:, 1:2], AF.Sqrt, ["ln_mv", "c_eps"], ["ln_sd"], bias=K["eps"][:], scale=1.0)
    C.recip(sd[:], sd[:], ["ln_sd"], ["ln_sd"])
    C.ts("dve", out, r, mv[:, 0:1], sd[:, 0:1], ALU.subtract, ALU.mult, [tagr, "ln_mv", "ln_sd"], [tagw])
    C.tt("pool", out, out, gbc, ALU.mult, [tagw, "lnp"], [tagw])
    C.tt("pool", out, out, bbc, ALU.add, [tagw, "lnp"], [tagw])


def emit_xT(C, K, src_bf, tt, xT, tag_src):
    for g in range(4):
        pt = K["ptr"][g % 2]
        for j in range(4):
            kc = 4 * g + j
            C.tr(pt[:, j, :], src_bf[:, 128 * kc:128 * kc + 128], K["ident_bf"][:], [tag_src, "k_ident_bf"], [f"ptr{g%2}"])
        C.cp("dve" if g % 2 == 0 else "act", xT[:, 4 * g:4 * g + 4, 128 * tt:128 * tt + 128], pt[:], [f"ptr{g%2}"], ["xT"])


def mem_attention(C, K, es, xT, hT_d, memd, wkv_d, w_in_d, qm_col0):
    P = C.P
    pb = K["pb"]
    wq = C.sb(es, "ma_wq", [128, 16, 512], BF16)
    wkv_v = wkv_d.rearrange("(kc p) c -> p kc c", p=128)
    w_in_v = w_in_d.rearrange("(kc p) c -> p kc c", p=128)
    C.dma("pool", wq[:], w_in_v[:, :, qm_col0:qm_col0 + 512], [], ["ma_wq"])
    wkv = C.sb(es, "ma_wkv", [128, 16, 1024], BF16)
    C.dma("pool", wkv[:, :, 0:512], wkv_v[:, :, 0:512], [], ["ma_wkv0"])
    C.dma("pool", wkv[:, :, 512:1024], wkv_v[:, :, 512:1024], [], ["ma_wkv1"])
    mem_bf = C.sb(es, "ma_mem", [128, 2, 2048], BF16)
    memT = C.sb(es, "ma_memT", [128, 16, 256], BF16)
    for mt in range(2):
        C.dma("pool", mem_bf[:, mt, :], memd[128 * mt:128 * mt + 128, :], [], [f"ma_mem{mt}"])
        for g in range(4):
            pt = K["ptr"][g % 2]
            for j in range(4):
                kc = 4 * g + j
                C.tr(pt[:, j, :], mem_bf[:, mt, 128 * kc:128 * kc + 128], K["ident_bf"][:], [f"ma_mem{mt}", "k_ident_bf"], [f"ptr{g%2}"])
            C.cp("dve", memT[:, 4 * g:4 * g + 4, 128 * mt:128 * mt + 128], pt[:], [f"ptr{g%2}"], ["ma_memT"])
    kmT = C.sb(es, "ma_kmT", [128, 4, 256], BF16)
    vm = C.sb(es, "ma_vm", [128, 2, 512], BF16)
    qmT = C.sb(es, "ma_qmT", [128, 4, 2048], BF16)
    for h in range(4):
        for kc in range(16):
            C.mm(pb[0][:, 0:256], wkv[:, kc, 128 * h:128 * h + 128], memT[:, kc, :], kc == 0, kc == 15, ["ma_wkv0", "ma_memT"], ["pb0"])
        C.cp("act", kmT[:, h, :], pb[0][:, 0:256], ["pb0"], ["ma_kmT"])
    for mt in range(2):
        for kc in range(16):
            C.mm(pb[1][:], memT[:, kc, 128 * mt:128 * mt + 128], wkv[:, kc, 512:1024], kc == 0, kc == 15, ["ma_wkv1", "ma_memT"], ["pb1"])
        C.cp("act", vm[:, mt, :], pb[1][:], ["pb1"], ["ma_vm"])
    for h in range(4):
        for tg in range(4):
            b = pb[(h * 4 + tg) % 2]
            bk = f"pb{(h * 4 + tg) % 2}"
            for kc in range(16):
                C.mm(b[:], wq[:, kc, 128 * h:128 * h + 128], xT[:, kc, 512 * tg:512 * tg + 512], kc == 0, kc == 15, ["ma_wq", "xT"], [bk])
            C.cp("dve" if tg % 2 else "act", qmT[:, h, 512 * tg:512 * tg + 512], b[:], [bk], ["ma_qmT"])
    eT = C.sb(es, "ma_eT", [128, 2, 512], BF16)
    rdn = C.sb(es, "ma_rdn", [128, 512], F32)
    hst = [C.sb(es, f"ma_hst{i}", [128, 512], BF16) for i in range(2)]
    scale = 128 ** -0.5
    it = 0
    for h in range(4):
        for tg in range(4):
            for mt in range(2):
                C.mm(pb[2 + mt][:], kmT[:, h, 128 * mt:128 * mt + 128], qmT[:, h, 512 * tg:512 * tg + 512], True, True, ["ma_kmT", "ma_qmT"], [f"pb{2+mt}"])
                C.act(eT[:, mt, :], pb[2 + mt][:], AF.Exp, [f"pb{2+mt}"], [f"ma_eT{mt}"], scale=scale)
            for mt in range(2):
                C.mm(pb[4][:], vm[:, mt, 128 * h:128 * h + 128], eT[:, mt, :], mt == 0, mt == 1, ["ma_vm", f"ma_eT{mt}"], ["pb4"])
            for mt in range(2):
                C.mm(pb[5][:], K["ones_bf"][:], eT[:, mt, :], mt == 0, mt == 1, ["k_ones_bf", f"ma_eT{mt}"], ["pb5"])
            C.recip(rdn[:], pb[5][:], ["pb5"], ["ma_rdn"])
            hs = hst[it % 2]
            C.tt("dve", hs[:], pb[4][:], rdn[:], ALU.mult, ["pb4", "ma_rdn"], [f"ma_hst{it%2}"])
            C.dma("sp", hT_d[1536 + 128 * h:1536 + 128 * h + 128, 512 * tg:512 * tg + 512], hs[:], [f"ma_hst{it%2}"], ["hT_d"])
            it += 1


def _interleave(ga, gb):
    da = db = False
    while not (da and db):
        if not da:
            try:
                next(ga)
            except StopIteration:
                da = True
        if not db:
            try:
                next(gb)
            except StopIteration:
                db = True


def _empty():
    return
    yield


def mixer_mlstm(C, K, es, xT, hT_d, w_in_d, convw_d, convb_d, gbias_d):
    pb = K["pb"]
    w_in_v = w_in_d.rearrange("(kc p) c -> p kc c", p=128)
    cw = C.sb(es, "ml_cw", [128, 12, 4], F32)
    cb = C.sb(es, "ml_cb", [128, 12], F32)
    gb = C.sb(es, "ml_gb", [128, 12], F32)
    C.dma("sp", cw[:], convw_d, [], ["ml_cw"])
    C.dma("sp", cb[:], convb_d, [], ["ml_cb"])
    C.dma("sp", gb[:], gbias_d, [], ["ml_gb"])
    wg = C.sb(es, "ml_wg", [128, 16, 12], BF16)
    C.dma("pool", wg[:], w_in_v[:, :, 4608:4620], [], ["ml_wg"])
    gts = C.sb(es, "ml_gts", [128, 16, 12], F32)
    for tt in range(NT):
        for kc in range(16):
            C.mm(pb[0][:, 12 * tt:12 * tt + 12], xT[:, kc, 128 * tt:128 * tt + 128], wg[:, kc, :], kc == 0, kc == 15, ["xT", "ml_wg"], ["pb0"])
    for tt in range(NT):
        C.tt("dve", gts[:, tt, :], pb[0][:, 12 * tt:12 * tt + 12], gb[:], ALU.add, ["pb0", "ml_gb"], ["ml_gts"])
    lf = C.sb(es, "ml_lf", [128, 16, 6], F32)
    C.act(lf[:], gts[:, :, 6:12], AF.Exp, ["ml_gts"], ["ml_lf"], scale=-1.0)
    C.act(lf[:], lf[:], AF.Ln, ["ml_lf", "c_one"], ["ml_lf"], bias=K["one"][:], scale=1.0)
    lf2 = lf[:].rearrange("p a b -> p (a b)")
    C.mm(pb[1][:, 0:96], K["tri_incl_f"][:], lf2, True, True, ["k_tri_incl_f", "ml_lf"], ["pb1"])
    C.mm(pb[1][:, 128:224], K["ones_f"][:], lf2, True, True, ["k_ones_f", "ml_lf"], ["pb1"])
    ksc = C.sb(es, "ml_ksc", [128, 96], F32)
    qsc = C.sb(es, "ml_qsc", [128, 96], F32)
    egd = C.sb(es, "ml_eg", [128, 96], F32)
    tmp96 = C.sb(es, "ml_t96", [128, 16, 6], F32)
    C.tt("dve", tmp96[:], gts[:, :, 0:6], pb[1][:, 0:96].rearrange("p (a b) -> p a b", b=6), ALU.add, ["ml_gts", "pb1"], ["ml_t96"])
    C.act(ksc[:], tmp96[:].rearrange("p a b -> p (a b)"), AF.Exp, ["ml_t96", "c_lnk"], ["ml_ksc"], bias=K["lnk"][:], scale=1.0)
    C.act(qsc[:], pb[1][:, 0:96], AF.Exp, ["pb1"], ["ml_qsc"], scale=-1.0)
    C.act(egd[:], pb[1][:, 128:224], AF.Exp, ["pb1"], ["ml_eg"], scale=-1.0)
    wq = C.sb(es, "ml_wq", [128, 16, 128], BF16)
    wk = C.sb(es, "ml_wk", [128, 16, 128], BF16)
    wv = C.sb(es, "ml_wv", [128, 16, 256], BF16)
    wo = C.sb(es, "ml_wo", [128, 16, 256], BF16)
    uq = C.sb(es, "ml_uq", [128, 3 + S], F32)
    cq = C.sb(es, "ml_cq", [128, S], F32)
    qTs = [C.sb(es, f"ml_qT{i}", [128, S], BF16) for i in range(2)]
    kTs = [C.sb(es, f"ml_kT{i}", [128, S], BF16) for i in range(2)]
    ktss = [C.sb(es, f"ml_kts{i}", [128, 16, 128], BF16) for i in range(2)]
    vxs = [C.sb(es, f"ml_vx{i}", [128, 16, 264], BF16) for i in range(2)]
    ogs = [C.sb(es, f"ml_og{i}", [128, 16, 256], BF16) for i in range(2)]
    Cf = C.sb(es, "ml_Cf", [128, 264], F32)
    Cb = C.sb(es, "ml_Cb", [128, 264], BF16)
    stm = C.sb(es, "ml_stm", [128, 128], BF16)
    hm = C.sb(es, "ml_hm", [128, 256], BF16)
    sm = C.sb(es, "ml_sm", [128, 4], F32)
    hst = C.sb(es, "ml_hst", [128, 2, S], BF16)
    mask = K["tri_incl_f"]
    C.memset("pool", uq[:, 0:3], 0.0, ["ml_uq"])
    NH = 6

    def prep(h):
        p = h % 2
        qT, kT, kts, vx, og = qTs[p], kTs[p], ktss[p], vxs[p], ogs[p]
        C.dma("pool", wq[:], w_in_v[:, :, 128 * h:128 * h + 128], [], ["ml_wq"])
        C.dma("pool", wk[:], w_in_v[:, :, 768 + 128 * h:768 + 128 * h + 128], [], ["ml_wk"])
        C.dma("pool", wv[:], w_in_v[:, :, 1536 + 256 * h:1536 + 256 * h + 256], [], ["ml_wv"])
        C.dma("pool", wo[:], w_in_v[:, :, 3072 + 256 * h:3072 + 256 * h + 256], [], ["ml_wo"])
        for (w_, wtag, cidx, dst, dtag) in ((wq, "ml_wq", h, qT, f"ml_qT{p}"), (wk, "ml_wk", 6 + h, kT, f"ml_kT{p}")):
            for tg in range(4):
                b = pb[2 + tg % 2]
                bk = f"pb{2 + tg % 2}"
                for kc in range(16):
                    C.mm(b[:], w_[:, kc, :], xT[:, kc, 512 * tg:512 * tg + 512], kc == 0, kc == 15, [wtag, "xT"], [bk])
                C.cp("act", uq[:, 3 + 512 * tg:3 + 512 * tg + 512], b[:], [bk], ["ml_uq"])
                yield
            C.ts("dve", cq[:], uq[:, 0:S], cw[:, cidx, 0:1], cb[:, cidx:cidx + 1], ALU.mult, ALU.add, ["ml_uq", "ml_cw", "ml_cb"], ["ml_cq"])
            for w in range(1, 4):
                C.stt(cq[:], uq[:, w:w + S], cw[:, cidx, w:w + 1], cq[:], ALU.mult, ALU.add, ["ml_uq", "ml_cw", "ml_cq"], ["ml_cq"])
            C.act(dst[:], cq[:], AF.Silu, ["ml_cq"], [dtag])
            yield
        for tt in range(NT):
            b = pb[2 + tt % 2]
            bk = f"pb{2 + tt % 2}"
            for kc in range(16):
                C.mm(b[:, 0:256], xT[:, kc, 128 * tt:128 * tt + 128], wv[:, kc, :], kc == 0, kc == 15, ["xT", "ml_wv"], [bk])
            C.cp("dve", vx[:, tt, 0:256], b[:, 0:256], [bk], [f"ml_vx{p}"])
            yield
        C.memset("pool", vx[:, :, 256:257], 1.0, [f"ml_vx{p}"])
        for tt in range(NT):
            b = pb[2 + tt % 2]
            bk = f"pb{2 + tt % 2}"
            for kc in range(16):
                C.mm(b[:, 0:256], xT[:, kc, 128 * tt:128 * tt + 128], wo[:, kc, :], kc == 0, kc == 15, ["xT", "ml_wo"], [bk])
            C.act(og[:, tt, :], b[:, 0:256], AF.Sigmoid, [bk], [f"ml_og{p}"])
            yield
        for tt in range(NT):
            pt = K["ptr"][0]
            C.tr(pt[:, tt % 4, :], kT[:, 128 * tt:128 * tt + 128], K["ident_bf"][:], [f"ml_kT{p}", "k_ident_bf"], ["ptr0"])
            C.ts("dve", kts[:, tt, :], pt[:, tt % 4, :], ksc[:, 6 * tt + h:6 * tt + h + 1], None, ALU.mult, None, ["ptr0", "ml_ksc"], [f"ml_kts{p}"])
            if tt % 4 == 3:
                yield

    def chunks(h):
        p = h % 2
        qT, kT, kts, vx, og = qTs[p], kTs[p], ktss[p], vxs[p], ogs[p]
        C.memset("pool", Cf[:], 0.0, ["ml_Cf"])
        C.memset("pool", Cb[:], 0.0, ["ml_Cb"])
        for c in range(NT):
            sl = slice(128 * c, 128 * c + 128)
            i6 = 6 * c + h
            C.mm(pb[4][:, 0:128], kT[:, sl], qT[:, sl], True, True, [f"ml_kT{p}", f"ml_qT{p}"], ["pb4"])
            C.stt(stm[:], pb[4][:, 0:128], ksc[:, i6:i6 + 1], mask[:], ALU.mult, ALU.mult, ["pb4", "ml_ksc", "k_tri_incl_f"], ["ml_stm"])
            C.mm(pb[0][:, 0:257], kts[:, c, :], vx[:, c, 0:257], True, True, [f"ml_kts{p}", f"ml_vx{p}"], ["pb0"])
            C.mm(pb[5][:, 0:257], qT[:, sl], Cb[:, 0:257], True, False, [f"ml_qT{p}", "ml_Cb"], ["pb5"])
            C.mm(pb[5][:, 0:257], stm[:], vx[:, c, 0:257], False, True, ["ml_stm", f"ml_vx{p}"], ["pb5"])
            yield
            C.act(sm[:, 3:4], pb[5][:, 256:257], AF.Abs, ["pb5", "ml_qsc"], ["ml_sm3"], scale=qsc[:, i6:i6 + 1])
            C.ts("dve", sm[:, 0:1], sm[:, 3:4], 1.0, None, ALU.max, None, ["ml_sm3"], ["ml_sm0"])
            C.recip(sm[:, 1:2], sm[:, 0:1], ["ml_sm0"], ["ml_sm1"])
            C.tt("dve", sm[:, 2:3], sm[:, 1:2], qsc[:, i6:i6 + 1], ALU.mult, ["ml_sm1", "ml_qsc"], ["ml_sm2"])
            C.stt(hm[:], pb[5][:, 0:256], sm[:, 2:3], og[:, c, :], ALU.mult, ALU.mult, ["pb5", "ml_sm2", f"ml_og{p}"], ["ml_hm"])
            C.tt("dve", Cf[:, 0:257], pb[0][:, 0:257], Cf[:, 0:257], ALU.add, ["pb0", "ml_Cf"], ["ml_Cf"])
            C.ts("dve", Cf[:, 0:257], Cf[:, 0:257], egd[:, i6:i6 + 1], None, ALU.mult, None, ["ml_Cf", "ml_eg"], ["ml_Cf"])
            C.cp("act", Cb[:, 0:257], Cf[:, 0:257], ["ml_Cf"], ["ml_Cb"])
            pt = K["ptr"][1]
            for j in range(2):
                C.tr(pt[:, j, :], hm[:, 128 * j:128 * j + 128], K["ident_bf"][:], ["ml_hm", "k_ident_bf"], ["ptr1"])
            C.cp("act", hst[:, :, sl], pt[:, 0:2, :], ["ptr1"], ["ml_hst"])
            yield
        for j in range(2):
            C.dma("sp", hT_d[256 * h + 128 * j:256 * h + 128 * j + 128, :], hst[:, j, :], ["ml_hst"], ["hT_d"])

    for _ in prep(0):
        pass
    for h in range(NH):
        _interleave(chunks(h), prep(h + 1) if h + 1 < NH else _empty())


def mixer_moba(C, K, es, xT, hT_d, w_in_d):
    pb = K["pb"]
    w_in_v = w_in_d.rearrange("(kc p) c -> p kc c", p=128)
    wq = C.sb(es, "mo_wq", [128, 16, 128], BF16)
    wk = C.sb(es, "mo_wk", [128, 16, 128], BF16)
    wv = C.sb(es, "mo_wv", [128, 16, 128], BF16)
    zf = C.sb(es, "mo_zf", [128, 512], F32)
    qf = C.sb(es, "mo_qf", [128, S], F32)
    kf = C.sb(es, "mo_kf", [128, S], F32)
    qTs = [C.sb(es, f"mo_qT{i}", [128, S], BF16) for i in range(2)]
    kTs = [C.sb(es, f"mo_kT{i}", [128, S], BF16) for i in range(2)]
    vts = [C.sb(es, f"mo_vt{i}", [128, 16, 128], BF16) for i in range(2)]
    negTs = [C.sb(es, f"mo_negT{i}", [128, S], BF16) for i in range(2)]
    t1 = C.sb(es, "mo_t1", [32, 512], F32)
    kbar = C.sb(es, "mo_kbar", [128, 8], F32)
    gm = C.sb(es, "mo_gm", [128, 16, 8], F32)
    top8 = C.sb(es, "mo_top8", [128, 8], F32)
    negm = C.sb(es, "mo_negm", [128, 16, 8], F32)
    pT = [C.sb(es, f"mo_pT{i}", [128, 512], BF16) for i in range(2)]
    rdn = C.sb(es, "mo_rdn", [128, 512], F32)
    hst = [C.sb(es, f"mo_hst{i}", [128, 512], BF16) for i in range(2)]
    cos, sin = K["rope_cos"], K["rope_sin"]
    for i in range(2):
        C.memset("pool", negTs[i][:], 0.0, [f"mo_negT{i}"])
    scale = 128 ** -0.5
    NH = 12

    def prep(h):
        p = h % 2
        qT, kT, vt, negT = qTs[p], kTs[p], vts[p], negTs[p]
        C.dma("pool", wq[:], w_in_v[:, :, 128 * h:128 * h + 128], [], ["mo_wq"])
        C.dma("pool", wk[:], w_in_v[:, :, 1536 + 128 * h:1536 + 128 * h + 128], [], ["mo_wk"])
        C.dma("pool", wv[:], w_in_v[:, :, 3072 + 128 * h:3072 + 128 * h + 128], [], ["mo_wv"])
        for (w_, wtag, df, dftag, db, dbtag) in ((wq, "mo_wq", qf, "mo_qf", qT, f"mo_qT{p}"), (wk, "mo_wk", kf, "mo_kf", kT, f"mo_kT{p}")):
            for tg in range(4):
                ts_ = slice(512 * tg, 512 * tg + 512)
                for kc in range(16):
                    C.mm(pb[4][:], w_[:, kc, :], xT[:, kc, ts_], kc == 0, kc == 15, [wtag, "xT"], ["pb4"])
                C.cp("act", zf[:], pb[4][:], ["pb4"], ["mo_zf"])
                yield
                C.cp("pool", df[:, ts_], zf[:], ["mo_zf"], [dftag])
                C.mm(pb[5][:], K["rot_f"][:], zf[:], True, True, ["k_rot_f", "mo_zf"], ["pb5"])
                C.tt("dve", t1[:], pb[5][0:32, :], sin[:, ts_], ALU.mult, ["pb5", "k_rope_sin"], ["mo_t1"])
                C.tt("pool", df[0:32, ts_], zf[0:32, :], cos[:, ts_], ALU.mult, ["mo_zf", "k_rope_cos", dftag], [dftag])
                C.tt("dve", df[0:32, ts_], df[0:32, ts_], t1[:], ALU.add, [dftag, "mo_t1"], [dftag])
                yield
            C.cp("act", db[:], df[:], [dftag], [dbtag])
            yield
        for tt in range(NT):
            b = pb[4 + tt % 2]
            bk = f"pb{4 + tt % 2}"
            for kc in range(16):
                C.mm(b[:, 0:128], xT[:, kc, 128 * tt:128 * tt + 128], wv[:, kc, :], kc == 0, kc == 15, ["xT", "mo_wv"], [bk])
            C.cp("dve", vt[:, tt, :], b[:, 0:128], [bk], [f"mo_vt{p}"])
            if tt % 2 == 1:
                yield
        C.P.op("dve", lambda e: e.tensor_reduce(out=kbar[:], in_=kf[:].rearrange("p (n b) -> p n b", b=256), axis=AX.X, op=ALU.add),
               reads=["mo_kf"], writes=["mo_kbar"])
        C.ts("dve", kbar[:], kbar[:], 1.0 / 256.0, None, ALU.mult, None, ["mo_kbar"], ["mo_kbar"])
        yield
        for tt in range(NT):
            C.mm(pb[4][:, 8 * tt:8 * tt + 8], qf[:, 128 * tt:128 * tt + 128], kbar[:], True, True, ["mo_qf", "mo_kbar"], ["pb4"])
        C.tt("dve", gm[:], pb[4][:, 0:128].rearrange("p (a b) -> p a b", b=8), K["moba_neg"][:], ALU.add, ["pb4", "k_moba_neg"], ["mo_gm"])
        yield
        for tt in range(NT):
            C.P.op("dve", lambda e, tt=tt: e.max(out=top8[:], in_=gm[:, tt, :]), reads=["mo_gm"], writes=["mo_top8"])
            C.ts("dve", negm[:, tt, :], gm[:, tt, :], top8[:, 2:3], None, ALU.is_ge, None, ["mo_gm", "mo_top8"], ["mo_negm"])
            if tt % 2 == 1:
                yield
        C.tt("dve", negm[:], negm[:], K["moba_valid"][:], ALU.mult, ["mo_negm", "k_moba_valid"], ["mo_negm"])
        C.tt("dve", negm[:], negm[:], K["moba_valid"][:], ALU.subtract, ["mo_negm", "k_moba_valid"], ["mo_negm"])
        C.ts("dve", negm[:], negm[:], BIGM, None, ALU.mult, None, ["mo_negm"], ["mo_negm"])
        yield
        for g in range(4):
            for j in range(4):
                tt = 4 * g + j
                C.tr(pb[5][0:8, 128 * j:128 * j + 128], negm[:, tt, :], K["ident_f"][:], ["mo_negm", "k_ident_f"], ["pb5"])
            C.cp("act", negT[0:8, 512 * g:512 * g + 512], pb[5][0:8, :], ["pb5"], [f"mo_negT{p}"])
            yield

    def attn(h):
        p = h % 2
        qT, kT, vt, negT = qTs[p], kTs[p], vts[p], negTs[p]
        for g in range(4):
            qs = slice(512 * g, 512 * g + 512)
            nt_ = 4 * g + 4
            for t in range(nt_):
                sb_ = pb[2 + t % 2]
                sk = f"pb{2 + t % 2}"
                p_ = pT[t % 2]
                pk = f"mo_pT{t % 2}"
                C.mm(sb_[:], kT[:, 128 * t:128 * t + 128], qT[:, qs], True, False, [f"mo_kT{p}", f"mo_qT{p}"], [sk])
                C.mm(sb_[:], K["esel_bf"][:, t // 2, :], negT[:, qs], False, True, ["k_esel_bf", f"mo_negT{p}"], [sk])
                C.act(p_[:], sb_[:], AF.Exp, [sk], [pk], scale=scale)
                if t >= 4 * g:
                    C.tt("pool", p_[:], p_[:], K["causal_bf"][:, t - 4 * g, :], ALU.mult, [pk, "k_causal_bf"], [pk])
                C.mm(pb[0][:], vt[:, t, :], p_[:], t == 0, t == nt_ - 1, [f"mo_vt{p}", pk], ["pb0"])
                C.mm(pb[1][:], K["ones_bf"][:], p_[:], t == 0, t == nt_ - 1, ["k_ones_bf", pk], ["pb1"])
                yield
            C.recip(rdn[:], pb[1][:], ["pb1"], ["mo_rdn"])
            hs = hst[g % 2]
            C.tt("dve", hs[:], pb[0][:], rdn[:], ALU.mult, ["pb0", "mo_rdn"], [f"mo_hst{g%2}"])
            C.dma("sp", hT_d[128 * h:128 * h + 128, qs], hs[:], [f"mo_hst{g%2}"], ["hT_d"])
            yield

    for _ in prep(0):
        pass
    for h in range(NH):
        _interleave(attn(h), prep(h + 1) if h + 1 < NH else _empty())


def post_mixer(C, K, es, hT_d, xres_d, wout_d, lnp_d, wr_d, br_d, x1_d, xg_d, idx_d, gk_d):
    pb = K["pb"]
    hcT = C.sb(es, "pm_hcT", [128, 16, S], BF16)
    hv = hT_d.rearrange("(kc p) t -> p kc t", p=128)
    for q4 in range(4):
        C.dma("sp", hcT[:, 4 * q4:4 * q4 + 4, :], hv[:, 4 * q4:4 * q4 + 4, :], ["hT_d"], ["pm_hcT"])
    wout = C.sb(es, "pm_wout", [128, 16, D], BF16)
    wv_ = wout_d.rearrange("(kc p) c -> p kc c", p=128)
    for cg in range(4):
        C.dma("pool", wout[:, :, 512 * cg:512 * cg + 512], wv_[:, :, 512 * cg:512 * cg + 512], [], ["pm_wout"])
    gbc = C.sb(es, "pm_g", [128, D], F32)
    bbc = C.sb(es, "pm_b", [128, D], F32)
    C.dma("sp", gbc[:], lnp_d[0], [], ["lnp"])
    C.dma("sp", bbc[:], lnp_d[1], [], ["lnp"])
    wr = C.sb(es, "pm_wr", [128, 16, NE], F32)
    C.dma("sp", wr[:], wr_d.rearrange("(kc p) e -> p kc e", p=128), [], ["pm_wr"])
    br = C.sb(es, "pm_br", [128, NE], F32)
    C.dma("sp", br[:], br_d, [], ["pm_br"])
    x1s = [C.sb(es, f"pm_x1{i}", [128, D], F32) for i in range(2)]
    x1bs = [C.sb(es, f"pm_x1b{i}", [128, D], BF16) for i in range(2)]
    x1T = C.sb(es, "pm_x1T", [128, 16, 128], F32)
    lg = C.sb(es, "pm_lg", [128, NE], F32)
    top8 = C.sb(es, "pm_top8", [128, 8], F32)
    sel = C.sb(es, "pm_sel", [128, NE], F32)
    selb = C.sb(es, "pm_selb", [128, NE], BF16)
    ex = C.sb(es, "pm_ex", [128, NE], F32)
    sm = C.sb(es, "pm_sm", [128, 4], F32)
    gmv = C.sb(es, "pm_gm", [128, NE], F32)
    cnt = C.sb(es, "pm_cnt", [128, NE], F32)
    pos = C.sb(es, "pm_pos", [128, NE], F32)
    rix = C.sb(es, "pm_rix", [128, NE], F32)
    val = C.sb(es, "pm_val", [128, NE], F32)
    junk = C.sb(es, "pm_junk", [128, NE], F32)
    rks = [C.sb(es, f"pm_rk{i}", [128, 4], F32) for i in range(2)]
    gks = [C.sb(es, f"pm_gk{i}", [128, 4], F32) for i in range(2)]
    ris = [C.sb(es, f"pm_ri{i}", [128, 4], I32) for i in range(2)]
    C.memset("pool", cnt[:], 0.0, ["pm_cnt"])

    def stage_a(tt):
        p = tt % 2
        x1, x1b = x1s[p], x1bs[p]
        xk, xbk = f"pm_x1{p}", f"pm_x1b{p}"
        rows = slice(128 * tt, 128 * tt + 128)
        C.dma("sp", x1[:], xres_d[rows, :], [], [xk])
        for cg in range(4):
            for kc in range(16):
                C.mm(pb[cg][:], hcT[:, kc, rows], wout[:, kc, 512 * cg:512 * cg + 512], kc == 0, kc == 15, ["pm_hcT", "pm_wout"], [f"pb{cg}"])
            C.stt(x1[:, 512 * cg:512 * cg + 512], x1[:, 512 * cg:512 * cg + 512], ALPHA, pb[cg][:], ALU.mult, ALU.add, [xk, f"pb{cg}"], [xk])
            yield
        layer_norm_tile(C, K, x1[:], gbc[:], bbc[:], x1[:], xk, xk)
        yield
        C.dma("sp", x1_d[rows, :], x1[:], [xk], ["x1_d"])
        C.cp("act", x1b[:], x1[:], [xk], [xbk])
        yield

    def stage_b(tt):
        p = tt % 2
        x1, x1b = x1s[p], x1bs[p]
        xk, xbk = f"pm_x1{p}", f"pm_x1b{p}"
        rk, gk, ri = rks[p], gks[p], ris[p]
        rows = slice(128 * tt, 128 * tt + 128)
        for kc in range(16):
            b = pb[4 + kc % 2]
            bk = f"pb{4 + kc % 2}"
            C.tr(b[:, 0:128], x1[:, 128 * kc:128 * kc + 128], K["ident_f"][:], [xk, "k_ident_f"], [bk])
            C.cp("act" if kc % 2 else "dve", x1T[:, kc, :], b[:, 0:128], [bk], [f"pm_x1T{kc}"])
            if kc % 4 == 3:
                yield
        for kc in range(16):
            C.mm(pb[4][:, 256:256 + NE], x1T[:, kc, :], wr[:, kc, :], kc == 0, kc == 15, [f"pm_x1T{kc}", "pm_wr"], ["pb4"])
        C.tt("dve", lg[:], pb[4][:, 256:256 + NE], br[:], ALU.add, ["pb4", "pm_br"], ["pm_lg"])
        yield
        C.P.op("dve", lambda e: e.max(out=top8[:], in_=lg[:]), reads=["pm_lg"], writes=["pm_top8"])
        C.ts("dve", sel[:], lg[:], top8[:, 3:4], None, ALU.is_ge, None, ["pm_lg", "pm_top8"], ["pm_sel"])
        C.cp("pool", selb[:], sel[:], ["pm_sel"], ["pm_selb"])
        C.ts("dve", sm[:, 0:1], top8[:, 0:1], -1.0, None, ALU.mult, None, ["pm_top8"], ["pm_sm0"])
        C.act(ex[:], lg[:], AF.Exp, ["pm_lg", "pm_sm0"], ["pm_ex"], bias=sm[:, 0:1], scale=1.0)
        yield
        C.stt(ex[:], ex[:], 1.0, sel[:], ALU.mult, ALU.mult, ["pm_ex", "pm_sel"], ["pm_ex"], accum_out=sm[:, 1:2])
        C.recip(sm[:, 2:3], sm[:, 1:2], ["pm_ex"], ["pm_sm2"])
        C.ts("dve", gmv[:], ex[:], sm[:, 2:3], None, ALU.mult, None, ["pm_ex", "pm_sm2"], ["pm_gm"])
        C.mm(pb[5][:, 256:256 + NE], K["tri_strict_bf"][:], selb[:], True, True, ["k_tri_strict_bf", "pm_selb"], ["pb5"])
        C.mm(pb[5][:, 320:320 + NE], K["ones_bf"][:], selb[:], True, True, ["k_ones_bf", "pm_selb"], ["pb5"])
        yield
        C.tt("dve", pos[:], pb[5][:, 256:256 + NE], cnt[:], ALU.add, ["pb5", "pm_cnt"], ["pm_pos"])
        C.tt("dve", cnt[:], pb[5][:, 320:320 + NE], cnt[:], ALU.add, ["pb5", "pm_cnt"], ["pm_cnt"])
        C.ts("dve", val[:], pos[:], CAP - 0.5, None, ALU.is_lt, None, ["pm_pos"], ["pm_val"])
        C.tt("dve", val[:], val[:], sel[:], ALU.mult, ["pm_val", "pm_sel"], ["pm_val"])
        C.tt("dve", rix[:], pos[:], K["ebase"][:], ALU.add, ["pm_pos", "k_ebase"], ["pm_rix"])
        yield
        C.ts("dve", rix[:], rix[:], -float(NROWS), None, ALU.add, None, ["pm_rix"], ["pm_rix"])
        C.tt("dve", rix[:], rix[:], val[:], ALU.mult, ["pm_rix", "pm_val"], ["pm_rix"])
        C.ts("dve", rix[:], rix[:], float(NROWS), None, ALU.add, None, ["pm_rix"], ["pm_rix"])
        yield
        for k in range(4):
            C.stt(junk[:], lg[:], top8[:, k:k + 1], rix[:], ALU.is_equal, ALU.mult, ["pm_lg", "pm_top8", "pm_rix"], ["pm_junk"], accum_out=rk[:, k:k + 1])
            C.stt(junk[:], lg[:], top8[:, k:k + 1], gmv[:], ALU.is_equal, ALU.mult, ["pm_lg", "pm_top8", "pm_gm"], ["pm_junk"], accum_out=gk[:, k:k + 1])
            if k % 2 == 1:
                yield
        C.cp("dve", ri[:], rk[:], ["pm_junk"], [f"pm_ri{p}"])
        C.dma("sp", idx_d[rows, :], ri[:], [f"pm_ri{p}"], ["idx_d"])
        C.dma("sp", gk_d[rows, :], gk[:], ["pm_junk"], ["gk_d"])
        yield
        for k in range(4):
            C.P.op("pool", lambda e, k=k, ri=ri, x1b=x1b: e.indirect_dma_start(
                out=xg_d[:, :], out_offset=bass.IndirectOffsetOnAxis(ap=ri[:, k:k + 1], axis=0),
                in_=x1b[:, :], in_offset=None, bounds_check=C.P.bc_reg, oob_is_err=False),
                reads=[f"pm_ri{p}", xbk], writes=["xg_d"], dma=True)
        yield

    for _ in stage_a(0):
        pass
    for tt in range(NT):
        _interleave(stage_b(tt), stage_a(tt + 1) if tt + 1 < NT else _empty())


def combine_ln2(C, K, es, x1_d, y_d, idx_d, gk_d, lnp_d, out_d, xT=None):
    gbc = C.sb(es, "cb_g", [128, D], F32)
    bbc = C.sb(es, "cb_b", [128, D], F32)
    C.dma("sp", gbc[:], lnp_d[0], [], ["lnp"])
    C.dma("sp", bbc[:], lnp_d[1], [], ["lnp"])
    accs = [C.sb(es, f"cb_acc{i}", [128, D], F32) for i in range(2)]
    yks = [[C.sb(es, f"cb_yk{i}_{k}", [128, D], F32) for k in range(4)] for i in range(2)]
    x2 = C.sb(es, "cb_x2", [128, D], F32)
    x2b = C.sb(es, "cb_x2b", [128, D], BF16)
    ris = [C.sb(es, f"cb_ri{i}", [128, 4], I32) for i in range(2)]
    gks = [C.sb(es, f"cb_gk{i}", [128, 4], F32) for i in range(2)]
    outs = []
    for tt in range(NT):
        p = tt % 2
        acc, yk, ri, gk = accs[p], yks[p], ris[p], gks[p]
        rows = slice(128 * tt, 128 * tt + 128)
        C.dma("sp", acc[:], x1_d[rows, :], ["x1_d"], [f"cb_acc{p}"])
        C.dma("sp", ri[:], idx_d[rows, :], ["idx_d"], [f"cb_ri{p}"])
        C.dma("sp", gk[:], gk_d[rows, :], ["gk_d"], [f"cb_gk{p}"])
        for k in range(4):
            C.memset("pool", yk[k][:], 0.0, [f"cb_yk{p}_{k}"])
            C.P.op("pool", lambda e, k=k, yk=yk, ri=ri: e.indirect_dma_start(
                out=yk[k][:, :], out_offset=None, in_=y_d[:, :],
                in_offset=bass.IndirectOffsetOnAxis(ap=ri[:, k:k + 1], axis=0),
                bounds_check=C.P.bc_reg, oob_is_err=False),
                reads=[f"cb_ri{p}", "y_d"], writes=[f"cb_yk{p}_{k}"], dma=True)
        C.ts("dve", acc[:], acc[:], ALPHA, None, ALU.mult, None, [f"cb_acc{p}"], [f"cb_acc{p}"])
        for k in range(4):
            C.stt(acc[:], yk[k][:], gk[:, k:k + 1], acc[:], ALU.mult, ALU.add, [f"cb_yk{p}_{k}", f"cb_gk{p}", f"cb_acc{p}"], [f"cb_acc{p}"])
        layer_norm_tile(C, K, acc[:], gbc[:], bbc[:], x2[:], f"cb_acc{p}", "cb_x2")
        outs.append(C.dma("sp", out_d[rows, :], x2[:], ["cb_x2"], ["x2_d"]))
        if xT is not None:
            C.cp("act", x2b[:], x2[:], ["cb_x2"], ["cb_x2b"])
            emit_xT(C, K, x2b, tt, xT, "cb_x2b")
    return outs


def _build_dp(kind):
    nc = bass.Bass("TRN2", target_bir_lowering=False)
    C = Ctx(nc)
    fin = []
    with C.es:
        es0 = C.root
        names = ["ident_bf", "ident_f", "ones_bf", "ones_f", "tri_incl_f", "tri_strict_bf", "ebase"]
        if kind == "k3":
            names += ["causal_bf", "rope_cos", "rope_sin", "rot_f", "moba_valid", "moba_neg", "esel_bf"]
        K = load_consts(C, es0, names)
        npb = 8 if kind == "k3" else 8
        K["ptr"] = [C.ps(f"ptr{i}", [128, 4, 128], BF16) for i in range(2)]
        K["pb"] = [C.ps(f"pb{i}", [128, 512], F32) for i in range(6)]
        for nm, shp in (("ln_st", [128, 24]), ("ln_mv", [128, 2]), ("ln_sd", [128, 1]), ("eps", [128, 1]), ("one", [128, 1]), ("lnk", [128, 1])):
            K[nm] = C.sb(es0, "s_" + nm, shp, F32)
        C.memset("pool", K["eps"][:], EPS, ["c_eps"])
        C.memset("pool", K["one"][:], 1.0, ["c_one"])
        C.memset("pool", K["lnk"][:], float(-0.5 * np.log(128.0)), ["c_lnk"])
        di = lambda n, s, dt=F32: C.dram(n, s, dt, "ExternalInput")
        do = lambda n, s, dt=F32: C.dram(n, s, dt, "ExternalOutput")
        if kind in ("k1", "k3"):
            hT_d = C.dram("hT_d", [D, S], BF16, "Internal")
            x1_d = do("x1_o", [S, D])
            xg_d = do("xg_o", [NROWS, D], BF16)
            idx_d = do("idx_o", [S, 4], I32)
            gk_d = do("gk_o", [S, 4])
            mem_d = di("mem", [256, D])
            wkv_d = di("w_mem_kv", [D, 1024])
            wout_d = di("w_out", [D, D])
            ln1_d = di("ln1", [2, 128, D])
            wr_d = di("w_router", [D, NE])
            br_d = di("b_router", [128, NE])
        if kind == "k1":
            x_d = di("x", [S, D])
            w_in_d = di("w_in", [D, 5132])
            convw_d = di("convw", [128, 12, 4])
            convb_d = di("convb", [128, 12])
            gbias_d = di("gbias", [128, 12])
            with C.scope(es0) as es:
                xT = C.sb(es, "xT", [128, 16, S], BF16)
                xb = [C.sb(es, f"xb{i}", [128, D], BF16) for i in range(2)]
                for tt in range(NT):
                    C.dma("pool", xb[tt % 2][:], x_d[128 * tt:128 * tt + 128, :], [], [f"xb{tt%2}"])
                    emit_xT(C, K, xb[tt % 2], tt, xT, f"xb{tt%2}")
                with C.scope(es) as es2:
                    mem_attention(C, K, es2, xT, hT_d, mem_d, wkv_d, w_in_d, 4620)
                C.P.barrier()
                with C.scope(es) as es2:
                    mixer_mlstm(C, K, es2, xT, hT_d, w_in_d, convw_d, convb_d, gbias_d)
                C.P.barrier()
            with C.scope(es0) as es:
                post_mixer(C, K, es, hT_d, x_d, wout_d, ln1_d, wr_d, br_d, x1_d, xg_d, idx_d, gk_d)
            fin = list(C.P.dma_last.values())
        if kind in ("k3", "k5"):
            x1i_d = di("x1_i", [S, D])
            y_d = di("y_i", [NROWS, D])
            idxi_d = di("idx_i", [S, 4], I32)
            gki_d = di("gk_i", [S, 4])
            ln2_d = di("ln2", [2, 128, D])
        if kind == "k5":
            out_d = do("out", [S, D])
            with C.scope(es0) as es:
                combine_ln2(C, K, es, x1i_d, y_d, idxi_d, gki_d, ln2_d, out_d)
            fin = list(C.P.dma_last.values())
        if kind == "k3":
            x2_d = C.dram("x2_d", [S, D], F32, "Internal")
            w_in_d = di("w_in", [D, 5120])
            with C.scope(es0) as es:
                xT = C.sb(es, "xT", [128, 16, S], BF16)
                with C.scope(es) as es2:
                    combine_ln2(C, K, es2, x1i_d, y_d, idxi_d, gki_d, ln2_d, x2_d, xT=xT)
                C.P.barrier()
                with C.scope(es) as es2:
                    mem_attention(C, K, es2, xT, hT_d, mem_d, wkv_d, w_in_d, 4608)
                C.P.barrier()
                with C.scope(es) as es2:
                    mixer_moba(C, K, es2, xT, hT_d, w_in_d)
                C.P.barrier()
            with C.scope(es0) as es:
                post_mixer(C, K, es, hT_d, x2_d, wout_d, ln1_d, wr_d, br_d, x1_d, xg_d, idx_d, gk_d)
            fin = list(C.P.dma_last.values())
        C.P.emit(final_wait_ops=fin)
    return nc


def build_ffn():
    nc = bass.Bass("TRN2", target_bir_lowering=False)
    C = Ctx(nc)
    NSL = NCORES * CAP
    GS = 1024
    NG = NSL // GS
    with C.es:
        es = C.root
        K = load_consts(C, es, ["ident_bf"])
        K["ptr"] = [C.ps(f"ptr{i}", [128, 4, 128], BF16) for i in range(2)]
        K["pb"] = [C.ps(f"pb{i}", [128, 512], F32) for i in range(6)]
        pb = K["pb"]
        xg_d = C.dram("xg_i", [4 * NSL, D], BF16, "ExternalInput")
        wgu_d = C.dram("w_gu", [4, D, 2 * D], F32, "ExternalInput")
        bgu_d = C.dram("b_gu", [4, 128, 32], F32, "ExternalInput")
        wdn_d = C.dram("w_down", [4, D, D], F32, "ExternalInput")
        bdn_d = C.dram("b_down", [4, 128, D], F32, "ExternalInput")
        y_d = C.dram("y_o", [4 * NSL, D], F32, "ExternalOutput")
        wt = [C.sb(es, f"wt{i}", [128, 16, 512], BF16) for i in range(4)]
        xgt = [C.sb(es, f"xgt{i}", [128, D], BF16) for i in range(2)]
        xgT = C.sb(es, "xgT", [128, 16, GS], BF16)
        hidT = C.sb(es, "hidT", [128, 16, GS], BF16)
        bgu = C.sb(es, "bgu", [128, 32], F32)
        bdn = C.sb(es, "bdn", [128, D], F32)
        hgc = C.sb(es, "hgc", [128, 512], F32)
        sg = C.sb(es, "sg", [128, 512], F32)
        huc = C.sb(es, "huc", [128, 512], F32)
        yst = [C.sb(es, f"yst{i}", [128, D], F32) for i in range(2)]
        wslot = 0
        it = 0
        for e in range(4):
            wgv = wgu_d[e].rearrange("(kc p) c -> p kc c", p=128)
            wdv = wdn_d[e].rearrange("(kc p) c -> p kc c", p=128)
            C.dma("sp", bgu[:], bgu_d[e], [], ["bgu"])
            C.dma("sp", bdn[:], bdn_d[e], [], ["bdn"])
            for gi in range(NG):
                r0 = e * NSL + gi * GS
                for st in range(GS // 128):
                    xt = xgt[st % 2]
                    C.dma("sp", xt[:], xg_d[r0 + 128 * st:r0 + 128 * st + 128, :], [], [f"xgt{st%2}"])
                    for g in range(4):
                        pt = K["ptr"][g % 2]
                        for j in range(4):
                            kc = 4 * g + j
                            C.tr(pt[:, j, :], xt[:, 128 * kc:128 * kc + 128], K["ident_bf"][:], [f"xgt{st%2}", "k_ident_bf"], [f"ptr{g%2}"])
                        C.cp("dve" if g % 2 == 0 else "act", xgT[:, 4 * g:4 * g + 4, 128 * st:128 * st + 128], pt[:], [f"ptr{g%2}"], ["xgT"])
                for j in range(4):
                    wg_ = wt[wslot % 4]; gk_ = f"wt{wslot % 4}"; wslot += 1
                    wu_ = wt[wslot % 4]; uk_ = f"wt{wslot % 4}"; wslot += 1
                    C.dma("pool", wg_[:], wgv[:, :, 512 * j:512 * j + 512], [], [gk_])
                    C.dma("pool", wu_[:], wgv[:, :, D + 512 * j:D + 512 * j + 512], [], [uk_])
                    for i in range(4):
                        ffc = 4 * j + i
                        for n in range(GS // 512):
                            ns = slice(512 * n, 512 * n + 512)
                            for kc in range(16):
                                C.mm(pb[0][:], wg_[:, kc, 128 * i:128 * i + 128], xgT[:, kc, ns], kc == 0, kc == 15, [gk_, "xgT"], ["pb0"])
                            for kc in range(16):
                                C.mm(pb[1][:], wu_[:, kc, 128 * i:128 * i + 128], xgT[:, kc, ns], kc == 0, kc == 15, [uk_, "xgT"], ["pb1"])
                            C.ts("dve", hgc[:], pb[0][:], bgu[:, ffc:ffc + 1], 7.0, ALU.add, ALU.min, ["pb0", "bgu"], ["hgc"])
                            C.act(sg[:], hgc[:], AF.Sigmoid, ["hgc"], ["sg"], scale=1.702)
                            C.ts("dve", huc[:], pb[1][:], bgu[:, 16 + ffc:16 + ffc + 1], 7.0, ALU.add, ALU.min, ["pb1", "bgu"], ["huc"])
                            C.ts("dve", huc[:], huc[:], -7.0, 1.0, ALU.max, ALU.add, ["huc"], ["huc"])
                            C.tt("dve", sg[:], sg[:], hgc[:], ALU.mult, ["sg", "hgc"], ["sg"])
                            C.tt("dve", hidT[:, ffc, ns], sg[:], huc[:], ALU.mult, ["sg", "huc"], ["hidT"])
                wd = []
                for dg in range(4):
                    w_ = wt[wslot % 4]; k_ = f"wt{wslot % 4}"; wslot += 1
                    C.dma("pool", w_[:], wdv[:, :, 512 * dg:512 * dg + 512], [], [k_])
                    wd.append((w_, k_))
                for st in range(GS // 128):
                    ys = yst[it % 2]; yk_ = f"yst{it % 2}"; it += 1
                    for dg in range(4):
                        w_, k_ = wd[dg]
                        for kc in range(16):
                            C.mm(pb[2 + dg][:], hidT[:, kc, 128 * st:128 * st + 128], w_[:, kc, :], kc == 0, kc == 15, ["hidT", k_], [f"pb{2+dg}"])
                        C.tt("dve", ys[:, 512 * dg:512 * dg + 512], pb[2 + dg][:], bdn[:, 512 * dg:512 * dg + 512], ALU.add, [f"pb{2+dg}", "bdn"], [yk_])
                    C.dma("sp", y_d[r0 + 128 * st:r0 + 128 * st + 128, :], ys[:], [yk_], ["y_d"])
        C.P.emit(final_wait_ops=list(C.P.dma_last.values()))
    return nc


def ffn_local(C, K, es, xg_d, y_d, wgu_d, bgu_d, wdn_d, bdn_d):
    pb = K["pb"]
    NW = 6
    wt = [C.sb(es, f"wt{i}", [128, 16, 512], BF16) for i in range(NW)]
    xgt = [C.sb(es, f"xgt{i}", [128, D], BF16) for i in range(2)]
    xgT = [C.sb(es, f"xgT{i}", [128, 16, CAP], BF16) for i in range(2)]
    hidT = C.sb(es, "hidT", [128, 16, CAP], BF16)
    bgu = [C.sb(es, f"bgu{i}", [128, 32], F32) for i in range(2)]
    bdn = [C.sb(es, f"bdn{i}", [128, D], F32) for i in range(2)]
    hgc = C.sb(es, "hgc", [128, CAP], F32)
    sg = C.sb(es, "sg", [128, CAP], F32)
    huc = C.sb(es, "huc", [128, CAP], F32)
    yst = [C.sb(es, f"yst{i}", [128, D], F32) for i in range(2)]
    wslot = 0
    it = 0
    xi = 0
    for e in range(NE):
        wgv = wgu_d[e].rearrange("(kc p) c -> p kc c", p=128)
        wdv = wdn_d[e].rearrange("(kc p) c -> p kc c", p=128)
        bg = bgu[e % 2]; bgk = f"bgu{e % 2}"
        bd = bdn[e % 2]; bdk = f"bdn{e % 2}"
        C.dma("sp", bg[:], bgu_d[e], [], [bgk])
        C.dma("sp", bd[:], bdn_d[e].partition_broadcast(128), [], [bdk])
        xT_ = xgT[e % 2]; xk = f"xgT{e % 2}"
        r0 = e * CAP
        for st in range(CAP // 128):
            xt = xgt[xi % 2]; xtk = f"xgt{xi % 2}"; xi += 1
            C.dma("sp", xt[:], xg_d[r0 + 128 * st:r0 + 128 * st + 128, :], [], [xtk])
            for g in range(4):
                pt = K["ptr"][g % 2]
                for j in range(4):
                    kc = 4 * g + j
                    C.tr(pt[:, j, :], xt[:, 128 * kc:128 * kc + 128], K["ident_bf"][:], [xtk, "k_ident_bf"], [f"ptr{g%2}"])
                C.cp("dve" if g % 2 == 0 else "act", xT_[:, 4 * g:4 * g + 4, 128 * st:128 * st + 128], pt[:], [f"ptr{g%2}"], [xk])
        for j in range(4):
            wg_ = wt[wslot % NW]; gk_ = f"wt{wslot % NW}"; wslot += 1
            wu_ = wt[wslot % NW]; uk_ = f"wt{wslot % NW}"; wslot += 1
            C.dma("pool", wg_[:], wgv[:, :, 512 * j:512 * j + 512], [], [gk_])
            C.dma("pool", wu_[:], wgv[:, :, D + 512 * j:D + 512 * j + 512], [], [uk_])
            for i in range(4):
                ffc = 4 * j + i
                bA, bB = (0, 1) if ffc % 2 == 0 else (4, 5)
                for kc in range(16):
                    C.mm(pb[bA][:, 0:CAP], wg_[:, kc, 128 * i:128 * i + 128], xT_[:, kc, :], kc == 0, kc == 15, [gk_, xk], [f"pb{bA}"])
                for kc in range(16):
                    C.mm(pb[bB][:, 0:CAP], wu_[:, kc, 128 * i:128 * i + 128], xT_[:, kc, :], kc == 0, kc == 15, [uk_, xk], [f"pb{bB}"])
                C.ts("dve", hgc[:], pb[bA][:, 0:CAP], bg[:, ffc:ffc + 1], 7.0, ALU.add, ALU.min, [f"pb{bA}", bgk], ["hgc"])
                C.act(sg[:], hgc[:], AF.Sigmoid, ["hgc"], ["sg"], scale=1.702)
                C.ts("dve", huc[:], pb[bB][:, 0:CAP], bg[:, 16 + ffc:16 + ffc + 1], 7.0, ALU.add, ALU.min, [f"pb{bB}", bgk], ["huc"])
                C.ts("dve", huc[:], huc[:], -7.0, 1.0, ALU.max, ALU.add, ["huc"], ["huc"])
                C.tt("dve", sg[:], sg[:], hgc[:], ALU.mult, ["sg", "hgc"], ["sg"])
                C.tt("dve", hidT[:, ffc, :], sg[:], huc[:], ALU.mult, ["sg", "huc"], ["hidT"])
        wd = []
        for dg in range(4):
            w_ = wt[wslot % NW]; k_ = f"wt{wslot % NW}"; wslot += 1
            C.dma("pool", w_[:], wdv[:, :, 512 * dg:512 * dg + 512], [], [k_])
            wd.append((w_, k_))
        for st in range(CAP // 128):
            ys = yst[it % 2]; yk_ = f"yst{it % 2}"; it += 1
            for dg in range(4):
                w_, k_ = wd[dg]
                for kc in range(16):
                    C.mm(pb[2 + dg][:], hidT[:, kc, 128 * st:128 * st + 128], w_[:, kc, :], kc == 0, kc == 15, ["hidT", k_], [f"pb{2+dg}"])
                C.tt("dve", ys[:, 512 * dg:512 * dg + 512], pb[2 + dg][:], bd[:, 512 * dg:512 * dg + 512], ALU.add, [f"pb{2+dg}", bdk], [yk_])
            C.dma("sp", y_d[r0 + 128 * st:r0 + 128 * st + 128, :], ys[:], [yk_], ["y_d"])


def build_fused():
    nc = bass.Bass("TRN2", target_bir_lowering=False)
    C = Ctx(nc)
    with C.es:
        es0 = C.root
        names = ["ident_bf", "ident_f", "ones_bf", "ones_f", "tri_incl_f", "tri_strict_bf", "ebase",
                 "causal_bf", "rope_cos", "rope_sin", "rot_f", "moba_valid", "moba_neg", "esel_bf"]
        K = load_consts(C, es0, names)
        K["ptr"] = [C.ps(f"ptr{i}", [128, 4, 128], BF16) for i in range(2)]
        K["pb"] = [C.ps(f"pb{i}", [128, 512], F32) for i in range(6)]
        for nm, shp in (("ln_st", [128, 24]), ("ln_mv", [128, 2]), ("ln_sd", [128, 1]), ("eps", [128, 1]), ("one", [128, 1]), ("lnk", [128, 1])):
            K[nm] = C.sb(es0, "s_" + nm, shp, F32)
        C.memset("pool", K["eps"][:], EPS, ["c_eps"])
        C.memset("pool", K["one"][:], 1.0, ["c_one"])
        C.memset("pool", K["lnk"][:], float(-0.5 * np.log(128.0)), ["c_lnk"])
        di = lambda n, s, dt=F32: C.dram(n, s, dt, "ExternalInput")
        dn = lambda n, s, dt=F32: C.dram(n, s, dt, "Internal")
        x_d = di("x", [S, D]); mem_d = di("mem", [256, D])
        w_in0 = di("w_in0", [D, 5132]); w_in1 = di("w_in1", [D, 5120])
        convw_d = di("convw", [128, 12, 4]); convb_d = di("convb", [128, 12]); gbias_d = di("gbias", [128, 12])
        wkv_d = di("w_mem_kv", [2, D, 1024]); wout_d = di("w_out", [2, D, D])
        ln1_d = di("ln1", [2, 2, 128, D]); ln2_d = di("ln2", [2, 2, 128, D])
        wr_d = di("w_router", [2, D, NE]); br_d = di("b_router", [2, 128, NE])
        wgu_d = di("w_gu", [2, NE, D, 2 * D]); bgu_d = di("b_gu", [2, NE, 128, 32])
        wdn_d = di("w_down", [2, NE, D, D]); bdn_d = di("b_down", [2, NE, D])
        out_d = C.dram("out", [S, D], F32, "ExternalOutput")
        hT_d = dn("hT_d", [D, S], BF16)
        x1_d = dn("x1_d", [S, D]); x2_d = dn("x2_d", [S, D])
        xg_d = dn("xg_d", [NROWS, D], BF16); y_d = dn("y_d", [NROWS, D])
        idx_d = dn("idx_d", [S, 4], I32); gk_d = dn("gk_d", [S, 4])
        with C.scope(es0) as es:
            xT = C.sb(es, "xT", [128, 16, S], BF16)
            with C.scope(es) as es2:
                xb = [C.sb(es2, f"xb{i}", [128, D], BF16) for i in range(2)]
                for tt in range(NT):
                    C.dma("pool", xb[tt % 2][:], x_d[128 * tt:128 * tt + 128, :], [], [f"xb{tt%2}"])
                    emit_xT(C, K, xb[tt % 2], tt, xT, f"xb{tt%2}")
            C.P.barrier()
            with C.scope(es) as es2:
                mem_attention(C, K, es2, xT, hT_d, mem_d, wkv_d[0], w_in0, 4620)
            C.P.barrier()
            with C.scope(es) as es2:
                mixer_mlstm(C, K, es2, xT, hT_d, w_in0, convw_d, convb_d, gbias_d)
            C.P.barrier()
        with C.scope(es0) as es:
            post_mixer(C, K, es, hT_d, x_d, wout_d[0], ln1_d[0], wr_d[0], br_d[0], x1_d, xg_d, idx_d, gk_d)
        C.P.barrier()
        with C.scope(es0) as es:
            ffn_local(C, K, es, xg_d, y_d, wgu_d[0], bgu_d[0], wdn_d[0], bdn_d[0])
        C.P.barrier()
        with C.scope(es0) as es:
            xT = C.sb(es, "xT1", [128, 16, S], BF16)
            with C.scope(es) as es2:
                combine_ln2(C, K, es2, x1_d, y_d, idx_d, gk_d, ln2_d[0], x2_d, xT=xT)
            C.P.barrier()
            with C.scope(es) as es2:
                mem_attention(C, K, es2, xT, hT_d, mem_d, wkv_d[1], w_in1, 4608)
            C.P.barrier()
            with C.scope(es) as es2:
                mixer_moba(C, K, es2, xT, hT_d, w_in1)
            C.P.barrier()
        with C.scope(es0) as es:
            post_mixer(C, K, es, hT_d, x2_d, wout_d[1], ln1_d[1], wr_d[1], br_d[1], x1_d, xg_d, idx_d, gk_d)
        C.P.barrier()
        with C.scope(es0) as es:
            ffn_local(C, K, es, xg_d, y_d, wgu_d[1], bgu_d[1], wdn_d[1], bdn_d[1])
        C.P.barrier()
        with C.scope(es0) as es:
            outs = combine_ln2(C, K, es, x1_d, y_d, idx_d, gk_d, ln2_d[1], out_d)
        C.P.emit(final_wait_ops=list(C.P.dma_last.values()))
    return nc


_PROG_CACHE = {}


def _prog(kind):
    if kind not in _PROG_CACHE:
        _PROG_CACHE[kind] = build_ffn() if kind == "ffn" else (build_fused() if kind == "fused" else _build_dp(kind))
    return _PROG_CACHE[kind]


def _bc(v, n=128):
    return np.ascontiguousarray(np.broadcast_to(np.asarray(v, np.float32)[None], (n,) + tuple(np.shape(v))))


def _run(kind, in_maps):
    nc = _prog(kind)
    res = run_bass_kernel_spmd(nc, in_maps, core_ids=list(range(len(in_maps))))
    return res.results


def _fused_maps(cores, x, mem, mlstm_w_in, mlstm_conv_w, mlstm_conv_b, mlstm_b_igate, mlstm_b_fgate,
                moba_w_in, w_mem_kv, w_out, ln1_g, ln1_b, w_router, b_router, w_gu, b_gu,
                w_down, b_down, ln2_g, ln2_b):
    f32 = np.float32
    cst = host_consts()
    A = lambda a: np.ascontiguousarray(np.asarray(a, f32))
    shared = {
        "w_in0": A(mlstm_w_in[0]), "w_in1": A(moba_w_in[0]),
        "convw": np.ascontiguousarray(A(mlstm_conv_w[0]).reshape(4, 12, 128).transpose(2, 1, 0)),
        "convb": np.ascontiguousarray(A(mlstm_conv_b[0]).reshape(12, 128).T),
        "gbias": _bc(np.concatenate([A(mlstm_b_igate[0]), A(mlstm_b_fgate[0])])),
        "w_mem_kv": A(w_mem_kv), "w_out": A(w_out),
        "ln1": np.stack([np.stack([_bc(ln1_g[i]), _bc(ln1_b[i])], 0) for i in range(2)], 0),
        "ln2": np.stack([np.stack([_bc(ln2_g[i]), _bc(ln2_b[i])], 0) for i in range(2)], 0),
        "w_router": A(w_router), "b_router": np.stack([_bc(b_router[i]) for i in range(2)], 0),
        "w_gu": A(w_gu),
        "b_gu": np.ascontiguousarray(A(b_gu).reshape(2, NE, 32, 128).transpose(0, 1, 3, 2)),
        "w_down": A(w_down), "b_down": A(b_down),
    }
    for n in CONST_SPECS:
        shared["c_" + n] = cst[n]
    maps = []
    for c in cores:
        m = dict(shared)
        m["x"] = A(x[c]); m["mem"] = A(mem[c])
        maps.append(m)
    return maps


def kernel(**inputs):
    maps = _fused_maps(list(range(NCORES)), **inputs)
    nc = _prog("fused")
    res = run_bass_kernel_spmd(nc, maps, core_ids=list(range(NCORES)))
    return np.stack([r["out"] for r in res.results], 0).astype(np.float32)


def kernel_unfused(x, mem, mlstm_w_in, mlstm_conv_w, mlstm_conv_b, mlstm_b_igate, mlstm_b_fgate,
           moba_w_in, w_mem_kv, w_out, ln1_g, ln1_b, w_router, b_router, w_gu, b_gu,
           w_down, b_down, ln2_g, ln2_b):
    f32 = np.float32
    cst = host_consts()

    def consts_for(names):
        return {"c_" + n: cst[n] for n in names}

    base = ["ident_bf", "ident_f", "ones_bf", "ones_f", "tri_incl_f", "tri_strict_bf", "ebase"]
    k3n = base + ["causal_bf", "rope_cos", "rope_sin", "rot_f", "moba_valid", "moba_neg", "esel_bf"]

    def lnp(g, b):
        return np.stack([_bc(g), _bc(b)], 0)

    def dp_common(i):
        return {"w_mem_kv": np.asarray(w_mem_kv[i], f32), "w_out": np.asarray(w_out[i], f32),
                "ln1": lnp(ln1_g[i], ln1_b[i]), "w_router": np.asarray(w_router[i], f32),
                "b_router": _bc(b_router[i])}

    def ffn_maps(i, xg_all):
        xs = np.stack([a.reshape(NE, CAP, D) for a in xg_all], 0)
        maps = []
        for c in range(NCORES):
            sl = slice(4 * c, 4 * c + 4)
            xi = np.ascontiguousarray(xs[:, sl].transpose(1, 0, 2, 3)).reshape(4 * NCORES * CAP, D)
            maps.append({
                "c_ident_bf": cst["ident_bf"], "xg_i": xi,
                "w_gu": np.asarray(w_gu[i, sl], f32),
                "b_gu": np.ascontiguousarray(np.asarray(b_gu[i, sl], f32).reshape(4, 32, 128).transpose(0, 2, 1)),
                "w_down": np.asarray(w_down[i, sl], f32),
                "b_down": np.stack([_bc(b_down[i, e]) for e in range(4 * c, 4 * c + 4)], 0),
            })
        return maps

    def y_back(res):
        ys = np.stack([r["y_o"].reshape(4, NCORES, CAP, D) for r in res], 0)
        out = []
        for s_ in range(NCORES):
            out.append(np.ascontiguousarray(ys[:, :, s_]).reshape(NROWS, D))
        return out

    convw = np.ascontiguousarray(np.asarray(mlstm_conv_w[0], f32).reshape(4, 12, 128).transpose(2, 1, 0))
    convb = np.ascontiguousarray(np.asarray(mlstm_conv_b[0], f32).reshape(12, 128).T)
    gbias = _bc(np.concatenate([np.asarray(mlstm_b_igate[0], f32), np.asarray(mlstm_b_fgate[0], f32)]))
    maps = []
    for c in range(NCORES):
        m = {"x": np.asarray(x[c], f32), "mem": np.asarray(mem[c], f32), "w_in": np.asarray(mlstm_w_in[0], f32),
             "convw": convw, "convb": convb, "gbias": gbias}
        m.update(dp_common(0)); m.update(consts_for(base))
        maps.append(m)
    r1 = _run("k1", maps)
    r2 = _run("ffn", ffn_maps(0, [r["xg_o"] for r in r1]))
    yb = y_back(r2)
    maps = []
    for c in range(NCORES):
        m = {"x1_i": r1[c]["x1_o"], "y_i": yb[c], "idx_i": r1[c]["idx_o"], "gk_i": r1[c]["gk_o"],
             "ln2": lnp(ln2_g[0], ln2_b[0]), "mem": np.asarray(mem[c], f32), "w_in": np.asarray(moba_w_in[0], f32)}
        m.update(dp_common(1)); m.update(consts_for(k3n))
        maps.append(m)
    r3 = _run("k3", maps)
    r4 = _run("ffn", ffn_maps(1, [r["xg_o"] for r in r3]))
    yb = y_back(r4)
    maps = []
    for c in range(NCORES):
        m = {"x1_i": r3[c]["x1_o"], "y_i": yb[c], "idx_i": r3[c]["idx_o"], "gk_i": r3[c]["gk_o"],
             "ln2": lnp(ln2_g[1], ln2_b[1])}
        m.update(consts_for(base))
        maps.append(m)
    r5 = _run("k5", maps)
    return np.stack([r["out"] for r in r5], 0).astype(np.float32)
```

```python
import contextlib
import numpy as np
import ml_dtypes
import concourse.bass as bass
import concourse.mybir as mybir
from concourse.bass_utils import run_bass_kernel_spmd

F32 = mybir.dt.float32
BF16 = mybir.dt.bfloat16
I32 = mybir.dt.int32
ALU = mybir.AluOpType
AF = mybir.ActivationFunctionType
AX = mybir.AxisListType
NPBF = ml_dtypes.bfloat16

S = 2048
D = 2048
NT = 16
NE = 32
CAP = 352
SLOT_TILES = [(0, 128), (128, 128), (256, 96)]
NROWS = NE * CAP
ALPHA = 4 ** 0.25
EPS = 1e-5
BIGM = 30000.0
NCORES = 8


class Prog:
    ENGS = ("pe", "act", "dve", "pool", "sp")

    def __init__(self, nc, n_dma_sems=10):
        self.nc = nc
        self.ops = []
        self.eng_ops = {e: [] for e in self.ENGS}
        self.state = {}
        self.n_dma_sems = n_dma_sems
        self.dma_rr = {e: 0 for e in self.ENGS}
        self.dma_last = {}
        self.last_on = {}

    def op(self, eng, fn, reads=(), writes=(), dma=False, extra=()):
        oid = len(self.ops)
        deps = set()
        for k in reads:
            st = self.state.get(k)
            if st is not None:
                for d in st[0].values():
                    deps.add((d, "raw"))
        for k in writes:
            st = self.state.get(k)
            if st is not None:
                for d in st[0].values():
                    deps.add((d, "waw"))
                for d in st[1].values():
                    deps.add((d, "war"))
        o = dict(id=oid, eng=eng, fn=fn, dma=dma, deps=set(extra), needed=False, slot=None)
        for (d, kind) in deps:
            do = self.ops[d]
            if not do["dma"] and not dma and do["eng"] == eng:
                if eng == "pe" or kind != "raw":
                    continue
            o["deps"].add(d)
        ek = (eng, None)
        if dma:
            slot = self.dma_rr[eng] % self.n_dma_sems
            self.dma_rr[eng] += 1
            o["slot"] = slot
            ek = (eng, slot)
            prev = self.dma_last.get((eng, slot))
            if prev is not None:
                o["deps"].add(prev)
            self.dma_last[(eng, slot)] = oid
        for d in o["deps"]:
            self.ops[d]["needed"] = True
        self.ops.append(o)
        self.eng_ops[eng].append(oid)
        if fn is not None and not dma:
            self.last_on[eng] = oid
        for k in reads:
            st = self.state.setdefault(k, [{}, {}])
            st[1][ek] = oid
        for k in writes:
            st = self.state.setdefault(k, [{}, {}])
            st[0][ek] = oid
        return oid

    def barrier(self):
        deps = set(self.last_on.values()) | set(self.dma_last.values())
        for e in self.ENGS:
            self.op(e, None, extra=[d for d in deps])
        self.state = {}

    def emit(self, final_wait_ops=()):
        nc = self.nc
        with contextlib.ExitStack() as es:
            sem_eng = {e: es.enter_context(nc.semaphore("s_" + e)) for e in self.ENGS}
            sem_dma = {}
            for e in ("sp", "act", "pool"):
                for s in range(min(self.n_dma_sems, self.dma_rr[e])):
                    sem_dma[(e, s)] = es.enter_context(nc.semaphore(f"d_{e}{s}"))
            cnt = {e: 0 for e in self.ENGS}
            dcnt = {}
            for o in self.ops:
                if o["dma"]:
                    key = (o["eng"], o["slot"])
                    dcnt[key] = dcnt.get(key, 0) + 16
                    o["tok"] = (sem_dma[key], dcnt[key], key)
                elif o["needed"]:
                    assert o["fn"] is not None
                    cnt[o["eng"]] += 1
                    o["tok"] = (sem_eng[o["eng"]], cnt[o["eng"]], o["eng"])
                else:
                    o["tok"] = None
            final = [self.ops[i]["tok"] for i in final_wait_ops]
            block = es.enter_context(nc.Block())
            regs = {"pe": block.tensor, "act": block.scalar, "dve": block.vector,
                    "pool": block.gpsimd, "sp": block.sync}
            for e in self.ENGS:
                def body(engine, e=e):
                    waited = {}
                    if e == "pool":
                        self.bc_reg = engine.to_reg(NROWS - 1)
                    for oid in self.eng_ops[e]:
                        o = self.ops[oid]
                        need = {}
                        for d in o["deps"]:
                            sem, val, key = self.ops[d]["tok"]
                            if waited.get(key, 0) >= val:
                                continue
                            if need.get(key, (None, 0))[1] < val:
                                need[key] = (sem, val)
                        for key, (sem, val) in need.items():
                            engine.wait_ge(sem, val)
                            waited[key] = val
                        if o["fn"] is None:
                            continue
                        ins = o["fn"](engine)
                        if o["tok"] is not None:
                            ins.then_inc(o["tok"][0], 16 if o["dma"] else 1)
                    if e == "sp":
                        for (sem, val, key) in final:
                            if waited.get(key, 0) < val:
                                engine.wait_ge(sem, val)
                                waited[key] = val
                regs[e](body)


SB_BASE = 16512
SB_LIMIT = 229376


class Scope:
    def __init__(self, top):
        self.top = top


class Ctx:
    def __init__(self, nc):
        self.nc = nc
        self.P = Prog(nc)
        self.es = contextlib.ExitStack()
        self.root = Scope(SB_BASE)
        self.uid = 0
        self.outs = []

    def dram(self, name, shape, dt, kind):
        return self.nc.dram_tensor(name, list(shape), dt, kind=kind).ap()

    def sb(self, sc, name, shape, dt):
        n = 1
        for d in shape[1:]:
            n *= d
        size = n * (4 if dt in (F32, I32) else 2)
        size = (size + 31) // 32 * 32
        off = sc.top
        sc.top += size
        assert sc.top <= SB_LIMIT, (name, sc.top)
        return self.nc.alloc_sbuf_tensor_at(name, list(shape), dt, offset=off)

    @contextlib.contextmanager
    def scope(self, parent):
        yield Scope(parent.top)

    def ps(self, name, shape, dt):
        return self.es.enter_context(self.nc.psum_tensor(name, list(shape), dt))

    def dma(self, q, out, in_, r, w):
        return self.P.op(q, lambda e: e.dma_start(out=out, in_=in_), reads=r, writes=w, dma=True)

    def mm(self, out, lhsT, rhs, start, stop, r, w):
        return self.P.op("pe", lambda e: e.matmul(out, lhsT=lhsT, rhs=rhs, start=start, stop=stop), reads=r, writes=w)

    def tr(self, out, in_, ident, r, w):
        return self.P.op("pe", lambda e: e.transpose(out, in_, ident), reads=r, writes=w)

    def act(self, out, in_, func, r, w, bias=0.0, scale=1.0, accum_out=None):
        if accum_out is None:
            f = lambda e: e.activation(out=out, in_=in_, func=func, bias=bias, scale=scale)
        else:
            f = lambda e: e.activation(out=out, in_=in_, func=func, bias=bias, scale=scale, accum_out=accum_out)
        return self.P.op("act", f, reads=r, writes=w)

    def ts(self, eng, out, in0, s1, s2, op0, op1, r, w):
        if op1 is None:
            f = lambda e: e.tensor_scalar(out=out, in0=in0, scalar1=s1, scalar2=None, op0=op0)
        else:
            f = lambda e: e.tensor_scalar(out=out, in0=in0, scalar1=s1, scalar2=s2, op0=op0, op1=op1)
        return self.P.op(eng, f, reads=r, writes=w)

    def tt(self, eng, out, in0, in1, op, r, w):
        return self.P.op(eng, lambda e: e.tensor_tensor(out=out, in0=in0, in1=in1, op=op), reads=r, writes=w)

    def stt(self, out, in0, scalar, in1, op0, op1, r, w, accum_out=None):
        if accum_out is None:
            f = lambda e: e.scalar_tensor_tensor(out=out, in0=in0, scalar=scalar, in1=in1, op0=op0, op1=op1)
        else:
            f = lambda e: e.scalar_tensor_tensor(out=out, in0=in0, scalar=scalar, in1=in1, op0=op0, op1=op1, accum_out=accum_out)
        return self.P.op("dve", f, reads=r, writes=w)

    def cp(self, eng, out, in_, r, w):
        if eng == "act":
            return self.P.op("act", lambda e: e.activation(out=out, in_=in_, func=AF.Copy), reads=r, writes=w)
        return self.P.op(eng, lambda e: e.tensor_copy(out=out, in_=in_), reads=r, writes=w)

    def memset(self, eng, ap, val, w):
        return self.P.op(eng, lambda e: e.memset(ap, val), writes=w)

    def recip(self, out, in_, r, w):
        return self.P.op("dve", lambda e: e.reciprocal(out=out, in_=in_), reads=r, writes=w)


def host_consts():
    c = {}
    c["ident_bf"] = np.eye(128, dtype=np.float32).astype(NPBF)
    c["ident_f"] = np.eye(128, dtype=np.float32)
    j = np.arange(128)[:, None]
    l = np.arange(128)[None, :]
    c["tri_incl_f"] = (j <= l).astype(np.float32)
    c["tri_strict_bf"] = (j < l).astype(np.float32).astype(NPBF)
    c["ones_f"] = np.ones((128, 128), np.float32)
    c["ones_bf"] = np.ones((128, 128), np.float32).astype(NPBF)
    q = np.arange(512)[None, None, :]
    r = np.arange(4)[None, :, None]
    jj = np.arange(128)[:, None, None]
    c["causal_bf"] = ((128 * r + jj) <= q).astype(np.float32).astype(NPBF)
    half = 16
    inv = 500000.0 ** (-np.arange(half, dtype=np.float32) * 2.0 / 32.0)
    ang = np.arange(S, dtype=np.float32)[None, :] * inv[:, None]
    cos = np.cos(ang).astype(np.float32)
    sin = np.sin(ang).astype(np.float32)
    c["rope_cos"] = np.concatenate([cos, cos], 0)
    c["rope_sin"] = np.concatenate([-sin, sin], 0)
    R = np.zeros((128, 128), np.float32)
    for i in range(16):
        R[i + 16, i] = 1.0
        R[i, i + 16] = 1.0
    c["rot_f"] = R
    vm = np.zeros((128, 16, 8), np.float32)
    for tt in range(16):
        vm[:, tt, : tt // 2] = 1.0
    c["moba_valid"] = vm
    c["moba_neg"] = ((1.0 - vm) * -1e30).astype(np.float32)
    es = np.zeros((128, 8, 128), np.float32)
    for n in range(8):
        es[n, n, :] = 1.0
    c["esel_bf"] = es.astype(NPBF)
    c["ebase"] = np.broadcast_to((np.arange(NE, dtype=np.float32) * CAP)[None, :], (128, NE)).copy()
    return c


CONST_SPECS = {
    "ident_bf": ([128, 128], BF16), "ident_f": ([128, 128], F32), "tri_incl_f": ([128, 128], F32),
    "tri_strict_bf": ([128, 128], BF16), "ones_f": ([128, 128], F32), "ones_bf": ([128, 128], BF16),
    "causal_bf": ([128, 4, 512], BF16), "rope_cos": ([32, S], F32), "rope_sin": ([32, S], F32),
    "rot_f": ([128, 128], F32), "moba_valid": ([128, 16, 8], F32), "moba_neg": ([128, 16, 8], F32),
    "esel_bf": ([128, 8, 128], BF16), "ebase": ([128, NE], F32),
}


def load_consts(C, es, names):
    t = {}
    for n in names:
        shape, dt = CONST_SPECS[n]
        d = C.dram("c_" + n, shape, dt, "ExternalInput")
        s = C.sb(es, "k_" + n, shape, dt)
        C.dma("sp", s[:], d, [], ["k_" + n])
        t[n] = s
    return t


def layer_norm_tile(C, K, r, gbc, bbc, out, tagr, tagw):
    st, mv, sd = K["ln_st"], K["ln_mv"], K["ln_sd"]
    for i in range(4):
        C.P.op("dve", lambda e, i=i: e.bn_stats(out=st[:, 6 * i:6 * i + 6], in_=r[:, 512 * i:512 * i + 512]),
               reads=[tagr], writes=["ln_st"])
    C.P.op("dve", lambda e: e.bn_aggr(out=mv[:], in_=st[:]), reads=["ln_st"], writes=["ln_mv"])
    C.act(sd[:], mv[:, 1:2], AF.Sqrt, ["ln_mv", "c_eps"], ["ln_sd"], bias=K["eps"][:], scale=1.0)
    C.recip(sd[:], sd[:], ["ln_sd"], ["ln_sd"])
    C.ts("dve", out, r, mv[:, 0:1], sd[:, 0:1], ALU.subtract, ALU.mult, [tagr, "ln_mv", "ln_sd"], [tagw])
    C.tt("pool", out, out, gbc, ALU.mult, [tagw, "lnp"], [tagw])
    C.tt("pool", out, out, bbc, ALU.add, [tagw, "lnp"], [tagw])


def emit_xT(C, K, src_bf, tt, xT, tag_src):
    for g in range(4):
        pt = K["ptr"][g % 2]
        for j in range(4):
            kc = 4 * g + j
            C.tr(pt[:, j, :], src_bf[:, 128 * kc:128 * kc + 128], K["ident_bf"][:], [tag_src, "k_ident_bf"], [f"ptr{g%2}"])
        C.cp("dve" if g % 2 == 0 else "act", xT[:, 4 * g:4 * g + 4, 128 * tt:128 * tt + 128], pt[:], [f"ptr{g%2}"], ["xT"])


def mem_attention(C, K, es, xT, hT_d, memd, wkv_d, w_in_d, qm_col0):
    P = C.P
    pb = K["pb"]
    wq = C.sb(es, "ma_wq", [128, 16, 512], BF16)
    wkv_v = wkv_d.rearrange("(kc p) c -> p kc c", p=128)
    w_in_v = w_in_d.rearrange("(kc p) c -> p kc c", p=128)
    C.dma("pool", wq[:], w_in_v[:, :, qm_col0:qm_col0 + 512], [], ["ma_wq"])
    wkv = C.sb(es, "ma_wkv", [128, 16, 1024], BF16)
    C.dma("pool", wkv[:, :, 0:512], wkv_v[:, :, 0:512], [], ["ma_wkv0"])
    C.dma("pool", wkv[:, :, 512:1024], wkv_v[:, :, 512:1024], [], ["ma_wkv1"])
    mem_bf = C.sb(es, "ma_mem", [128, 2, 2048], BF16)
    memT = C.sb(es, "ma_memT", [128, 16, 256], BF16)
    for mt in range(2):
        C.dma("pool", mem_bf[:, mt, :], memd[128 * mt:128 * mt + 128, :], [], [f"ma_mem{mt}"])
        for g in range(4):
            pt = K["ptr"][g % 2]
            for j in range(4):
                kc = 4 * g + j
                C.tr(pt[:, j, :], mem_bf[:, mt, 128 * kc:128 * kc + 128], K["ident_bf"][:], [f"ma_mem{mt}", "k_ident_bf"], [f"ptr{g%2}"])
            C.cp("dve", memT[:, 4 * g:4 * g + 4, 128 * mt:128 * mt + 128], pt[:], [f"ptr{g%2}"], ["ma_memT"])
    kmT = C.sb(es, "ma_kmT", [128, 4, 256], BF16)
    vm = C.sb(es, "ma_vm", [128, 2, 512], BF16)
    qmT = C.sb(es, "ma_qmT", [128, 4, 2048], BF16)
    for h in range(4):
        for kc in range(16):
            C.mm(pb[0][:, 0:256], wkv[:, kc, 128 * h:128 * h + 128], memT[:, kc, :], kc == 0, kc == 15, ["ma_wkv0", "ma_memT"], ["pb0"])
        C.cp("act", kmT[:, h, :], pb[0][:, 0:256], ["pb0"], ["ma_kmT"])
    for mt in range(2):
        for kc in range(16):
            C.mm(pb[1][:], memT[:, kc, 128 * mt:128 * mt + 128], wkv[:, kc, 512:1024], kc == 0, kc == 15, ["ma_wkv1", "ma_memT"], ["pb1"])
        C.cp("act", vm[:, mt, :], pb[1][:], ["pb1"], ["ma_vm"])
    for h in range(4):
        for tg in range(4):
            b = pb[(h * 4 + tg) % 2]
            bk = f"pb{(h * 4 + tg) % 2}"
            for kc in range(16):
                C.mm(b[:], wq[:, kc, 128 * h:128 * h + 128], xT[:, kc, 512 * tg:512 * tg + 512], kc == 0, kc == 15, ["ma_wq", "xT"], [bk])
            C.cp("dve" if tg % 2 else "act", qmT[:, h, 512 * tg:512 * tg + 512], b[:], [bk], ["ma_qmT"])
    eT = C.sb(es, "ma_eT", [128, 2, 512], BF16)
    rdn = C.sb(es, "ma_rdn", [128, 512], F32)
    hst = [C.sb(es, f"ma_hst{i}", [128, 512], BF16) for i in range(2)]
    scale = 128 ** -0.5
    it = 0
    for h in range(4):
        for tg in range(4):
            for mt in range(2):
                C.mm(pb[2 + mt][:], kmT[:, h, 128 * mt:128 * mt + 128], qmT[:, h, 512 * tg:512 * tg + 512], True, True, ["ma_kmT", "ma_qmT"], [f"pb{2+mt}"])
                C.act(eT[:, mt, :], pb[2 + mt][:], AF.Exp, [f"pb{2+mt}"], [f"ma_eT{mt}"], scale=scale)
            for mt in range(2):
                C.mm(pb[4][:], vm[:, mt, 128 * h:128 * h + 128], eT[:, mt, :], mt == 0, mt == 1, ["ma_vm", f"ma_eT{mt}"], ["pb4"])
            for mt in range(2):
                C.mm(pb[5][:], K["ones_bf"][:], eT[:, mt, :], mt == 0, mt == 1, ["k_ones_bf", f"ma_eT{mt}"], ["pb5"])
            C.recip(rdn[:], pb[5][:], ["pb5"], ["ma_rdn"])
            hs = hst[it % 2]
            C.tt("dve", hs[:], pb[4][:], rdn[:], ALU.mult, ["pb4", "ma_rdn"], [f"ma_hst{it%2}"])
            C.dma("sp", hT_d[1536 + 128 * h:1536 + 128 * h + 128, 512 * tg:512 * tg + 512], hs[:], [f"ma_hst{it%2}"], ["hT_d"])
            it += 1


def _interleave(ga, gb):
    da = db = False
    while not (da and db):
        if not da:
            try:
                next(ga)
            except StopIteration:
                da = True
        if not db:
            try:
                next(gb)
            except StopIteration:
                db = True


def _empty():
    return
    yield


def mixer_mlstm(C, K, es, xT, hT_d, w_in_d, convw_d, convb_d, gbias_d):
    pb = K["pb"]
    w_in_v = w_in_d.rearrange("(kc p) c -> p kc c", p=128)
    cw = C.sb(es, "ml_cw", [128, 12, 4], F32)
    cb = C.sb(es, "ml_cb", [128, 12], F32)
    gb = C.sb(es, "ml_gb", [128, 12], F32)
    C.dma("sp", cw[:], convw_d, [], ["ml_cw"])
    C.dma("sp", cb[:], convb_d, [], ["ml_cb"])
    C.dma("sp", gb[:], gbias_d, [], ["ml_gb"])
    wg = C.sb(es, "ml_wg", [128, 16, 12], BF16)
    C.dma("pool", wg[:], w_in_v[:, :, 4608:4620], [], ["ml_wg"])
    gts = C.sb(es, "ml_gts", [128, 16, 12], F32)
    for tt in range(NT):
        for kc in range(16):
            C.mm(pb[0][:, 12 * tt:12 * tt + 12], xT[:, kc, 128 * tt:128 * tt + 128], wg[:, kc, :], kc == 0, kc == 15, ["xT", "ml_wg"], ["pb0"])
    for tt in range(NT):
        C.tt("dve", gts[:, tt, :], pb[0][:, 12 * tt:12 * tt + 12], gb[:], ALU.add, ["pb0", "ml_gb"], ["ml_gts"])
    lf = C.sb(es, "ml_lf", [128, 16, 6], F32)
    C.act(lf[:], gts[:, :, 6:12], AF.Exp, ["ml_gts"], ["ml_lf"], scale=-1.0)
    C.act(lf[:], lf[:], AF.Ln, ["ml_lf", "c_one"], ["ml_lf"], bias=K["one"][:], scale=1.0)
    lf2 = lf[:].rearrange("p a b -> p (a b)")
    C.mm(pb[1][:, 0:96], K["tri_incl_f"][:], lf2, True, True, ["k_tri_incl_f", "ml_lf"], ["pb1"])
    C.mm(pb[1][:, 128:224], K["ones_f"][:], lf2, True, True, ["k_ones_f", "ml_lf"], ["pb1"])
    ksc = C.sb(es, "ml_ksc", [128, 96], F32)
    qsc = C.sb(es, "ml_qsc", [128, 96], F32)
    egd = C.sb(es, "ml_eg", [128, 96], F32)
    tmp96 = C.sb(es, "ml_t96", [128, 16, 6], F32)
    C.tt("dve", tmp96[:], gts[:, :, 0:6], pb[1][:, 0:96].rearrange("p (a b) -> p a b", b=6), ALU.add, ["ml_gts", "pb1"], ["ml_t96"])
    C.act(ksc[:], tmp96[:].rearrange("p a b -> p (a b)"), AF.Exp, ["ml_t96", "c_lnk"], ["ml_ksc"], bias=K["lnk"][:], scale=1.0)
    C.act(qsc[:], pb[1][:, 0:96], AF.Exp, ["pb1"], ["ml_qsc"], scale=-1.0)
    C.act(egd[:], pb[1][:, 128:224], AF.Exp, ["pb1"], ["ml_eg"], scale=-1.0)
    wq = C.sb(es, "ml_wq", [128, 16, 128], BF16)
    wk = C.sb(es, "ml_wk", [128, 16, 128], BF16)
    wv = C.sb(es, "ml_wv", [128, 16, 256], BF16)
    wo = C.sb(es, "ml_wo", [128, 16, 256], BF16)
    uq = C.sb(es, "ml_uq", [128, 3 + S], F32)
    cq = C.sb(es, "ml_cq", [128, S], F32)
    qTs = [C.sb(es, f"ml_qT{i}", [128, S], BF16) for i in range(2)]
    kTs = [C.sb(es, f"ml_kT{i}", [128, S], BF16) for i in range(2)]
    ktss = [C.sb(es, f"ml_kts{i}", [128, 16, 128], BF16) for i in range(2)]
    vxs = [C.sb(es, f"ml_vx{i}", [128, 16, 264], BF16) for i in range(2)]
    ogs = [C.sb(es, f"ml_og{i}", [128, 16, 256], BF16) for i in range(2)]
    Cf = C.sb(es, "ml_Cf", [128, 264], F32)
    Cb = C.sb(es, "ml_Cb", [128, 264], BF16)
    stm = C.sb(es, "ml_stm", [128, 128], BF16)
    hm = C.sb(es, "ml_hm", [128, 256], BF16)
    sm = C.sb(es, "ml_sm", [128, 4], F32)
    hst = C.sb(es, "ml_hst", [128, 2, S], BF16)
    mask = K["tri_incl_f"]
    C.memset("pool", uq[:, 0:3], 0.0, ["ml_uq"])
    NH = 6

    def prep(h):
        p = h % 2
        qT, kT, kts, vx, og = qTs[p], kTs[p], ktss[p], vxs[p], ogs[p]
        C.dma("pool", wq[:], w_in_v[:, :, 128 * h:128 * h + 128], [], ["ml_wq"])
        C.dma("pool", wk[:], w_in_v[:, :, 768 + 128 * h:768 + 128 * h + 128], [], ["ml_wk"])
        C.dma("pool", wv[:], w_in_v[:, :, 1536 + 256 * h:1536 + 256 * h + 256], [], ["ml_wv"])
        C.dma("pool", wo[:], w_in_v[:, :, 3072 + 256 * h:3072 + 256 * h + 256], [], ["ml_wo"])
        for (w_, wtag, cidx, dst, dtag) in ((wq, "ml_wq", h, qT, f"ml_qT{p}"), (wk, "ml_wk", 6 + h, kT, f"ml_kT{p}")):
            for tg in range(4):
                b = pb[2 + tg % 2]
                bk = f"pb{2 + tg % 2}"
                for kc in range(16):
                    C.mm(b[:], w_[:, kc, :], xT[:, kc, 512 * tg:512 * tg + 512], kc == 0, kc == 15, [wtag, "xT"], [bk])
                C.cp("act", uq[:, 3 + 512 * tg:3 + 512 * tg + 512], b[:], [bk], ["ml_uq"])
                yield
            C.ts("dve", cq[:], uq[:, 0:S], cw[:, cidx, 0:1], cb[:, cidx:cidx + 1], ALU.mult, ALU.add, ["ml_uq", "ml_cw", "ml_cb"], ["ml_cq"])
            for w in range(1, 4):
                C.stt(cq[:], uq[:, w:w + S], cw[:, cidx, w:w + 1], cq[:], ALU.mult, ALU.add, ["ml_uq", "ml_cw", "ml_cq"], ["ml_cq"])
            C.act(dst[:], cq[:], AF.Silu, ["ml_cq"], [dtag])
            yield
        for tt in range(NT):
            b = pb[2 + tt % 2]
            bk = f"pb{2 + tt % 2}"
            for kc in range(16):
                C.mm(b[:, 0:256], xT[:, kc, 128 * tt:128 * tt + 128], wv[:, kc, :], kc == 0, kc == 15, ["xT", "ml_wv"], [bk])
            C.cp("dve", vx[:, tt, 0:256], b[:, 0:256], [bk], [f"ml_vx{p}"])
            yield
        C.memset("pool", vx[:, :, 256:257], 1.0, [f"ml_vx{p}"])
        for tt in range(NT):
            b = pb[2 + tt % 2]
            bk = f"pb{2 + tt % 2}"
            for kc in range(16):
                C.mm(b[:, 0:256], xT[:, kc, 128 * tt:128 * tt + 128], wo[:, kc, :], kc == 0, kc == 15, ["xT", "ml_wo"], [bk])
            C.act(og[:, tt, :], b[:, 0:256], AF.Sigmoid, [bk], [f"ml_og{p}"])
            yield
        for tt in range(NT):
            pt = K["ptr"][0]
            C.tr(pt[:, tt % 4, :], kT[:, 128 * tt:128 * tt + 128], K["ident_bf"][:], [f"ml_kT{p}", "k_ident_bf"], ["ptr0"])
            C.ts("dve", kts[:, tt, :], pt[:, tt % 4, :], ksc[:, 6 * tt + h:6 * tt + h + 1], None, ALU.mult, None, ["ptr0", "ml_ksc"], [f"ml_kts{p}"])
            if tt % 4 == 3:
                yield

    def chunks(h):
        p = h % 2
        qT, kT, kts, vx, og = qTs[p], kTs[p], ktss[p], vxs[p], ogs[p]
        C.memset("pool", Cf[:], 0.0, ["ml_Cf"])
        C.memset("pool", Cb[:], 0.0, ["ml_Cb"])
        for c in range(NT):
            sl = slice(128 * c, 128 * c + 128)
            i6 = 6 * c + h
            C.mm(pb[4][:, 0:128], kT[:, sl], qT[:, sl], True, True, [f"ml_kT{p}", f"ml_qT{p}"], ["pb4"])
            C.stt(stm[:], pb[4][:, 0:128], ksc[:, i6:i6 + 1], mask[:], ALU.mult, ALU.mult, ["pb4", "ml_ksc", "k_tri_incl_f"], ["ml_stm"])
            C.mm(pb[0][:, 0:257], kts[:, c, :], vx[:, c, 0:257], True, True, [f"ml_kts{p}", f"ml_vx{p}"], ["pb0"])
            C.mm(pb[5][:, 0:257], qT[:, sl], Cb[:, 0:257], True, False, [f"ml_qT{p}", "ml_Cb"], ["pb5"])
            C.mm(pb[5][:, 0:257], stm[:], vx[:, c, 0:257], False, True, ["ml_stm", f"ml_vx{p}"], ["pb5"])
            yield
            C.act(sm[:, 3:4], pb[5][:, 256:257], AF.Abs, ["pb5", "ml_qsc"], ["ml_sm3"], scale=qsc[:, i6:i6 + 1])
            C.ts("dve", sm[:, 0:1], sm[:, 3:4], 1.0, None, ALU.max, None, ["ml_sm3"], ["ml_sm0"])
            C.recip(sm[:, 1:2], sm[:, 0:1], ["ml_sm0"], ["ml_sm1"])
            C.tt("dve", sm[:, 2:3], sm[:, 1:2], qsc[:, i6:i6 + 1], ALU.mult, ["ml_sm1", "ml_qsc"], ["ml_sm2"])
            C.stt(hm[:], pb[5][:, 0:256], sm[:, 2:3], og[:, c, :], ALU.mult, ALU.mult, ["pb5", "ml_sm2", f"ml_og{p}"], ["ml_hm"])
            C.tt("dve", Cf[:, 0:257], pb[0][:, 0:257], Cf[:, 0:257], ALU.add, ["pb0", "ml_Cf"], ["ml_Cf"])
            C.ts("dve", Cf[:, 0:257], Cf[:, 0:257], egd[:, i6:i6 + 1], None, ALU.mult, None, ["ml_Cf", "ml_eg"], ["ml_Cf"])
            C.cp("act", Cb[:, 0:257], Cf[:, 0:257], ["ml_Cf"], ["ml_Cb"])
            pt = K["ptr"][1]
            for j in range(2):
                C.tr(pt[:, j, :], hm[:, 128 * j:128 * j + 128], K["ident_bf"][:], ["ml_hm", "k_ident_bf"], ["ptr1"])
            C.cp("act", hst[:, :, sl], pt[:, 0:2, :], ["ptr1"], ["ml_hst"])
            yield
        for j in range(2):
            C.dma("sp", hT_d[256 * h + 128 * j:256 * h + 128 * j + 128, :], hst[:, j, :], ["ml_hst"], ["hT_d"])

    for _ in prep(0):
        pass
    for h in range(NH):
        _interleave(chunks(h), prep(h + 1) if h + 1 < NH else _empty())


def mixer_moba(C, K, es, xT, hT_d, w_in_d):
    pb = K["pb"]
    w_in_v = w_in_d.rearrange("(kc p) c -> p kc c", p=128)
    wq = C.sb(es, "mo_wq", [128, 16, 128], BF16)
    wk = C.sb(es, "mo_wk", [128, 16, 128], BF16)
    wv = C.sb(es, "mo_wv", [128, 16, 128], BF16)
    zf = C.sb(es, "mo_zf", [128, 512], F32)
    qf = C.sb(es, "mo_qf", [128, S], F32)
    kf = C.sb(es, "mo_kf", [128, S], F32)
    qTs = [C.sb(es, f"mo_qT{i}", [128, S], BF16) for i in range(2)]
    kTs = [C.sb(es, f"mo_kT{i}", [128, S], BF16) for i in range(2)]
    vts = [C.sb(es, f"mo_vt{i}", [128, 16, 128], BF16) for i in range(2)]
    negTs = [C.sb(es, f"mo_negT{i}", [128, S], BF16) for i in range(2)]
    t1 = C.sb(es, "mo_t1", [32, 512], F32)
    kbar = C.sb(es, "mo_kbar", [128, 8], F32)
    gm = C.sb(es, "mo_gm", [128, 16, 8], F32)
    top8 = C.sb(es, "mo_top8", [128, 8], F32)
    negm = C.sb(es, "mo_negm", [128, 16, 8], F32)
    pT = [C.sb(es, f"mo_pT{i}", [128, 512], BF16) for i in range(2)]
    rdn = C.sb(es, "mo_rdn", [128, 512], F32)
    hst = [C.sb(es, f"mo_hst{i}", [128, 512], BF16) for i in range(2)]
    cos, sin = K["rope_cos"], K["rope_sin"]
    for i in range(2):
        C.memset("pool", negTs[i][:], 0.0, [f"mo_negT{i}"])
    scale = 128 ** -0.5
    NH = 12

    def prep(h):
        p = h % 2
        qT, kT, vt, negT = qTs[p], kTs[p], vts[p], negTs[p]
        C.dma("pool", wq[:], w_in_v[:, :, 128 * h:128 * h + 128], [], ["mo_wq"])
        C.dma("pool", wk[:], w_in_v[:, :, 1536 + 128 * h:1536 + 128 * h + 128], [], ["mo_wk"])
        C.dma("pool", wv[:], w_in_v[:, :, 3072 + 128 * h:3072 + 128 * h + 128], [], ["mo_wv"])
        for (w_, wtag, df, dftag, db, dbtag) in ((wq, "mo_wq", qf, "mo_qf", qT, f"mo_qT{p}"), (wk, "mo_wk", kf, "mo_kf", kT, f"mo_kT{p}")):
            for tg in range(4):
                ts_ = slice(512 * tg, 512 * tg + 512)
                for kc in range(16):
                    C.mm(pb[4][:], w_[:, kc, :], xT[:, kc, ts_], kc == 0, kc == 15, [wtag, "xT"], ["pb4"])
                C.cp("act", zf[:], pb[4][:], ["pb4"], ["mo_zf"])
                yield
                C.cp("pool", df[:, ts_], zf[:], ["mo_zf"], [dftag])
                C.mm(pb[5][:], K["rot_f"][:], zf[:], True, True, ["k_rot_f", "mo_zf"], ["pb5"])
                C.tt("dve", t1[:], pb[5][0:32, :], sin[:, ts_], ALU.mult, ["pb5", "k_rope_sin"], ["mo_t1"])
                C.tt("pool", df[0:32, ts_], zf[0:32, :], cos[:, ts_], ALU.mult, ["mo_zf", "k_rope_cos", dftag], [dftag])
                C.tt("dve", df[0:32, ts_], df[0:32, ts_], t1[:], ALU.add, [dftag, "mo_t1"], [dftag])
                yield
            C.cp("act", db[:], df[:], [dftag], [dbtag])
            yield
        for tt in range(NT):
            b = pb[4 + tt % 2]
            bk = f"pb{4 + tt % 2}"
            for kc in range(16):
                C.mm(b[:, 0:128], xT[:, kc, 128 * tt:128 * tt + 128], wv[:, kc, :], kc == 0, kc == 15, ["xT", "mo_wv"], [bk])
            C.cp("dve", vt[:, tt, :], b[:, 0:128], [bk], [f"mo_vt{p}"])
            if tt % 2 == 1:
                yield
        C.P.op("dve", lambda e: e.tensor_reduce(out=kbar[:], in_=kf[:].rearrange("p (n b) -> p n b", b=256), axis=AX.X, op=ALU.add),
               reads=["mo_kf"], writes=["mo_kbar"])
        C.ts("dve", kbar[:], kbar[:], 1.0 / 256.0, None, ALU.mult, None, ["mo_kbar"], ["mo_kbar"])
        yield
        for tt in range(NT):
            C.mm(pb[4][:, 8 * tt:8 * tt + 8], qf[:, 128 * tt:128 * tt + 128], kbar[:], True, True, ["mo_qf", "mo_kbar"], ["pb4"])
        C.tt("dve", gm[:], pb[4][:, 0:128].rearrange("p (a b) -> p a b", b=8), K["moba_neg"][:], ALU.add, ["pb4", "k_moba_neg"], ["mo_gm"])
        yield
        for tt in range(NT):
            C.P.op("dve", lambda e, tt=tt: e.max(out=top8[:], in_=gm[:, tt, :]), reads=["mo_gm"], writes=["mo_top8"])
            C.ts("dve", negm[:, tt, :], gm[:, tt, :], top8[:, 2:3], None, ALU.is_ge, None, ["mo_gm", "mo_top8"], ["mo_negm"])
            if tt % 2 == 1:
                yield
        C.tt("dve", negm[:], negm[:], K["moba_valid"][:], ALU.mult, ["mo_negm", "k_moba_valid"], ["mo_negm"])
        C.tt("dve", negm[:], negm[:], K["moba_valid"][:], ALU.subtract, ["mo_negm", "k_moba_valid"], ["mo_negm"])
        C.ts("dve", negm[:], negm[:], BIGM, None, ALU.mult, None, ["mo_negm"], ["mo_negm"])
        yield
        for g in range(4):
            for j in range(4):
                tt = 4 * g + j
                C.tr(pb[5][0:8, 128 * j:128 * j + 128], negm[:, tt, :], K["ident_f"][:], ["mo_negm", "k_ident_f"], ["pb5"])
            C.cp("act", negT[0:8, 512 * g:512 * g + 512], pb[5][0:8, :], ["pb5"], [f"mo_negT{p}"])
            yield

    def attn(h):
        p = h % 2
        qT, kT, vt, negT = qTs[p], kTs[p], vts[p], negTs[p]
        for g in range(4):
            qs = slice(512 * g, 512 * g + 512)
            nt_ = 4 * g + 4
            for t in range(nt_):
                sb_ = pb[2 + t % 2]
                sk = f"pb{2 + t % 2}"
                p_ = pT[t % 2]
                pk = f"mo_pT{t % 2}"
                C.mm(sb_[:], kT[:, 128 * t:128 * t + 128], qT[:, qs], True, False, [f"mo_kT{p}", f"mo_qT{p}"], [sk])
                C.mm(sb_[:], K["esel_bf"][:, t // 2, :], negT[:, qs], False, True, ["k_esel_bf", f"mo_negT{p}"], [sk])
                C.act(p_[:], sb_[:], AF.Exp, [sk], [pk], scale=scale)
                if t >= 4 * g:
                    C.tt("pool", p_[:], p_[:], K["causal_bf"][:, t - 4 * g, :], ALU.mult, [pk, "k_causal_bf"], [pk])
                C.mm(pb[0][:], vt[:, t, :], p_[:], t == 0, t == nt_ - 1, [f"mo_vt{p}", pk], ["pb0"])
                C.mm(pb[1][:], K["ones_bf"][:], p_[:], t == 0, t == nt_ - 1, ["k_ones_bf", pk], ["pb1"])
                yield
            C.recip(rdn[:], pb[1][:], ["pb1"], ["mo_rdn"])
            hs = hst[g % 2]
            C.tt("dve", hs[:], pb[0][:], rdn[:], ALU.mult, ["pb0", "mo_rdn"], [f"mo_hst{g%2}"])
            C.dma("sp", hT_d[128 * h:128 * h + 128, qs], hs[:], [f"mo_hst{g%2}"], ["hT_d"])
            yield

    for _ in prep(0):
        pass
    for h in range(NH):
        _interleave(attn(h), prep(h + 1) if h + 1 < NH else _empty())


def post_mixer(C, K, es, hT_d, xres_d, wout_d, lnp_d, wr_d, br_d, x1_d, xg_d, idx_d, gk_d):
    pb = K["pb"]
    hcT = C.sb(es, "pm_hcT", [128, 16, S], BF16)
    hv = hT_d.rearrange("(kc p) t -> p kc t", p=128)
    for q4 in range(4):
        C.dma("sp", hcT[:, 4 * q4:4 * q4 + 4, :], hv[:, 4 * q4:4 * q4 + 4, :], ["hT_d"], ["pm_hcT"])
    wout = C.sb(es, "pm_wout", [128, 16, D], BF16)
    wv_ = wout_d.rearrange("(kc p) c -> p kc c", p=128)
    for cg in range(4):
        C.dma("pool", wout[:, :, 512 * cg:512 * cg + 512], wv_[:, :, 512 * cg:512 * cg + 512], [], ["pm_wout"])
    gbc = C.sb(es, "pm_g", [128, D], F32)
    bbc = C.sb(es, "pm_b", [128, D], F32)
    C.dma("sp", gbc[:], lnp_d[0], [], ["lnp"])
    C.dma("sp", bbc[:], lnp_d[1], [], ["lnp"])
    wr = C.sb(es, "pm_wr", [128, 16, NE], F32)
    C.dma("sp", wr[:], wr_d.rearrange("(kc p) e -> p kc e", p=128), [], ["pm_wr"])
    br = C.sb(es, "pm_br", [128, NE], F32)
    C.dma("sp", br[:], br_d, [], ["pm_br"])
    x1s = [C.sb(es, f"pm_x1{i}", [128, D], F32) for i in range(2)]
    x1bs = [C.sb(es, f"pm_x1b{i}", [128, D], BF16) for i in range(2)]
    x1T = C.sb(es, "pm_x1T", [128, 16, 128], F32)
    lg = C.sb(es, "pm_lg", [128, NE], F32)
    top8 = C.sb(es, "pm_top8", [128, 8], F32)
    sel = C.sb(es, "pm_sel", [128, NE], F32)
    selb = C.sb(es, "pm_selb", [128, NE], BF16)
    ex = C.sb(es, "pm_ex", [128, NE], F32)
    sm = C.sb(es, "pm_sm", [128, 4], F32)
    gmv = C.sb(es, "pm_gm", [128, NE], F32)
    cnt = C.sb(es, "pm_cnt", [128, NE], F32)
    pos = C.sb(es, "pm_pos", [128, NE], F32)
    rix = C.sb(es, "pm_rix", [128, NE], F32)
    val = C.sb(es, "pm_val", [128, NE], F32)
    junk = C.sb(es, "pm_junk", [128, NE], F32)
    rks = [C.sb(es, f"pm_rk{i}", [128, 4], F32) for i in range(2)]
    gks = [C.sb(es, f"pm_gk{i}", [128, 4], F32) for i in range(2)]
    ris = [C.sb(es, f"pm_ri{i}", [128, 4], I32) for i in range(2)]
    C.memset("pool", cnt[:], 0.0, ["pm_cnt"])

    def stage_a(tt):
        p = tt % 2
        x1, x1b = x1s[p], x1bs[p]
        xk, xbk = f"pm_x1{p}", f"pm_x1b{p}"
        rows = slice(128 * tt, 128 * tt + 128)
        C.dma("sp", x1[:], xres_d[rows, :], [], [xk])
        for cg in range(4):
            for kc in range(16):
                C.mm(pb[cg][:], hcT[:, kc, rows], wout[:, kc, 512 * cg:512 * cg + 512], kc == 0, kc == 15, ["pm_hcT", "pm_wout"], [f"pb{cg}"])
            C.stt(x1[:, 512 * cg:512 * cg + 512], x1[:, 512 * cg:512 * cg + 512], ALPHA, pb[cg][:], ALU.mult, ALU.add, [xk, f"pb{cg}"], [xk])
            yield
        layer_norm_tile(C, K, x1[:], gbc[:], bbc[:], x1[:], xk, xk)
        yield
        C.dma("sp", x1_d[rows, :], x1[:], [xk], ["x1_d"])
        C.cp("act", x1b[:], x1[:], [xk], [xbk])
        yield

    def stage_b(tt):
        p = tt % 2
        x1, x1b = x1s[p], x1bs[p]
        xk, xbk = f"pm_x1{p}", f"pm_x1b{p}"
        rk, gk, ri = rks[p], gks[p], ris[p]
        rows = slice(128 * tt, 128 * tt + 128)
        for kc in range(16):
            b = pb[4 + kc % 2]
            bk = f"pb{4 + kc % 2}"
            C.tr(b[:, 0:128], x1[:, 128 * kc:128 * kc + 128], K["ident_f"][:], [xk, "k_ident_f"], [bk])
            C.cp("act" if kc % 2 else "dve", x1T[:, kc, :], b[:, 0:128], [bk], [f"pm_x1T{kc}"])
            if kc % 4 == 3:
                yield
        for kc in range(16):
            C.mm(pb[4][:, 256:256 + NE], x1T[:, kc, :], wr[:, kc, :], kc == 0, kc == 15, [f"pm_x1T{kc}", "pm_wr"], ["pb4"])
        C.tt("dve", lg[:], pb[4][:, 256:256 + NE], br[:], ALU.add, ["pb4", "pm_br"], ["pm_lg"])
        yield
        C.P.op("dve", lambda e: e.max(out=top8[:], in_=lg[:]), reads=["pm_lg"], writes=["pm_top8"])
        C.ts("dve", sel[:], lg[:], top8[:, 3:4], None, ALU.is_ge, None, ["pm_lg", "pm_top8"], ["pm_sel"])
        C.cp("pool", selb[:], sel[:], ["pm_sel"], ["pm_selb"])
        C.ts("dve", sm[:, 0:1], top8[:, 0:1], -1.0, None, ALU.mult, None, ["pm_top8"], ["pm_sm0"])
        C.act(ex[:], lg[:], AF.Exp, ["pm_lg", "pm_sm0"], ["pm_ex"], bias=sm[:, 0:1], scale=1.0)
        yield
        C.stt(ex[:], ex[:], 1.0, sel[:], ALU.mult, ALU.mult, ["pm_ex", "pm_sel"], ["pm_ex"], accum_out=sm[:, 1:2])
        C.recip(sm[:, 2:3], sm[:, 1:2], ["pm_ex"], ["pm_sm2"])
        C.ts("dve", gmv[:], ex[:], sm[:, 2:3], None, ALU.mult, None, ["pm_ex", "pm_sm2"], ["pm_gm"])
        C.mm(pb[5][:, 256:256 + NE], K["tri_strict_bf"][:], selb[:], True, True, ["k_tri_strict_bf", "pm_selb"], ["pb5"])
        C.mm(pb[5][:, 320:320 + NE], K["ones_bf"][:], selb[:], True, True, ["k_ones_bf", "pm_selb"], ["pb5"])
        yield
        C.tt("dve", pos[:], pb[5][:, 256:256 + NE], cnt[:], ALU.add, ["pb5", "pm_cnt"], ["pm_pos"])
        C.tt("dve", cnt[:], pb[5][:, 320:320 + NE], cnt[:], ALU.add, ["pb5", "pm_cnt"], ["pm_cnt"])
        C.ts("dve", val[:], pos[:], CAP - 0.5, None, ALU.is_lt, None, ["pm_pos"], ["pm_val"])
        C.tt("dve", val[:], val[:], sel[:], ALU.mult, ["pm_val", "pm_sel"], ["pm_val"])
        C.tt("dve", rix[:], pos[:], K["ebase"][:], ALU.add, ["pm_pos", "k_ebase"], ["pm_rix"])
        yield
        C.ts("dve", rix[:], rix[:], -float(NROWS), None, ALU.add, None, ["pm_rix"], ["pm_rix"])
        C.tt("dve", rix[:], rix[:], val[:], ALU.mult, ["pm_rix", "pm_val"], ["pm_rix"])
        C.ts("dve", rix[:], rix[:], float(NROWS), None, ALU.add, None, ["pm_rix"], ["pm_rix"])
        yield
        for k in range(4):
            C.stt(junk[:], lg[:], top8[:, k:k + 1], rix[:], ALU.is_equal, ALU.mult, ["pm_lg", "pm_top8", "pm_rix"], ["pm_junk"], accum_out=rk[:, k:k + 1])
            C.stt(junk[:], lg[:], top8[:, k:k + 1], gmv[:], ALU.is_equal, ALU.mult, ["pm_lg", "pm_top8", "pm_gm"], ["pm_junk"], accum_out=gk[:, k:k + 1])
            if k % 2 == 1:
                yield
        C.cp("dve", ri[:], rk[:], ["pm_junk"], [f"pm_ri{p}"])
        C.dma("sp", idx_d[rows, :], ri[:], [f"pm_ri{p}"], ["idx_d"])
        C.dma("sp", gk_d[rows, :], gk[:], ["pm_junk"], ["gk_d"])
        yield
        for k in range(4):
            C.P.op("pool", lambda e, k=k, ri=ri, x1b=x1b: e.indirect_dma_start(
                out=xg_d[:, :], out_offset=bass.IndirectOffsetOnAxis(ap=ri[:, k:k + 1], axis=0),
                in_=x1b[:, :], in_offset=None, bounds_check=C.P.bc_reg, oob_is_err=False),
                reads=[f"pm_ri{p}", xbk], writes=["xg_d"], dma=True)
        yield

    for _ in stage_a(0):
        pass
    for tt in range(NT):
        _interleave(stage_b(tt), stage_a(tt + 1) if tt + 1 < NT else _empty())


def combine_ln2(C, K, es, x1_d, y_d, idx_d, gk_d, lnp_d, out_d, xT=None):
    gbc = C.sb(es, "cb_g", [128, D], F32)
    bbc = C.sb(es, "cb_b", [128, D], F32)
    C.dma("sp", gbc[:], lnp_d[0], [], ["lnp"])
    C.dma("sp", bbc[:], lnp_d[1], [], ["lnp"])
    accs = [C.sb(es, f"cb_acc{i}", [128, D], F32) for i in range(2)]
    yks = [[C.sb(es, f"cb_yk{i}_{k}", [128, D], F32) for k in range(4)] for i in range(2)]
    x2 = C.sb(es, "cb_x2", [128, D], F32)
    x2b = C.sb(es, "cb_x2b", [128, D], BF16)
    ris = [C.sb(es, f"cb_ri{i}", [128, 4], I32) for i in range(2)]
    gks = [C.sb(es, f"cb_gk{i}", [128, 4], F32) for i in range(2)]
    outs = []
    for tt in range(NT):
        p = tt % 2
        acc, yk, ri, gk = accs[p], yks[p], ris[p], gks[p]
        rows = slice(128 * tt, 128 * tt + 128)
        C.dma("sp", acc[:], x1_d[rows, :], ["x1_d"], [f"cb_acc{p}"])
        C.dma("sp", ri[:], idx_d[rows, :], ["idx_d"], [f"cb_ri{p}"])
        C.dma("sp", gk[:], gk_d[rows, :], ["gk_d"], [f"cb_gk{p}"])
        for k in range(4):
            C.memset("pool", yk[k][:], 0.0, [f"cb_yk{p}_{k}"])
            C.P.op("pool", lambda e, k=k, yk=yk, ri=ri: e.indirect_dma_start(
                out=yk[k][:, :], out_offset=None, in_=y_d[:, :],
                in_offset=bass.IndirectOffsetOnAxis(ap=ri[:, k:k + 1], axis=0),
                bounds_check=C.P.bc_reg, oob_is_err=False),
                reads=[f"cb_ri{p}", "y_d"], writes=[f"cb_yk{p}_{k}"], dma=True)
        C.ts("dve", acc[:], acc[:], ALPHA, None, ALU.mult, None, [f"cb_acc{p}"], [f"cb_acc{p}"])
        for k in range(4):
            C.stt(acc[:], yk[k][:], gk[:, k:k + 1], acc[:], ALU.mult, ALU.add, [f"cb_yk{p}_{k}", f"cb_gk{p}", f"cb_acc{p}"], [f"cb_acc{p}"])
        layer_norm_tile(C, K, acc[:], gbc[:], bbc[:], x2[:], f"cb_acc{p}", "cb_x2")
        outs.append(C.dma("sp", out_d[rows, :], x2[:], ["cb_x2"], ["x2_d"]))
        if xT is not None:
            C.cp("act", x2b[:], x2[:], ["cb_x2"], ["cb_x2b"])
            emit_xT(C, K, x2b, tt, xT, "cb_x2b")
    return outs


def _build_dp(kind):
    nc = bass.Bass("TRN2", target_bir_lowering=False)
    C = Ctx(nc)
    fin = []
    with C.es:
        es0 = C.root
        names = ["ident_bf", "ident_f", "ones_bf", "ones_f", "tri_incl_f", "tri_strict_bf", "ebase"]
        if kind == "k3":
            names += ["causal_bf", "rope_cos", "rope_sin", "rot_f", "moba_valid", "moba_neg", "esel_bf"]
        K = load_consts(C, es0, names)
        npb = 8 if kind == "k3" else 8
        K["ptr"] = [C.ps(f"ptr{i}", [128, 4, 128], BF16) for i in range(2)]
        K["pb"] = [C.ps(f"pb{i}", [128, 512], F32) for i in range(6)]
        for nm, shp in (("ln_st", [128, 24]), ("ln_mv", [128, 2]), ("ln_sd", [128, 1]), ("eps", [128, 1]), ("one", [128, 1]), ("lnk", [128, 1])):
            K[nm] = C.sb(es0, "s_" + nm, shp, F32)
        C.memset("pool", K["eps"][:], EPS, ["c_eps"])
        C.memset("pool", K["one"][:], 1.0, ["c_one"])
        C.memset("pool", K["lnk"][:], float(-0.5 * np.log(128.0)), ["c_lnk"])
        di = lambda n, s, dt=F32: C.dram(n, s, dt, "ExternalInput")
        do = lambda n, s, dt=F32: C.dram(n, s, dt, "ExternalOutput")
        if kind in ("k1", "k3"):
            hT_d = C.dram("hT_d", [D, S], BF16, "Internal")
            x1_d = do("x1_o", [S, D])
            xg_d = do("xg_o", [NROWS, D], BF16)
            idx_d = do("idx_o", [S, 4], I32)
            gk_d = do("gk_o", [S, 4])
            mem_d = di("mem", [256, D])
            wkv_d = di("w_mem_kv", [D, 1024])
            wout_d = di("w_out", [D, D])
            ln1_d = di("ln1", [2, 128, D])
            wr_d = di("w_router", [D, NE])
            br_d = di("b_router", [128, NE])
        if kind == "k1":
            x_d = di("x", [S, D])
            w_in_d = di("w_in", [D, 5132])
            convw_d = di("convw", [128, 12, 4])
            convb_d = di("convb", [128, 12])
            gbias_d = di("gbias", [128, 12])
            with C.scope(es0) as es:
                xT = C.sb(es, "xT", [128, 16, S], BF16)
                xb = [C.sb(es, f"xb{i}", [128, D], BF16) for i in range(2)]
                for tt in range(NT):
                    C.dma("pool", xb[tt % 2][:], x_d[128 * tt:128 * tt + 128, :], [], [f"xb{tt%2}"])
                    emit_xT(C, K, xb[tt % 2], tt, xT, f"xb{tt%2}")
                with C.scope(es) as es2:
                    mem_attention(C, K, es2, xT, hT_d, mem_d, wkv_d, w_in_d, 4620)
                C.P.barrier()
                with C.scope(es) as es2:
                    mixer_mlstm(C, K, es2, xT, hT_d, w_in_d, convw_d, convb_d, gbias_d)
                C.P.barrier()
            with C.scope(es0) as es:
                post_mixer(C, K, es, hT_d, x_d, wout_d, ln1_d, wr_d, br_d, x1_d, xg_d, idx_d, gk_d)
            fin = list(C.P.dma_last.values())
        if kind in ("k3", "k5"):
            x1i_d = di("x1_i", [S, D])
            y_d = di("y_i", [NROWS, D])
            idxi_d = di("idx_i", [S, 4], I32)
            gki_d = di("gk_i", [S, 4])
            ln2_d = di("ln2", [2, 128, D])
        if kind == "k5":
            out_d = do("out", [S, D])
            with C.scope(es0) as es:
                combine_ln2(C, K, es, x1i_d, y_d, idxi_d, gki_d, ln2_d, out_d)
            fin = list(C.P.dma_last.values())
        if kind == "k3":
            x2_d = C.dram("x2_d", [S, D], F32, "Internal")
            w_in_d = di("w_in", [D, 5120])
            with C.scope(es0) as es:
                xT = C.sb(es, "xT", [128, 16, S], BF16)
                with C.scope(es) as es2:
                    combine_ln2(C, K, es2, x1i_d, y_d, idxi_d, gki_d, ln2_d, x2_d, xT=xT)
                C.P.barrier()
                with C.scope(es) as es2:
                    mem_attention(C, K, es2, xT, hT_d, mem_d, wkv_d, w_in_d, 4608)
                C.P.barrier()
                with C.scope(es) as es2:
                    mixer_moba(C, K, es2, xT, hT_d, w_in_d)
                C.P.barrier()
            with C.scope(es0) as es:
                post_mixer(C, K, es, hT_d, x2_d, wout_d, ln1_d, wr_d, br_d, x1_d, xg_d, idx_d, gk_d)
            fin = list(C.P.dma_last.values())
        C.P.emit(final_wait_ops=fin)
    return nc


def build_ffn():
    nc = bass.Bass("TRN2", target_bir_lowering=False)
    C = Ctx(nc)
    NSL = NCORES * CAP
    GS = 1024
    NG = NSL // GS
    with C.es:
        es = C.root
        K = load_consts(C, es, ["ident_bf"])
        K["ptr"] = [C.ps(f"ptr{i}", [128, 4, 128], BF16) for i in range(2)]
        K["pb"] = [C.ps(f"pb{i}", [128, 512], F32) for i in range(6)]
        pb = K["pb"]
        xg_d = C.dram("xg_i", [4 * NSL, D], BF16, "ExternalInput")
        wgu_d = C.dram("w_gu", [4, D, 2 * D], F32, "ExternalInput")
        bgu_d = C.dram("b_gu", [4, 128, 32], F32, "ExternalInput")
        wdn_d = C.dram("w_down", [4, D, D], F32, "ExternalInput")
        bdn_d = C.dram("b_down", [4, 128, D], F32, "ExternalInput")
        y_d = C.dram("y_o", [4 * NSL, D], F32, "ExternalOutput")
        wt = [C.sb(es, f"wt{i}", [128, 16, 512], BF16) for i in range(4)]
        xgt = [C.sb(es, f"xgt{i}", [128, D], BF16) for i in range(2)]
        xgT = C.sb(es, "xgT", [128, 16, GS], BF16)
        hidT = C.sb(es, "hidT", [128, 16, GS], BF16)
        bgu = C.sb(es, "bgu", [128, 32], F32)
        bdn = C.sb(es, "bdn", [128, D], F32)
        hgc = C.sb(es, "hgc", [128, 512], F32)
        sg = C.sb(es, "sg", [128, 512], F32)
        huc = C.sb(es, "huc", [128, 512], F32)
        yst = [C.sb(es, f"yst{i}", [128, D], F32) for i in range(2)]
        wslot = 0
        it = 0
        for e in range(4):
            wgv = wgu_d[e].rearrange("(kc p) c -> p kc c", p=128)
            wdv = wdn_d[e].rearrange("(kc p) c -> p kc c", p=128)
            C.dma("sp", bgu[:], bgu_d[e], [], ["bgu"])
            C.dma("sp", bdn[:], bdn_d[e], [], ["bdn"])
            for gi in range(NG):
                r0 = e * NSL + gi * GS
                for st in range(GS // 128):
                    xt = xgt[st % 2]
                    C.dma("sp", xt[:], xg_d[r0 + 128 * st:r0 + 128 * st + 128, :], [], [f"xgt{st%2}"])
                    for g in range(4):
                        pt = K["ptr"][g % 2]
                        for j in range(4):
                            kc = 4 * g + j
                            C.tr(pt[:, j, :], xt[:, 128 * kc:128 * kc + 128], K["ident_bf"][:], [f"xgt{st%2}", "k_ident_bf"], [f"ptr{g%2}"])
                        C.cp("dve" if g % 2 == 0 else "act", xgT[:, 4 * g:4 * g + 4, 128 * st:128 * st + 128], pt[:], [f"ptr{g%2}"], ["xgT"])
                for j in range(4):
                    wg_ = wt[wslot % 4]; gk_ = f"wt{wslot % 4}"; wslot += 1
                    wu_ = wt[wslot % 4]; uk_ = f"wt{wslot % 4}"; wslot += 1
                    C.dma("pool", wg_[:], wgv[:, :, 512 * j:512 * j + 512], [], [gk_])
                    C.dma("pool", wu_[:], wgv[:, :, D + 512 * j:D + 512 * j + 512], [], [uk_])
                    for i in range(4):
                        ffc = 4 * j + i
                        for n in range(GS // 512):
                            ns = slice(512 * n, 512 * n + 512)
                            for kc in range(16):
                                C.mm(pb[0][:], wg_[:, kc, 128 * i:128 * i + 128], xgT[:, kc, ns], kc == 0, kc == 15, [gk_, "xgT"], ["pb0"])
                            for kc in range(16):
                                C.mm(pb[1][:], wu_[:, kc, 128 * i:128 * i + 128], xgT[:, kc, ns], kc == 0, kc == 15, [uk_, "xgT"], ["pb1"])
                            C.ts("dve", hgc[:], pb[0][:], bgu[:, ffc:ffc + 1], 7.0, ALU.add, ALU.min, ["pb0", "bgu"], ["hgc"])
                            C.act(sg[:], hgc[:], AF.Sigmoid, ["hgc"], ["sg"], scale=1.702)
                            C.ts("dve", huc[:], pb[1][:], bgu[:, 16 + ffc:16 + ffc + 1], 7.0, ALU.add, ALU.min, ["pb1", "bgu"], ["huc"])
                            C.ts("dve", huc[:], huc[:], -7.0, 1.0, ALU.max, ALU.add, ["huc"], ["huc"])
                            C.tt("dve", sg[:], sg[:], hgc[:], ALU.mult, ["sg", "hgc"], ["sg"])
                            C.tt("dve", hidT[:, ffc, ns], sg[:], huc[:], ALU.mult, ["sg", "huc"], ["hidT"])
                wd = []
                for dg in range(4):
                    w_ = wt[wslot % 4]; k_ = f"wt{wslot % 4}"; wslot += 1
                    C.dma("pool", w_[:], wdv[:, :, 512 * dg:512 * dg + 512], [], [k_])
                    wd.append((w_, k_))
                for st in range(GS // 128):
                    ys = yst[it % 2]; yk_ = f"yst{it % 2}"; it += 1
                    for dg in range(4):
                        w_, k_ = wd[dg]
                        for kc in range(16):
                            C.mm(pb[2 + dg][:], hidT[:, kc, 128 * st:128 * st + 128], w_[:, kc, :], kc == 0, kc == 15, ["hidT", k_], [f"pb{2+dg}"])
                        C.tt("dve", ys[:, 512 * dg:512 * dg + 512], pb[2 + dg][:], bdn[:, 512 * dg:512 * dg + 512], ALU.add, [f"pb{2+dg}", "bdn"], [yk_])
                    C.dma("sp", y_d[r0 + 128 * st:r0 + 128 * st + 128, :], ys[:], [yk_], ["y_d"])
        C.P.emit(final_wait_ops=list(C.P.dma_last.values()))
    return nc


def ffn_local(C, K, es, xg_d, y_d, wgu_d, bgu_d, wdn_d, bdn_d):
    pb = K["pb"]
    NW = 6
    wt = [C.sb(es, f"wt{i}", [128, 16, 512], BF16) for i in range(NW)]
    xgt = [C.sb(es, f"xgt{i}", [128, D], BF16) for i in range(2)]
    xgT = [C.sb(es, f"xgT{i}", [128, 16, CAP], BF16) for i in range(2)]
    hidT = C.sb(es, "hidT", [128, 16, CAP], BF16)
    bgu = [C.sb(es, f"bgu{i}", [128, 32], F32) for i in range(2)]
    bdn = [C.sb(es, f"bdn{i}", [128, D], F32) for i in range(2)]
    hgc = C.sb(es, "hgc", [128, CAP], F32)
    sg = C.sb(es, "sg", [128, CAP], F32)
    huc = C.sb(es, "huc", [128, CAP], F32)
    yst = [C.sb(es, f"yst{i}", [128, D], F32) for i in range(2)]
    wslot = 0
    it = 0
    xi = 0
    for e in range(NE):
        wgv = wgu_d[e].rearrange("(kc p) c -> p kc c", p=128)
        wdv = wdn_d[e].rearrange("(kc p) c -> p kc c", p=128)
        bg = bgu[e % 2]; bgk = f"bgu{e % 2}"
        bd = bdn[e % 2]; bdk = f"bdn{e % 2}"
        C.dma("sp", bg[:], bgu_d[e], [], [bgk])
        C.dma("sp", bd[:], bdn_d[e].partition_broadcast(128), [], [bdk])
        xT_ = xgT[e % 2]; xk = f"xgT{e % 2}"
        r0 = e * CAP
        for (s0, sn) in SLOT_TILES:
            xt = xgt[xi % 2]; xtk = f"xgt{xi % 2}"; xi += 1
            C.dma("sp", xt[0:sn, :], xg_d[r0 + s0:r0 + s0 + sn, :], [], [xtk])
            for g in range(4):
                pt = K["ptr"][g % 2]
                for j in range(4):
                    kc = 4 * g + j
                    C.tr(pt[:, j, 0:sn], xt[0:sn, 128 * kc:128 * kc + 128], K["ident_bf"][0:sn, 0:sn], [xtk, "k_ident_bf"], [f"ptr{g%2}"])
                C.cp("dve" if g % 2 == 0 else "act", xT_[:, 4 * g:4 * g + 4, s0:s0 + sn], pt[:, :, 0:sn], [f"ptr{g%2}"], [xk])
        for j in range(4):
            wg_ = wt[wslot % NW]; gk_ = f"wt{wslot % NW}"; wslot += 1
            wu_ = wt[wslot % NW]; uk_ = f"wt{wslot % NW}"; wslot += 1
            C.dma("pool", wg_[:], wgv[:, :, 512 * j:512 * j + 512], [], [gk_])
            C.dma("pool", wu_[:], wgv[:, :, D + 512 * j:D + 512 * j + 512], [], [uk_])
            for i in range(4):
                ffc = 4 * j + i
                bA, bB = (0, 1) if ffc % 2 == 0 else (4, 5)
                for kc in range(16):
                    C.mm(pb[bA][:, 0:CAP], wg_[:, kc, 128 * i:128 * i + 128], xT_[:, kc, :], kc == 0, kc == 15, [gk_, xk], [f"pb{bA}"])
                for kc in range(16):
                    C.mm(pb[bB][:, 0:CAP], wu_[:, kc, 128 * i:128 * i + 128], xT_[:, kc, :], kc == 0, kc == 15, [uk_, xk], [f"pb{bB}"])
                C.ts("dve", hgc[:], pb[bA][:, 0:CAP], bg[:, ffc:ffc + 1], 7.0, ALU.add, ALU.min, [f"pb{bA}", bgk], ["hgc"])
                C.act(sg[:], hgc[:], AF.Sigmoid, ["hgc"], ["sg"], scale=1.702)
                C.ts("dve", huc[:], pb[bB][:, 0:CAP], bg[:, 16 + ffc:16 + ffc + 1], 7.0, ALU.add, ALU.min, [f"pb{bB}", bgk], ["huc"])
                C.ts("dve", huc[:], huc[:], -7.0, 1.0, ALU.max, ALU.add, ["huc"], ["huc"])
                C.tt("dve", sg[:], sg[:], hgc[:], ALU.mult, ["sg", "hgc"], ["sg"])
                C.tt("dve", hidT[:, ffc, :], sg[:], huc[:], ALU.mult, ["sg", "huc"], ["hidT"])
        wd = []
        for dg in range(4):
            w_ = wt[wslot % NW]; k_ = f"wt{wslot % NW}"; wslot += 1
            C.dma("pool", w_[:], wdv[:, :, 512 * dg:512 * dg + 512], [], [k_])
            wd.append((w_, k_))
        for (s0, sn) in SLOT_TILES:
            ys = yst[it % 2]; yk_ = f"yst{it % 2}"; it += 1
            for dg in range(4):
                w_, k_ = wd[dg]
                for kc in range(16):
                    C.mm(pb[2 + dg][0:sn, :], hidT[:, kc, s0:s0 + sn], w_[:, kc, :], kc == 0, kc == 15, ["hidT", k_], [f"pb{2+dg}"])
                C.tt("dve", ys[0:sn, 512 * dg:512 * dg + 512], pb[2 + dg][0:sn, :], bd[0:sn, 512 * dg:512 * dg + 512], ALU.add, [f"pb{2+dg}", bdk], [yk_])
            C.dma("sp", y_d[r0 + s0:r0 + s0 + sn, :], ys[0:sn, :], [yk_], ["y_d"])


def build_fused():
    nc = bass.Bass("TRN2", target_bir_lowering=False)
    C = Ctx(nc)
    with C.es:
        es0 = C.root
        names = ["ident_bf", "ident_f", "ones_bf", "ones_f", "tri_incl_f", "tri_strict_bf", "ebase",
                 "causal_bf", "rope_cos", "rope_sin", "rot_f", "moba_valid", "moba_neg", "esel_bf"]
        K = load_consts(C, es0, names)
        K["ptr"] = [C.ps(f"ptr{i}", [128, 4, 128], BF16) for i in range(2)]
        K["pb"] = [C.ps(f"pb{i}", [128, 512], F32) for i in range(6)]
        for nm, shp in (("ln_st", [128, 24]), ("ln_mv", [128, 2]), ("ln_sd", [128, 1]), ("eps", [128, 1]), ("one", [128, 1]), ("lnk", [128, 1])):
            K[nm] = C.sb(es0, "s_" + nm, shp, F32)
        C.memset("pool", K["eps"][:], EPS, ["c_eps"])
        C.memset("pool", K["one"][:], 1.0, ["c_one"])
        C.memset("pool", K["lnk"][:], float(-0.5 * np.log(128.0)), ["c_lnk"])
        di = lambda n, s, dt=F32: C.dram(n, s, dt, "ExternalInput")
        dn = lambda n, s, dt=F32: C.dram(n, s, dt, "Internal")
        x_d = di("x", [S, D]); mem_d = di("mem", [256, D])
        w_in0 = di("w_in0", [D, 5132]); w_in1 = di("w_in1", [D, 5120])
        convw_d = di("convw", [128, 12, 4]); convb_d = di("convb", [128, 12]); gbias_d = di("gbias", [128, 12])
        wkv_d = di("w_mem_kv", [2, D, 1024]); wout_d = di("w_out", [2, D, D])
        ln1_d = di("ln1", [2, 2, 128, D]); ln2_d = di("ln2", [2, 2, 128, D])
        wr_d = di("w_router", [2, D, NE]); br_d = di("b_router", [2, 128, NE])
        wgu_d = di("w_gu", [2, NE, D, 2 * D]); bgu_d = di("b_gu", [2, NE, 128, 32])
        wdn_d = di("w_down", [2, NE, D, D]); bdn_d = di("b_down", [2, NE, D])
        out_d = C.dram("out", [S, D], F32, "ExternalOutput")
        hT_d = dn("hT_d", [D, S], BF16)
        x1_d = dn("x1_d", [S, D]); x2_d = dn("x2_d", [S, D])
        xg_d = dn("xg_d", [NROWS, D], BF16); y_d = dn("y_d", [NROWS, D])
        idx_d = dn("idx_d", [S, 4], I32); gk_d = dn("gk_d", [S, 4])
        with C.scope(es0) as es:
            xT = C.sb(es, "xT", [128, 16, S], BF16)
            with C.scope(es) as es2:
                xb = [C.sb(es2, f"xb{i}", [128, D], BF16) for i in range(2)]
                for tt in range(NT):
                    C.dma("pool", xb[tt % 2][:], x_d[128 * tt:128 * tt + 128, :], [], [f"xb{tt%2}"])
                    emit_xT(C, K, xb[tt % 2], tt, xT, f"xb{tt%2}")
            C.P.barrier()
            with C.scope(es) as es2:
                mem_attention(C, K, es2, xT, hT_d, mem_d, wkv_d[0], w_in0, 4620)
            C.P.barrier()
            with C.scope(es) as es2:
                mixer_mlstm(C, K, es2, xT, hT_d, w_in0, convw_d, convb_d, gbias_d)
            C.P.barrier()
        with C.scope(es0) as es:
            post_mixer(C, K, es, hT_d, x_d, wout_d[0], ln1_d[0], wr_d[0], br_d[0], x1_d, xg_d, idx_d, gk_d)
        C.P.barrier()
        with C.scope(es0) as es:
            ffn_local(C, K, es, xg_d, y_d, wgu_d[0], bgu_d[0], wdn_d[0], bdn_d[0])
        C.P.barrier()
        with C.scope(es0) as es:
            xT = C.sb(es, "xT1", [128, 16, S], BF16)
            with C.scope(es) as es2:
                combine_ln2(C, K, es2, x1_d, y_d, idx_d, gk_d, ln2_d[0], x2_d, xT=xT)
            C.P.barrier()
            with C.scope(es) as es2:
                mem_attention(C, K, es2, xT, hT_d, mem_d, wkv_d[1], w_in1, 4608)
            C.P.barrier()
            with C.scope(es) as es2:
                mixer_moba(C, K, es2, xT, hT_d, w_in1)
            C.P.barrier()
        with C.scope(es0) as es:
            post_mixer(C, K, es, hT_d, x2_d, wout_d[1], ln1_d[1], wr_d[1], br_d[1], x1_d, xg_d, idx_d, gk_d)
        C.P.barrier()
        with C.scope(es0) as es:
            ffn_local(C, K, es, xg_d, y_d, wgu_d[1], bgu_d[1], wdn_d[1], bdn_d[1])
        C.P.barrier()
        with C.scope(es0) as es:
            outs = combine_ln2(C, K, es, x1_d, y_d, idx_d, gk_d, ln2_d[1], out_d)
        C.P.emit(final_wait_ops=list(C.P.dma_last.values()))
    return nc


_PROG_CACHE = {}


def _prog(kind):
    if kind not in _PROG_CACHE:
        _PROG_CACHE[kind] = build_ffn() if kind == "ffn" else (build_fused() if kind == "fused" else _build_dp(kind))
    return _PROG_CACHE[kind]


def _bc(v, n=128):
    return np.ascontiguousarray(np.broadcast_to(np.asarray(v, np.float32)[None], (n,) + tuple(np.shape(v))))


def _run(kind, in_maps):
    nc = _prog(kind)
    res = run_bass_kernel_spmd(nc, in_maps, core_ids=list(range(len(in_maps))))
    return res.results


def _fused_maps(cores, x, mem, mlstm_w_in, mlstm_conv_w, mlstm_conv_b, mlstm_b_igate, mlstm_b_fgate,
                moba_w_in, w_mem_kv, w_out, ln1_g, ln1_b, w_router, b_router, w_gu, b_gu,
                w_down, b_down, ln2_g, ln2_b):
    f32 = np.float32
    cst = host_consts()
    A = lambda a: np.ascontiguousarray(np.asarray(a, f32))
    shared = {
        "w_in0": A(mlstm_w_in[0]), "w_in1": A(moba_w_in[0]),
        "convw": np.ascontiguousarray(A(mlstm_conv_w[0]).reshape(4, 12, 128).transpose(2, 1, 0)),
        "convb": np.ascontiguousarray(A(mlstm_conv_b[0]).reshape(12, 128).T),
        "gbias": _bc(np.concatenate([A(mlstm_b_igate[0]), A(mlstm_b_fgate[0])])),
        "w_mem_kv": A(w_mem_kv), "w_out": A(w_out),
        "ln1": np.stack([np.stack([_bc(ln1_g[i]), _bc(ln1_b[i])], 0) for i in range(2)], 0),
        "ln2": np.stack([np.stack([_bc(ln2_g[i]), _bc(ln2_b[i])], 0) for i in range(2)], 0),
        "w_router": A(w_router), "b_router": np.stack([_bc(b_router[i]) for i in range(2)], 0),
        "w_gu": A(w_gu),
        "b_gu": np.ascontiguousarray(A(b_gu).reshape(2, NE, 32, 128).transpose(0, 1, 3, 2)),
        "w_down": A(w_down), "b_down": A(b_down),
    }
    for n in CONST_SPECS:
        shared["c_" + n] = cst[n]
    maps = []
    for c in cores:
        m = dict(shared)
        m["x"] = A(x[c]); m["mem"] = A(mem[c])
        maps.append(m)
    return maps


def kernel(**inputs):
    maps = _fused_maps(list(range(NCORES)), **inputs)
    nc = _prog("fused")
    res = run_bass_kernel_spmd(nc, maps, core_ids=list(range(NCORES)))
    return np.stack([r["out"] for r in res.results], 0).astype(np.float32)


def kernel_unfused(x, mem, mlstm_w_in, mlstm_conv_w, mlstm_conv_b, mlstm_b_igate, mlstm_b_fgate,
           moba_w_in, w_mem_kv, w_out, ln1_g, ln1_b, w_router, b_router, w_gu, b_gu,
           w_down, b_down, ln2_g, ln2_b):
    f32 = np.float32
    cst = host_consts()

    def consts_for(names):
        return {"c_" + n: cst[n] for n in names}

    base = ["ident_bf", "ident_f", "ones_bf", "ones_f", "tri_incl_f", "tri_strict_bf", "ebase"]
    k3n = base + ["causal_bf", "rope_cos", "rope_sin", "rot_f", "moba_valid", "moba_neg", "esel_bf"]

    def lnp(g, b):
        return np.stack([_bc(g), _bc(b)], 0)

    def dp_common(i):
        return {"w_mem_kv": np.asarray(w_mem_kv[i], f32), "w_out": np.asarray(w_out[i], f32),
                "ln1": lnp(ln1_g[i], ln1_b[i]), "w_router": np.asarray(w_router[i], f32),
                "b_router": _bc(b_router[i])}

    def ffn_maps(i, xg_all):
        xs = np.stack([a.reshape(NE, CAP, D) for a in xg_all], 0)
        maps = []
        for c in range(NCORES):
            sl = slice(4 * c, 4 * c + 4)
            xi = np.ascontiguousarray(xs[:, sl].transpose(1, 0, 2, 3)).reshape(4 * NCORES * CAP, D)
            maps.append({
                "c_ident_bf": cst["ident_bf"], "xg_i": xi,
                "w_gu": np.asarray(w_gu[i, sl], f32),
                "b_gu": np.ascontiguousarray(np.asarray(b_gu[i, sl], f32).reshape(4, 32, 128).transpose(0, 2, 1)),
                "w_down": np.asarray(w_down[i, sl], f32),
                "b_down": np.stack([_bc(b_down[i, e]) for e in range(4 * c, 4 * c + 4)], 0),
            })
        return maps

    def y_back(res):
        ys = np.stack([r["y_o"].reshape(4, NCORES, CAP, D) for r in res], 0)
        out = []
        for s_ in range(NCORES):
            out.append(np.ascontiguousarray(ys[:, :, s_]).reshape(NROWS, D))
        return out

    convw = np.ascontiguousarray(np.asarray(mlstm_conv_w[0], f32).reshape(4, 12, 128).transpose(2, 1, 0))
    convb = np.ascontiguousarray(np.asarray(mlstm_conv_b[0], f32).reshape(12, 128).T)
    gbias = _bc(np.concatenate([np.asarray(mlstm_b_igate[0], f32), np.asarray(mlstm_b_fgate[0], f32)]))
    maps = []
    for c in range(NCORES):
        m = {"x": np.asarray(x[c], f32), "mem": np.asarray(mem[c], f32), "w_in": np.asarray(mlstm_w_in[0], f32),
             "convw": convw, "convb": convb, "gbias": gbias}
        m.update(dp_common(0)); m.update(consts_for(base))
        maps.append(m)
    r1 = _run("k1", maps)
    r2 = _run("ffn", ffn_maps(0, [r["xg_o"] for r in r1]))
    yb = y_back(r2)
    maps = []
    for c in range(NCORES):
        m = {"x1_i": r1[c]["x1_o"], "y_i": yb[c], "idx_i": r1[c]["idx_o"], "gk_i": r1[c]["gk_o"],
             "ln2": lnp(ln2_g[0], ln2_b[0]), "mem": np.asarray(mem[c], f32), "w_in": np.asarray(moba_w_in[0], f32)}
        m.update(dp_common(1)); m.update(consts_for(k3n))
        maps.append(m)
    r3 = _run("k3", maps)
    r4 = _run("ffn", ffn_maps(1, [r["xg_o"] for r in r3]))
    yb = y_back(r4)
    maps = []
    for c in range(NCORES):
        m = {"x1_i": r3[c]["x1_o"], "y_i": yb[c], "idx_i": r3[c]["idx_o"], "gk_i": r3[c]["gk_o"],
             "ln2": lnp(ln2_g[1], ln2_b[1])}
        m.update(consts_for(base))
        maps.append(m)
    r5 = _run("k5", maps)
    return np.stack([r["out"] for r in r5], 0).astype(np.float32)
```
